# Optimizing a Trainium2 kernel written in Bass

```python
import jax
import jax.numpy as jnp
from jax import lax
import numpy as np

D_MODEL = 1024
BATCH = 8
SEQ = 2048
DEPTH = 4

GRID_W = 64
CTX_LEN = 256
D_MIX = D_MODEL
N_GROUPS = 4
GROUP_W = D_MIX // N_GROUPS
HEAD_DIM = 64
NORM_EPS = 1e-6
ROPE_BASE = 10000.0
NEG_INF = -1e30

CONV_W = 4
LRU_BLOCKS = GROUP_W // HEAD_DIM
LRU_BLOCK = GROUP_W // LRU_BLOCKS
LRU_C = 8.0
GDN_HEADS = GROUP_W // HEAD_DIM
GDN_CHUNK = 64
GDN_INV_STEPS = GDN_CHUNK.bit_length() - 2
RET_HEADS = GROUP_W // HEAD_DIM
RET_CHUNK = 64
SWA_HEADS = GROUP_W // HEAD_DIM
SWA_KV_HEADS = 2
SWA_GROUP = SWA_HEADS // SWA_KV_HEADS
SWA_KV_W = SWA_KV_HEADS * HEAD_DIM
WINDOW = 128
SWA_BLOCK = WINDOW

IN_SPLITS = (
    ('lru_x', GROUP_W), ('lru_z', GROUP_W),
    ('gdn_qkv', 3 * GROUP_W), ('gdn_z', GROUP_W), ('gdn_alpha', 2 * GDN_HEADS), ('gdn_beta', 2 * GDN_HEADS),
    ('ret_q', GROUP_W), ('ret_k', GROUP_W), ('ret_v', GROUP_W), ('ret_z', GROUP_W),
    ('swa_q', GROUP_W), ('swa_k', SWA_KV_W), ('swa_v', SWA_KV_W), ('swa_z', GROUP_W),
)
IN_W = sum(w for _, w in IN_SPLITS)

kernel_name = 'hybrid_parallel_group_diffusion_block'


def rmsnorm(x, g):
    xf = x.astype(jnp.float32)
    y = xf * lax.rsqrt(jnp.mean(xf * xf, axis=-1, keepdims=True) + NORM_EPS)
    return (y * g.astype(jnp.float32)).astype(x.dtype)


def l2norm(x):
    return x * lax.rsqrt(jnp.sum(x * x, axis=-1, keepdims=True) + NORM_EPS)


def head_groupnorm(o):
    mu = jnp.mean(o, axis=-1, keepdims=True)
    d = o - mu
    return d * lax.rsqrt(jnp.mean(d * d, axis=-1, keepdims=True) + NORM_EPS)


def split_in(u):
    parts, off = {}, 0
    for name, w in IN_SPLITS:
        parts[name] = u[..., off:off + w]
        off += w
    return parts


def dwconv(x, w):
    k = w.shape[0]
    return lax.conv_general_dilated(
        x, w[:, None, :].astype(x.dtype), window_strides=(1,),
        padding=[(k // 2, k - 1 - k // 2)],
        dimension_numbers=('NWC', 'WIO', 'NWC'), feature_group_count=x.shape[-1])


def rope_freqs(pos, n):
    inv = ROPE_BASE ** (-jnp.arange(0, n, 2, dtype=jnp.float32) / n)
    return pos[:, None] * inv[None, :]


def apply_rope(x, ang):
    half = x.shape[-1] // 2
    cos = jnp.cos(ang)[None, :, None, :]
    sin = jnp.sin(ang)[None, :, None, :]
    x1, x2 = x[..., :half], x[..., half:]
    return jnp.concatenate([x1 * cos - x2 * sin, x1 * sin + x2 * cos], axis=-1).astype(x.dtype)


def _chunks4(t, c):
    bsz, tl, h, d = t.shape
    return t.reshape(bsz, tl // c, c, h, d).transpose(1, 0, 3, 2, 4)


def _chunks3(t, c):
    bsz, tl, h = t.shape
    return t.reshape(bsz, tl // c, c, h).transpose(1, 0, 3, 2)


def _unchunk(o):
    n, bsz, h, c, d = o.shape
    return o.transpose(1, 0, 3, 2, 4).reshape(bsz, n * c, h, d)


def linear_scan(a, b, h0):
    b = b.at[:, 0].add(a[:, 0] * h0)

    def combine(left, right):
        a_l, b_l = left
        a_r, b_r = right
        return a_l * a_r, a_r * b_l + b_r

    _, h = lax.associative_scan(combine, (a, b), axis=1)
    return h


def rglru_dir(u, w_r, b_r, w_i, b_i, lam, h0):
    bsz, t, w = u.shape
    ub = u.reshape(bsz, t, LRU_BLOCKS, LRU_BLOCK)
    r = jax.nn.sigmoid(jnp.einsum('btnc,ncd->btnd', ub, w_r.astype(jnp.float32)).reshape(bsz, t, w) + b_r)
    i = jax.nn.sigmoid(jnp.einsum('btnc,ncd->btnd', ub, w_i.astype(jnp.float32)).reshape(bsz, t, w) + b_i)
    log_a = -LRU_C * r * jax.nn.softplus(-lam.astype(jnp.float32))
    a = jnp.exp(log_a)
    b = jnp.sqrt(-jnp.expm1(2.0 * log_a)) * (i * u)
    return linear_scan(a, b, h0)


def mixer_rglru(u, uc, conv_w, conv_b, w_r, b_r, w_i, b_i, lam, with_ctx):
    f32 = jnp.float32
    x_l, x_c = u['lru_x'], uc['lru_x']
    u_l = (dwconv(x_l, conv_w) + conv_b).astype(f32)
    u_c = (dwconv(x_c, conv_w) + conv_b).astype(f32)
    h0 = jnp.zeros((x_l.shape[0], GROUP_W), f32)
    hc_f = rglru_dir(u_c, w_r[0], b_r[0], w_i[0], b_i[0], lam[0], h0)
    hl_f = rglru_dir(u_l, w_r[0], b_r[0], w_i[0], b_i[0], lam[0], hc_f[:, -1])
    hc_b = rglru_dir(u_c[:, ::-1], w_r[1], b_r[1], w_i[1], b_i[1], lam[1], h0)
    hl_b = rglru_dir(u_l[:, ::-1], w_r[1], b_r[1], w_i[1], b_i[1], lam[1], hc_b[:, -1])[:, ::-1]
    y_l = ((hl_f + hl_b) * jax.nn.silu(u['lru_z'].astype(f32))).astype(x_l.dtype)
    y_c = None
    if with_ctx:
        y_c = ((hc_f + hc_b[:, ::-1]) * jax.nn.silu(uc['lru_z'].astype(f32))).astype(x_c.dtype)
    return y_l, y_c


def unit_lower_inverse(l_mat):
    eye = jnp.eye(l_mat.shape[-1], dtype=l_mat.dtype)
    n = -l_mat
    p = eye + n
    for _ in range(GDN_INV_STEPS):
        n = n @ n
        p = p @ (eye + n)
    return p


def gdn_chunked(q, k, v, g, beta, s0, with_out):
    c = GDN_CHUNK
    ks, vs = _chunks4(k, c), _chunks4(v, c)
    gs, bs = _chunks3(g, c), _chunks3(beta, c)
    gcum = jnp.cumsum(gs, axis=-1)
    idx = jnp.arange(c)
    lower = idx[:, None] >= idx[None, :]
    strict = idx[:, None] > idx[None, :]
    diff = gcum[..., :, None] - gcum[..., None, :]
    decay = jnp.where(lower, jnp.exp(jnp.where(lower, diff, 0.0)), 0.0)
    kb = ks * bs[..., None]
    l_mat = jnp.where(strict, jnp.einsum('nbhid,nbhjd->nbhij', kb, ks) * decay, 0.0)
    t_mat = unit_lower_inverse(l_mat)
    u_val = t_mat @ (vs * bs[..., None])
    w_dec = t_mat @ (kb * jnp.exp(gcum)[..., None])
    k_tail = ks * jnp.exp(gcum[..., -1:] - gcum)[..., None]
    g_tail = jnp.exp(gcum[..., -1])
    if with_out:
        qs = _chunks4(q, c)
        att = jnp.where(lower, jnp.einsum('nbhid,nbhjd->nbhij', qs, ks) * decay, 0.0)
        q_dec = qs * jnp.exp(gcum)[..., None]
        xs = (u_val, w_dec, k_tail, g_tail, att, q_dec)
    else:
        xs = (u_val, w_dec, k_tail, g_tail)

    def step(s, inp):
        u_i, w_i, kt_i, gt_i = inp[0], inp[1], inp[2], inp[3]
        v_new = u_i - jnp.einsum('bhcd,bhde->bhce', w_i, s)
        s_new = s * gt_i[..., None, None] + jnp.einsum('bhcd,bhce->bhde', kt_i, v_new)
        if not with_out:
            return s_new, None
        att_i, qd_i = inp[4], inp[5]
        o = jnp.einsum('bhcd,bhde->bhce', qd_i, s) + jnp.einsum('bhij,bhje->bhie', att_i, v_new)
        return s_new, o

    s_fin, o = lax.scan(step, s0, xs)
    return (_unchunk(o) if with_out else None), s_fin


def gdn_prep(qkv, alpha, beta, conv_w, a_log, dt_bias):
    f32 = jnp.float32
    bsz, t, _ = qkv.shape
    h = jax.nn.silu(dwconv(qkv, conv_w).astype(f32)).reshape(bsz, t, 3, GDN_HEADS, HEAD_DIM)
    q = l2norm(h[:, :, 0]) * HEAD_DIM ** -0.5
    k = l2norm(h[:, :, 1])
    v = h[:, :, 2]
    g = -jnp.exp(a_log.astype(f32)) * jax.nn.softplus(
        alpha.astype(f32).reshape(bsz, t, 2, GDN_HEADS) + dt_bias.astype(f32))
    b = jax.nn.sigmoid(beta.astype(f32).reshape(bsz, t, 2, GDN_HEADS))
    return q, k, v, g, b


def mixer_gdn(u, uc, conv_w, a_log, dt_bias, norm_g, with_ctx):
    ql, kl, vl, gl, bl = gdn_prep(u['gdn_qkv'], u['gdn_alpha'], u['gdn_beta'], conv_w, a_log, dt_bias)
    qc, kc, vc, gc, bc = gdn_prep(uc['gdn_qkv'], uc['gdn_alpha'], uc['gdn_beta'], conv_w, a_log, dt_bias)
    s0 = jnp.zeros((ql.shape[0], GDN_HEADS, HEAD_DIM, HEAD_DIM), jnp.float32)
    rev = lambda t: t[:, ::-1]
    oc_f, sc_f = gdn_chunked(qc, kc, vc, gc[:, :, 0], bc[:, :, 0], s0, with_ctx)
    ol_f, _ = gdn_chunked(ql, kl, vl, gl[:, :, 0], bl[:, :, 0], sc_f, True)
    oc_b, sc_b = gdn_chunked(rev(qc), rev(kc), rev(vc), rev(gc[:, :, 1]), rev(bc[:, :, 1]), s0, with_ctx)
    ol_b, _ = gdn_chunked(rev(ql), rev(kl), rev(vl), rev(gl[:, :, 1]), rev(bl[:, :, 1]), sc_b, True)

    def finish(o, z):
        bsz, t = z.shape[0], z.shape[1]
        return (rmsnorm(o, norm_g).reshape(bsz, t, GROUP_W) * jax.nn.silu(z.astype(jnp.float32))).astype(z.dtype)

    y_l = finish(ol_f + rev(ol_b), u['gdn_z'])
    y_c = finish(oc_f + rev(oc_b), uc['gdn_z']) if with_ctx else None
    return y_l, y_c


def retention_chunked(q, k, v, log_gamma, s0, with_out):
    c = RET_CHUNK
    idx = jnp.arange(c, dtype=jnp.float32)
    diff = idx[:, None] - idx[None, :]
    lower = diff >= 0
    dmat = jnp.where(lower, jnp.exp(jnp.where(lower, diff, 0.0)[None] * log_gamma[:, None, None]), 0.0)
    q_dec = jnp.exp((idx + 1.0)[None, :] * log_gamma[:, None])
    k_dec = jnp.exp((c - 1.0 - idx)[None, :] * log_gamma[:, None])
    c_dec = jnp.exp(c * log_gamma)
    ks, vs = _chunks4(k, c), _chunks4(v, c)
    xs = (ks, vs, _chunks4(q, c)) if with_out else (ks, vs)

    def step(s, inp):
        k_i, v_i = inp[0], inp[1]
        s_new = s * c_dec[None, :, None, None] + jnp.einsum('bhcd,bhce->bhde', k_i * k_dec[None, :, :, None], v_i)
        if not with_out:
            return s_new, None
        q_i = inp[2]
        att = jnp.einsum('bhid,bhjd->bhij', q_i, k_i) * dmat[None]
        o = jnp.einsum('bhcd,bhde->bhce', q_i * q_dec[None, :, :, None], s) + jnp.einsum('bhij,bhje->bhie', att, v_i)
        return s_new, o

    s_fin, o = lax.scan(step, s0, xs)
    return (_unchunk(o) if with_out else None), s_fin


def mixer_retention(u, uc, decay_logit, ang1d, with_ctx):
    f32 = jnp.float32
    heads = lambda t: t.astype(f32).reshape(t.shape[0], t.shape[1], RET_HEADS, HEAD_DIM)
    kscale = HEAD_DIM ** -0.5
    ql = apply_rope(heads(u['ret_q']), ang1d)
    kl = apply_rope(heads(u['ret_k']), ang1d) * kscale
    vl = heads(u['ret_v'])
    qc = heads(uc['ret_q']) if with_ctx else None
    kc = heads(uc['ret_k']) * kscale
    vc = heads(uc['ret_v'])
    lg = jax.nn.log_sigmoid(decay_logit.astype(f32))
    s0 = jnp.zeros((ql.shape[0], RET_HEADS, HEAD_DIM, HEAD_DIM), f32)
    rev = lambda t: None if t is None else t[:, ::-1]
    oc_f, s_f = retention_chunked(qc, kc, vc, lg[0], s0, with_ctx)
    ol_f, _ = retention_chunked(ql, kl, vl, lg[0], s_f, True)
    oc_b, s_b = retention_chunked(rev(qc), rev(kc), rev(vc), lg[1], s0, with_ctx)
    ol_b, _ = retention_chunked(rev(ql), rev(kl), rev(vl), lg[1], s_b, True)

    def finish(o, z):
        bsz, t = z.shape[0], z.shape[1]
        return (head_groupnorm(o).reshape(bsz, t, GROUP_W) * jax.nn.silu(z.astype(f32))).astype(z.dtype)

    y_l = finish(ol_f + ol_b[:, ::-1], u['ret_z'])
    y_c = finish(oc_f + oc_b[:, ::-1], uc['ret_z']) if with_ctx else None
    return y_l, y_c


def mixer_swa(u, uc, sink, ang2d, with_ctx):
    f32 = jnp.float32
    bsz, t, _ = u['swa_q'].shape
    lc = uc['swa_k'].shape[1]
    blk = SWA_BLOCK
    nb = t // blk
    scale = HEAD_DIM ** -0.5
    q = apply_rope(u['swa_q'].reshape(bsz, t, SWA_HEADS, HEAD_DIM), ang2d) * scale
    k = apply_rope(u['swa_k'].reshape(bsz, t, SWA_KV_HEADS, HEAD_DIM), ang2d)
    v = u['swa_v'].reshape(bsz, t, SWA_KV_HEADS, HEAD_DIM)
    kc = uc['swa_k'].reshape(bsz, lc, SWA_KV_HEADS, HEAD_DIM)
    vc = uc['swa_v'].reshape(bsz, lc, SWA_KV_HEADS, HEAD_DIM)
    sink_g = sink.astype(f32).reshape(SWA_KV_HEADS, SWA_GROUP)

    qb = q.reshape(bsz, nb, blk, SWA_KV_HEADS, SWA_GROUP, HEAD_DIM)
    pad = ((0, 0), (blk, blk), (0, 0), (0, 0))
    kp = jnp.pad(k, pad).reshape(bsz, nb + 2, blk, SWA_KV_HEADS, HEAD_DIM)
    vp = jnp.pad(v, pad).reshape(bsz, nb + 2, blk, SWA_KV_HEADS, HEAD_DIM)
    kw = jnp.concatenate([kp[:, :-2], kp[:, 1:-1], kp[:, 2:]], axis=2)
    vw = jnp.concatenate([vp[:, :-2], vp[:, 1:-1], vp[:, 2:]], axis=2)
    qi = jnp.arange(blk)[:, None]
    kj = jnp.arange(3 * blk)[None, :]
    in_win = jnp.abs(kj - blk - qi) <= WINDOW
    kpos = jnp.arange(nb)[:, None] * blk - blk + jnp.arange(3 * blk)[None, :]
    valid = (kpos >= 0) & (kpos < t)
    mask = in_win[None, :, :] & valid[:, None, :]
    s_loc = jnp.einsum('bnqhgd,bnkhd->bnhgqk', qb, kw).astype(f32)
    s_loc = jnp.where(mask[None, :, None, None], s_loc, NEG_INF)
    s_ctx = jnp.einsum('bnqhgd,bchd->bnhgqc', qb, kc).astype(f32)
    s_snk = jnp.broadcast_to(sink_g[None, None, :, :, None, None], s_loc.shape[:-1] + (1,))
    p = jax.nn.softmax(jnp.concatenate([s_loc, s_ctx, s_snk], axis=-1), axis=-1).astype(v.dtype)
    o = (jnp.einsum('bnhgqk,bnkhd->bnqhgd', p[..., :3 * blk], vw)
         + jnp.einsum('bnhgqc,bchd->bnqhgd', p[..., 3 * blk:3 * blk + lc], vc))
    z_l = u['swa_z']
    y_l = (o.reshape(bsz, t, GROUP_W).astype(f32) * jax.nn.silu(z_l.astype(f32))).astype(z_l.dtype)
    y_c = None
    if with_ctx:
        qcx = uc['swa_q'].reshape(bsz, lc, SWA_KV_HEADS, SWA_GROUP, HEAD_DIM) * scale
        s_c = jnp.einsum('bqhgd,bkhd->bhgqk', qcx, kc).astype(f32)
        s_cs = jnp.broadcast_to(sink_g[None, :, :, None, None], s_c.shape[:-1] + (1,))
        p_c = jax.nn.softmax(jnp.concatenate([s_c, s_cs], axis=-1), axis=-1).astype(vc.dtype)
        o_c = jnp.einsum('bhgqk,bkhd->bqhgd', p_c[..., :lc], vc)
        z_c = uc['swa_z']
        y_c = (o_c.reshape(bsz, lc, GROUP_W).astype(f32) * jax.nn.silu(z_c.astype(f32))).astype(z_c.dtype)
    return y_l, y_c


def setup_inputs(seed: int = 0) -> dict:
    key = jax.random.key(seed)
    ks = jax.random.split(key, 24)
    f32 = jnp.float32
    nrm = lambda k, shape, s: jax.random.normal(k, shape, f32) * s
    x = nrm(ks[0], (BATCH, SEQ, D_MODEL), 1.0)
    c = nrm(ks[1], (BATCH, D_MODEL), 1.0)
    ctx = nrm(ks[2], (BATCH, CTX_LEN, D_MODEL), 1.0)
    c_ctx = nrm(ks[3], (D_MODEL,), 1.0)
    w_mod = nrm(ks[4], (DEPTH, D_MODEL, 3 * D_MODEL), 0.5 * D_MODEL ** -0.5)
    b_mod = nrm(ks[5], (DEPTH, 3 * D_MODEL), 0.02)
    pre_norm_g = 1.0 + nrm(ks[6], (DEPTH, D_MODEL), 0.02)
    post_norm_g = 1.0 + nrm(ks[7], (DEPTH, D_MODEL), 0.02)
    w_in = nrm(ks[8], (DEPTH, D_MODEL, IN_W), D_MODEL ** -0.5)
    w_out = nrm(ks[9], (DEPTH, D_MIX, D_MODEL), D_MIX ** -0.5)
    lru_conv_w = nrm(ks[10], (DEPTH, CONV_W, GROUP_W), CONV_W ** -0.5)
    lru_conv_b = nrm(ks[11], (DEPTH, GROUP_W), 0.02)
    lru_w_r = nrm(ks[12], (DEPTH, 2, LRU_BLOCKS, LRU_BLOCK, LRU_BLOCK), LRU_BLOCK ** -0.5)
    lru_b_r = nrm(ks[13], (DEPTH, 2, GROUP_W), 0.02)
    lru_w_i = nrm(ks[14], (DEPTH, 2, LRU_BLOCKS, LRU_BLOCK, LRU_BLOCK), LRU_BLOCK ** -0.5)
    lru_b_i = nrm(ks[15], (DEPTH, 2, GROUP_W), 0.02)
    a0 = jax.random.uniform(ks[16], (DEPTH, 2, GROUP_W), f32, 0.9, 0.999)
    lru_lambda = jnp.log(a0) - jnp.log1p(-a0)
    gdn_conv_w = nrm(ks[17], (DEPTH, CONV_W, 3 * GROUP_W), CONV_W ** -0.5)
    gdn_a_log = jnp.log(jax.random.uniform(ks[18], (DEPTH, 2, GDN_HEADS), f32, 1.0, 16.0))
    dt = jnp.exp(jax.random.uniform(ks[19], (DEPTH, 2, GDN_HEADS), f32, np.log(1e-3), np.log(1e-1)))
    gdn_dt_bias = dt + jnp.log(-jnp.expm1(-dt))
    gdn_norm_g = 1.0 + nrm(ks[20], (DEPTH, HEAD_DIM), 0.02)
    ret_base = jnp.log(2.0 ** (5.0 + jnp.arange(RET_HEADS, dtype=f32)) - 1.0)
    ret_decay_logit = ret_base + nrm(ks[21], (DEPTH, 2, RET_HEADS), 0.1)
    swa_sink = nrm(ks[22], (DEPTH, SWA_HEADS), 0.5)
    return {'x': x, 'c': c, 'ctx': ctx, 'c_ctx': c_ctx, 'w_mod': w_mod, 'b_mod': b_mod,
            'pre_norm_g': pre_norm_g, 'post_norm_g': post_norm_g, 'w_in': w_in, 'w_out': w_out,
            'lru_conv_w': lru_conv_w, 'lru_conv_b': lru_conv_b, 'lru_w_r': lru_w_r, 'lru_b_r': lru_b_r,
            'lru_w_i': lru_w_i, 'lru_b_i': lru_b_i, 'lru_lambda': lru_lambda,
            'gdn_conv_w': gdn_conv_w, 'gdn_a_log': gdn_a_log, 'gdn_dt_bias': gdn_dt_bias,
            'gdn_norm_g': gdn_norm_g, 'ret_decay_logit': ret_decay_logit, 'swa_sink': swa_sink}


def reference(x, c, ctx, c_ctx, w_mod, b_mod, pre_norm_g, post_norm_g, w_in, w_out,
              lru_conv_w, lru_conv_b, lru_w_r, lru_b_r, lru_w_i, lru_b_i, lru_lambda,
              gdn_conv_w, gdn_a_log, gdn_dt_bias, gdn_norm_g, ret_decay_logit, swa_sink):
    bsz, t, _ = x.shape
    rows = t // GRID_W
    row = jnp.repeat(jnp.arange(rows, dtype=jnp.float32), GRID_W)
    col = jnp.tile(jnp.arange(GRID_W, dtype=jnp.float32), rows)
    ang2d = jnp.concatenate([rope_freqs(row, HEAD_DIM // 2), rope_freqs(col, HEAD_DIM // 2)], axis=-1)
    ang1d = rope_freqs(jnp.arange(t, dtype=jnp.float32), HEAD_DIM)
    s_lat = jax.nn.silu(c)
    s_ctx = jax.nn.silu(c_ctx)
    xc = ctx
    for l in range(DEPTH):
        with_ctx = l < DEPTH - 1
        shift, scale, gate = jnp.split((s_lat @ w_mod[l] + b_mod[l])[:, None, :], 3, axis=-1)
        shift_c, scale_c, gate_c = jnp.split(s_ctx @ w_mod[l] + b_mod[l], 3, axis=-1)
        h = rmsnorm(x, pre_norm_g[l]) * (1 + scale) + shift
        hc = rmsnorm(xc, pre_norm_g[l]) * (1 + scale_c) + shift_c
        u = split_in(h @ w_in[l])
        uc = split_in(hc @ w_in[l])
        ya_l, ya_c = mixer_rglru(u, uc, lru_conv_w[l], lru_conv_b[l], lru_w_r[l], lru_b_r[l],
                                 lru_w_i[l], lru_b_i[l], lru_lambda[l], with_ctx)
        yb_l, yb_c = mixer_gdn(u, uc, gdn_conv_w[l], gdn_a_log[l], gdn_dt_bias[l], gdn_norm_g[l], with_ctx)
        yr_l, yr_c = mixer_retention(u, uc, ret_decay_logit[l], ang1d, with_ctx)
        yd_l, yd_c = mixer_swa(u, uc, swa_sink[l], ang2d, with_ctx)
        y = jnp.concatenate([ya_l, yb_l, yr_l, yd_l], axis=-1) @ w_out[l]
        if with_ctx:
            yc = jnp.concatenate([ya_c, yb_c, yr_c, yd_c], axis=-1) @ w_out[l]
            xc = xc + gate_c * rmsnorm(yc, post_norm_g[l])
        x = x + gate * rmsnorm(y, post_norm_g[l])
    return x
```

```python
import numpy as np
from contextlib import ExitStack
import ml_dtypes
import concourse.bass as bass
import concourse.mybir as mybir
from concourse.bass_utils import run_bass_kernel_spmd

F32 = mybir.dt.float32
BF16 = mybir.dt.bfloat16
AF = mybir.ActivationFunctionType
ALU = mybir.AluOpType
AX = mybir.AxisListType

D = 1024
T = 2048
LC = 256
NT = T + LC
NCH = NT // 128
DEPTH = 4
EPS = 1e-6
ENGS = ("pe", "act", "dve", "pool", "sp")


class Res:
    __slots__ = ("name", "w", "r")

    def __init__(self, name=""):
        self.name = name
        self.w = None
        self.r = {}


class DmaGroup:
    def __init__(self, sem, idx, base):
        self.sem = sem
        self.n = 0
        self.key = ("g", idx)
        self.base = base

    @property
    def total(self):
        return self.base + 16 * self.n


class Prog:
    def __init__(self, nc, stack, nchan=24):
        self.nc = nc
        self.stack = stack
        self.ops = {e: [] for e in ENGS}
        self.count = {e: 0 for e in ENGS}
        self.waited = {e: {} for e in ENGS}
        self.sem = {e: stack.enter_context(nc.semaphore("sem_" + e)) for e in ENGS}
        self.chan_sem = [stack.enter_context(nc.semaphore("semc%d" % i)) for i in range(nchan)]
        self.chan_last = [None] * nchan
        self.chan_rr = 0
        self.groups = []
        self.gmap = {}
        self.open_groups = []

    def group(self, dedicated=False):
        if dedicated:
            sem = self.stack.enter_context(self.nc.semaphore("semd%d" % len(self.groups)))
            g = DmaGroup(sem, len(self.groups), 0)
            g.prev = None
            g.issued = set()
            self.groups.append(g)
            self.gmap[g.key] = g
            self.open_groups.append(g)
            return g
        ch = self.chan_rr
        self.chan_rr = (self.chan_rr + 1) % len(self.chan_sem)
        prev = self.chan_last[ch]
        base = prev.total if prev is not None else 0
        g = DmaGroup(self.chan_sem[ch], len(self.groups), base)
        g.prev = prev
        g.issued = set()
        self.chan_last[ch] = g
        self.groups.append(g)
        self.gmap[g.key] = g
        self.open_groups.append(g)
        return g

    def _deps(self, eng, reads, writes):
        deps = {}

        def add(k, v, same_ok):
            if k == eng and not same_ok and eng == "pe":
                return
            if k in deps:
                if isinstance(v, int):
                    deps[k] = max(deps[k], v)
            else:
                deps[k] = v

        for r in reads:
            if r.w is not None:
                add(r.w[0], r.w[1], True)
        for r in writes:
            if r.w is not None:
                add(r.w[0], r.w[1], False)
            for k, v in r.r.items():
                add(k, v, False)
        out = []
        wd = self.waited[eng]
        for k, v in deps.items():
            if isinstance(k, tuple):
                if wd.get(k):
                    continue
                wd[k] = True
                out.append((k, None))
            else:
                if wd.get(k, 0) >= v:
                    continue
                wd[k] = v
                out.append((k, v))
        return out

    def op(self, eng, fn, reads=(), writes=()):
        waits = self._deps(eng, reads, writes)
        self.count[eng] += 1
        idx = self.count[eng]
        self.ops[eng].append((fn, waits, None))
        for r in reads:
            r.r[eng] = idx
        for r in writes:
            r.w = (eng, idx)
            r.r = {}
        return idx

    def dma(self, eng, group, fn, reads=(), writes=()):
        waits = self._deps(eng, reads, writes)
        if eng not in group.issued:
            group.issued.add(eng)
            if group.prev is not None and not self.waited[eng].get(group.prev.key):
                self.waited[eng][group.prev.key] = True
                waits.append((group.prev.key, None))
        group.n += 1
        self.ops[eng].append((fn, waits, group))
        for r in reads:
            r.r[group.key] = None
        for r in writes:
            r.w = (group.key, None)
            r.r = {}

    def wait_group(self, eng, group):
        self.ops[eng].append((None, [(group.key, None)], None))

    def barrier(self):
        comp = ("pe", "act", "dve", "pool")
        for e in ENGS:
            waits = []
            for f in comp:
                if f != e and self.count[f] > self.waited[e].get(f, 0):
                    self.waited[e][f] = self.count[f]
                    waits.append((f, self.count[f]))
            for g in self.open_groups:
                if g.n > 0 and not self.waited[e].get(g.key):
                    self.waited[e][g.key] = True
                    waits.append((g.key, None))
            if waits:
                self.ops[e].append((None, waits, None))
        self.open_groups = [g for g in self.open_groups if g.n == 0]

    def replay(self):
        nc = self.nc

        def run(name, e):
            own = self.sem[name]
            for fn, waits, group in self.ops[name]:
                for k, v in waits:
                    if isinstance(k, tuple):
                        g = self.gmap[k]
                        e.wait_ge(g.sem, g.total)
                    else:
                        e.wait_ge(self.sem[k], v)
                if fn is None:
                    continue
                ins = fn(e)
                if group is not None:
                    ins.then_inc(group.sem, 16)
                else:
                    ins.then_inc(own, 1)

        with nc.Block() as block:
            @block.tensor
            def _(e):
                run("pe", e)

            @block.scalar
            def _(e):
                run("act", e)

            @block.vector
            def _(e):
                run("dve", e)

            @block.gpsimd
            def _(e):
                run("pool", e)

            @block.sync
            def _(e):
                run("sp", e)


def _sw(cols, nheads):
    c = np.asarray(cols).reshape(nheads, 2, 32)
    return c[:, ::-1, :].reshape(-1)


O_LRUX, O_LRUZ, O_GQKV, O_GZ, O_GA, O_GB = 0, 256, 512, 1280, 1536, 1544
O_RQ, O_RK, O_RV, O_RZ = 1552, 1808, 2064, 2320
O_SQ, O_SK, O_SV, O_SZ = 2576, 2832, 2960, 3088
AR = np.arange


def _colperm():
    A = np.concatenate([AR(O_LRUX, O_LRUX + 256), AR(O_LRUZ, O_LRUZ + 256)])
    B = np.concatenate([AR(O_GQKV, O_GQKV + 768), AR(O_GA, O_GA + 16)])
    Bz = AR(O_GZ, O_GZ + 256)
    rq, rk = AR(O_RQ, O_RQ + 256), AR(O_RK, O_RK + 256)
    C = np.concatenate([rq, _sw(rq, 4), rk, _sw(rk, 4), AR(O_RV, O_RV + 256)])
    Cz = AR(O_RZ, O_RZ + 256)
    sq = AR(O_SQ, O_SQ + 256)
    sk = AR(O_SK, O_SK + 128)
    kd = np.concatenate([sk[0:64], sk[0:64], sk[64:128], sk[64:128]])
    Dm = np.concatenate([sq, _sw(sq, 4), kd, _sw(kd, 4), AR(O_SV, O_SV + 128)])
    Dz = AR(O_SZ, O_SZ + 256)
    parts = [A, B, Bz, C, Cz, Dm, Dz]
    offs = np.cumsum([0] + [len(p) for p in parts])
    return np.concatenate(parts), offs


COLPERM, COLOFF = _colperm()
NCOL = int(COLOFF[-1])
WA, WB, WBZ, WC, WCZ, WD, WDZ = [int(v) for v in COLOFF[:7]]
NCP = 54
NRS = 92


def _rope_tables():
    inv64 = 10000.0 ** (-np.arange(0, 64, 2, dtype=np.float32) / 64)
    ang1 = np.arange(T, dtype=np.float32)[:, None] * inv64[None, :]
    inv32 = 10000.0 ** (-np.arange(0, 32, 2, dtype=np.float32) / 32)
    row = np.repeat(np.arange(T // 64, dtype=np.float32), 64)
    col = np.tile(np.arange(64, dtype=np.float32), T // 64)
    ang2 = np.concatenate([row[:, None] * inv32[None, :], col[:, None] * inv32[None, :]], axis=-1)
    out = np.zeros((4, 128, T), np.float32)
    for i, ang in enumerate((ang1, ang2)):
        c = np.cos(ang).T
        s = np.sin(ang).T
        ctab = np.concatenate([c, c, c, c], axis=0)
        stab = np.concatenate([-s, s, -s, s], axis=0)
        out[2 * i] = ctab
        out[2 * i + 1] = stab
    return out.astype(ml_dtypes.bfloat16)


def _prep_inputs(inp, sharded=False):
    L = DEPTH
    f = lambda a: np.ascontiguousarray(np.asarray(a, dtype=np.float32))
    w_in_p = f(inp["w_in"][:, :, COLPERM])
    colp = np.zeros((L, 128, NCP), np.float32)
    fm = lambda v: np.asarray(v).reshape(-1, 128).T
    for l in range(L):
        c = 0
        colp[l, :, c:c + 8] = fm(inp["pre_norm_g"][l]); c += 8
        for k in range(4):
            colp[l, :, c:c + 2] = fm(inp["lru_conv_w"][l, k]); c += 2
        colp[l, :, c:c + 2] = fm(inp["lru_conv_b"][l]); c += 2
        for nm in ("lru_b_r", "lru_b_i", "lru_lambda"):
            for d in range(2):
                colp[l, :, c:c + 2] = fm(inp[nm][l, d]); c += 2
        for k in range(4):
            colp[l, :, c:c + 6] = fm(inp["gdn_conv_w"][l, k]); c += 6
        assert c == NCP
    rows = np.zeros((L, NRS), np.float32)
    for l in range(L):
        rows[l, 0:8] = np.asarray(inp["gdn_a_log"][l]).reshape(-1)
        rows[l, 8:16] = np.asarray(inp["gdn_dt_bias"][l]).reshape(-1)
        rows[l, 16:80] = np.asarray(inp["gdn_norm_g"][l])
        rows[l, 80:88] = np.asarray(inp["ret_decay_logit"][l]).reshape(-1)
        rows[l, 88:92] = np.asarray(inp["swa_sink"][l])
    wg = np.ascontiguousarray(np.stack([f(inp["lru_w_r"]), f(inp["lru_w_i"])], axis=1))
    shared = {
        "w_mod": f(inp["w_mod"]), "b_mod": f(inp["b_mod"]), "post_g": f(inp["post_norm_g"]),
        "w_in": w_in_p, "w_out": f(inp["w_out"]), "colp": colp, "rows": rows, "lruw": wg,
        "rope": _rope_tables(),
        "bmodc": np.ascontiguousarray(f(inp["b_mod"]).reshape(L, 24, 128).transpose(0, 2, 1)),
    }
    maps = []
    for b in range(8):
        sv = np.zeros((128, 16), np.float32)
        sv[:, 0:8] = fm(inp["c"][b])
        sv[:, 8:16] = fm(inp["c_ctx"])
        m = dict(shared)
        m["x"] = f(inp["x"][b])
        m["ctx"] = f(inp["ctx"][b])
        m["svec"] = sv
        if sharded:
            for k in ("w_mod", "w_in", "w_out"):
                m[k] = np.ascontiguousarray(shared[k][:, b * 128:(b + 1) * 128, :])
        maps.append(m)
    return maps


class Tl:
    __slots__ = ("a", "r")

    def __init__(self, a, r):
        self.a = a
        self.r = r


def _prod(s):
    n = 1
    for v in s:
        n *= v
    return n


class Arena:
    def __init__(self, nc, st, nwords, name="arena"):
        self.t = st.enter_context(nc.sbuf_tensor(name, [128, nwords], F32))
        self.nwords = nwords
        self.off = 0
        self.peak = 0

    def alloc(self, shape, dtype, name=""):
        n = _prod(shape[1:])
        words = n if dtype == F32 else (n + 1) // 2
        words = (words + 7) // 8 * 8
        assert self.off + words <= self.nwords, ("arena overflow", name, self.off, words, self.nwords)
        ap = self.t[:, self.off:self.off + words]
        if dtype != F32:
            ap = ap.bitcast(dtype)
        ap = ap[:, 0:n]
        if len(shape) == 3:
            ap = ap.rearrange("p (a b) -> p a b", a=shape[1])
        elif len(shape) == 4:
            ap = ap.rearrange("p (a b c) -> p a b c", a=shape[1], b=shape[2])
        if not hasattr(self, "log"):
            self.log = {}
        self.log[name] = (self.off, tuple(shape), "f32" if dtype == F32 else "bf16")
        self.off += words
        self.peak = max(self.peak, self.off)
        return Tl(ap, Res(name))

    def mark(self):
        return self.off

    def reset(self, m):
        self.off = m


BLKS = [(0, 512), (512, 512), (1024, 512), (1536, 512), (2048, 256)]


def build_nc(n_layers=DEPTH, mixers="ABCD", debug=False, first_from_scratch=False, final_layer=True, phases="ABD", slim=False, sharded=False):
    nc = bass.Bass("TRN2", target_bir_lowering=False)
    DEPTH = n_layers if slim else 4
    dt_in = lambda name, shape, dt=F32: nc.dram_tensor(name, shape, dt, kind="ExternalInput").ap()
    x_d = dt_in("x", [T, D])
    ctx_d = dt_in("ctx", [LC, D])
    svec_d = dt_in("svec", [128, 16])
    KS = 128 if sharded else D
    wmod_in = dt_in("w_mod", [DEPTH, KS, 3 * D])
    bmod_d = dt_in("b_mod", [DEPTH, 3 * D])
    bmodc_d = dt_in("bmodc", [DEPTH, 128, 24])
    postg_d = dt_in("post_g", [DEPTH, D])
    win_in = dt_in("w_in", [DEPTH, KS, NCOL])
    wout_in = dt_in("w_out", [DEPTH, KS, D])
    if sharded:
        wsh = {k: nc.dram_tensor("wsh_" + k, [DEPTH, 128, n], F32, kind="Internal").ap() for k, n in (("mod", 3 * D), ("in", NCOL), ("out", D))}
        wfull = {k: nc.dram_tensor("wfull_" + k, [DEPTH, D, n], F32, kind="Internal").ap() for k, n in (("mod", 3 * D), ("in", NCOL), ("out", D))}
        wmod_d, win_d, wout_d = wfull["mod"], wfull["in"], wfull["out"]
    else:
        wmod_d, win_d, wout_d = wmod_in, win_in, wout_in
    colp_d = dt_in("colp", [DEPTH, 128, NCP])
    rows_d = dt_in("rows", [DEPTH, NRS])
    lruw_d = dt_in("lruw", [DEPTH, 2, 2, 4, 64, 64])
    rope_d = dt_in("rope", [4, 128, T], BF16)
    out_d = nc.dram_tensor("out", [T, D], F32, kind="ExternalOutput").ap()
    xs_d = nc.dram_tensor("xs", [NT, D], F32, kind="Internal").ap()
    ycat_d = nc.dram_tensor("ycat_s", [8, 128, NT], BF16, kind="Internal").ap()
    if debug:
        dbg_h = nc.dram_tensor("dbg_h", [8, 128, NT], BF16, kind="ExternalOutput").ap()
        dbg_y = nc.dram_tensor("dbg_y", [8, 128, NT], BF16, kind="ExternalOutput").ap()
        dbg_xs = nc.dram_tensor("dbg_xs", [NT, D], F32, kind="ExternalOutput").ap()

    with ExitStack() as st:
        P = Prog(nc, st)
        sbt = lambda name, shape, dt=F32: Tl(st.enter_context(nc.sbuf_tensor("sb_" + name, shape, dt))[:], Res(name))
        banks = [Tl(st.enter_context(nc.psum_tensor("bank%d" % i, [128, 512], F32))[:], Res("bank%d" % i)) for i in range(8)]
        bank_rr = [0]

        def nb():
            b = banks[bank_rr[0]]
            bank_rr[0] = (bank_rr[0] + 1) % 8
            return b

        def ACT(out, in_, func, R, W, bias=None, scale=None, accum=None):
            kw = {}
            if bias is not None:
                kw["bias"] = bias
            if scale is not None:
                kw["scale"] = scale
            if accum is not None:
                kw["accum_out"] = accum
            P.op("act", lambda e: e.activation(out=out, in_=in_, func=func, **kw), R, W)

        def TT(eng, out, in0, in1, op, R, W):
            P.op(eng, lambda e: e.tensor_tensor(out=out, in0=in0, in1=in1, op=op), R, W)

        def TS(eng, out, in0, s1, op0, R, W, s2=None, op1=None):
            if op1 is None:
                P.op(eng, lambda e: e.tensor_scalar(out=out, in0=in0, scalar1=s1, scalar2=None, op0=op0), R, W)
            else:
                P.op(eng, lambda e: e.tensor_scalar(out=out, in0=in0, scalar1=s1, scalar2=s2, op0=op0, op1=op1), R, W)

        def STT(eng, out, in0, scalar, in1, op0, op1, R, W):
            P.op(eng, lambda e: e.scalar_tensor_tensor(out=out, in0=in0, scalar=scalar, in1=in1, op0=op0, op1=op1), R, W)

        def CP(eng, out, in_, R, W):
            if eng == "act":
                P.op("act", lambda e: e.activation(out=out, in_=in_, func=AF.Copy), R, W)
            else:
                P.op(eng, lambda e: e.tensor_copy(out=out, in_=in_), R, W)

        def MSET(eng, out, val, W):
            P.op(eng, lambda e: e.memset(out, val), [], W)

        def MM(out, lhsT, rhs, start, stop, R, W):
            P.op("pe", lambda e: e.matmul(out, lhsT=lhsT, rhs=rhs, start=start, stop=stop), R, W)

        def TR(out, in_, ident, R, W):
            P.op("pe", lambda e: e.transpose(out=out, in_=in_, identity=ident), R, W)

        def DMA(eng, out, in_, R, W, g=None):
            if g is None:
                g = P.group()
            P.dma(eng, g, lambda e: e.dma_start(out=out, in_=in_), R, W)
            return g

        def SCAN(out, d0, d1, init, R, W):
            P.op("dve", lambda e: e.tensor_tensor_scan(out=out, data0=d0, data1=d1, initial=init, op0=ALU.mult, op1=ALU.add), R, W)

        def rev(ap2d):
            n = ap2d.shape[1]
            return bass.AP(ap2d.tensor, ap2d.offset + (n - 1), [list(ap2d.ap[0]), [-1, n]])

        def bc(ap, shape, axis):
            return ap.unsqueeze(axis).to_broadcast(shape)

        wres = [{k: Res("w_%s_%d" % (k, l_)) for k in ("mod", "in", "out")} for l_ in range(DEPTH)]
        if sharded:
            srcs = {"mod": wmod_in, "in": win_in, "out": wout_in}
            shres = {}
            for l_ in range(n_layers):
                for k in ("mod", "in", "out"):
                    shres[(l_, k)] = Res("sh")
                    DMA("sp", wsh[k][l_], srcs[k][l_], [], [shres[(l_, k)]])
            for l_ in range(n_layers):
                for k in ("mod", "in", "out"):
                    g_ = P.group()
                    P.dma("pool", g_, lambda e, i_=wsh[k][l_], o_=wfull[k][l_]: e.collective_compute(
                        "AllGather", op=ALU.bypass, replica_groups=[list(range(8))], ins=[i_], outs=[o_]),
                        [shres[(l_, k)]], [wres[l_][k]])

        ones_f = sbt("ones_f", [128, 128])
        ident_f = sbt("ident_f", [128, 128])
        ident_b = sbt("ident_b", [128, 128], BF16)
        cst = sbt("cst", [128, 4])
        MSET("pool", ones_f.a, 1.0, [ones_f.r])
        MSET("pool", cst.a[:, 0:1], EPS, [cst.r])
        MSET("pool", cst.a[:, 1:2], 1.0, [cst.r])
        MSET("pool", cst.a[:, 2:3], -1.0, [cst.r])
        MSET("pool", cst.a[:, 3:4], 0.0, [cst.r])

        def aff(out_t, pattern, cm, op, base=0, fill=0.0, src=None):
            src = src or ones_f
            P.op("pool", lambda e: e.affine_select(out=out_t.a, in_=src.a, pattern=pattern, compare_op=op, fill=fill, base=base, channel_multiplier=cm), [src.r], [out_t.r])

        aff(ident_f, [[-1, 128]], 1, ALU.is_equal)
        CP("dve", ident_b.a, ident_f.a, [ident_f.r], [ident_b.r])
        maskF = sbt("maskF", [128, 128])
        maskB = sbt("maskB", [128, 128])
        blockmask = sbt("blockmask", [128, 128])
        aff(maskF, [[1, 128]], -1, ALU.is_ge)
        aff(maskB, [[-1, 128]], 1, ALU.is_ge)
        MSET("pool", blockmask.a, 0.0, [blockmask.r])
        MSET("pool", blockmask.a[0:64, 0:64], 1.0, [blockmask.r])
        MSET("pool", blockmask.a[64:128, 64:128], 1.0, [blockmask.r])
        blockones_b = sbt("blockones_b", [128, 128], BF16)
        CP("dve", blockones_b.a, blockmask.a, [blockmask.r], [blockones_b.r])
        offdiag = sbt("offdiag", [128, 128])
        TT("dve", offdiag.a, ones_f.a, ident_f.a, ALU.subtract, [ones_f.r, ident_f.r], [offdiag.r])
        maskneg = sbt("maskneg", [128, 2, 4, 128])
        for d_, mk_ in ((0, maskF), (1, maskB)):
            TS("dve", maskneg.a[:, d_], bc(mk_.a, [128, 4, 128], 1), -1.0, ALU.add, [mk_.r], [maskneg.r], s2=30000.0, op1=ALU.mult)
        bd = []
        Ebuf = sbt("Ebuf", [128, 128])
        for si, sz in enumerate((8, 16, 32, 64)):
            E = Ebuf
            aff(E, [[1, 128]], -sz, ALU.is_ge)
            P.op("pool", lambda e, E=E, sz=sz: e.affine_select(out=E.a, in_=E.a, pattern=[[-1, 128]], compare_op=ALU.is_ge, fill=0.0, base=sz - 1, channel_multiplier=sz), [E.r], [E.r])
            ng = 128 // sz
            bb = nb()
            MM(bb.a[:, 0:128], E.a[0:ng, :], E.a[0:ng, :], True, True, [E.r], [bb.r])
            m_ = sbt("bd%d" % sz, [128, 128])
            CP("dve", m_.a, bb.a[:, 0:128], [bb.r], [m_.r])
            bd.append(m_)
        bdm = [bd[0]]
        offm = []
        for si in range(4):
            o_ = sbt("off%d" % si, [128, 128])
            hi = bd[si + 1] if si < 3 else ones_f
            TT("dve", o_.a, hi.a, bd[si].a, ALU.subtract, [hi.r, bd[si].r], [o_.r])
            offm.append(o_)
        iota_i = sbt("iota_i", [128, 128], mybir.dt.int32)
        iota_ij = sbt("iota_ij", [128, 128])
        P.op("pool", lambda e: e.iota(out=iota_i.a, pattern=[[1, 128]], base=0, channel_multiplier=-1), [], [iota_i.r])
        CP("dve", iota_ij.a, iota_i.a, [iota_i.r], [iota_ij.r])
        pidx_i = sbt("pidx_i", [128, 1], mybir.dt.int32)
        pidx = sbt("pidx", [128, 1])
        P.op("pool", lambda e: e.iota(out=pidx_i.a, pattern=[[0, 1]], base=0, channel_multiplier=1), [], [pidx_i.r])
        CP("dve", pidx.a, pidx_i.a, [pidx_i.r], [pidx.r])
        rowt = sbt("rowt", [128, NRS])
        s2 = sbt("s2", [128, 8, 2])
        svt = sbt("svt", [128, 16])
        DMA("sp", svt.a, svec_d[:, :], [], [svt.r])
        ACT(s2.a.rearrange("p k w -> p w k"), svt.a.rearrange("p (w k) -> p w k", w=2), AF.Silu, [svt.r], [s2.r])
        modc = sbt("modc", [128, 16, 2])
        acol = sbt("acol", [128, 8, 2])
        grow = [sbt("grow%d" % w, [128, D]) for w in range(2)]
        colp = sbt("colp", [128, NCP])
        ss = sbt("ss", [128, 2 * NCH])
        rstd = sbt("rstd", [128, 2 * NCH])
        rope = sbt("rope", [128, 4, T], BF16)
        for i in range(4):
            DMA("sp", rope.a[:, i, :], rope_d[i], [], [rope.r])

        ycat_res = Res("ycat_dram")
        arena = Arena(nc, st, 43200)
        hT = arena.alloc([128, 8, NT], BF16, "hT")
        base_mark = arena.mark()

        def load_cast(dst_ap, dst_res, src_rows_ap, ncols, stage, wr=None):
            DMA("sp", stage.a[:, :ncols], src_rows_ap, [wr] if wr is not None else [], [stage.r])
            CP("pool", dst_ap, stage.a[:, :ncols], [stage.r], [dst_res])

        for l in range(n_layers):
            last = final_layer and (l == n_layers - 1)
            from_x = (l == 0) and not first_from_scratch
            arena.reset(base_mark)
            P.barrier()
            DMA("sp", colp.a, colp_d[l], [], [colp.r])
            DMA("sp", rowt.a, rows_d[l, :].partition_broadcast(128), [], [rowt.r])
            m0 = arena.mark()
            wmb = [arena.alloc([128, 8, 512], F32, "wm%d" % i) for i in range(2)]
            bgrow = arena.alloc([128, D], F32, "bgrow")
            pgrow = arena.alloc([128, D], F32, "pgrow")
            bmc = arena.alloc([128, 24], F32, "bmc")
            sbc = arena.alloc([128, 8, 2, 128], F32, "sbc")
            CP("dve", sbc.a, bc(s2.a, [128, 8, 2, 128], 3), [s2.r], [sbc.r])
            DMA("sp", bgrow.a, bmod_d[l, 2 * D:3 * D].partition_broadcast(128), [], [bgrow.r])
            DMA("sp", pgrow.a, postg_d[l, :].partition_broadcast(128), [], [pgrow.r])
            DMA("sp", bmc.a, bmodc_d[l], [], [bmc.r])
            pmod = nb()
            pmv = pmod.a[:, 0:32].rearrange("p (j w) -> p j w", w=2)
            wsrc = wmod_d[l].rearrange("(k p) n -> p k n", p=128)
            for cg in range(6):
                wm = wmb[cg % 2]
                DMA("sp", wm.a, wsrc[:, :, cg * 512:(cg + 1) * 512], [wres[l]["mod"]], [wm.r])
                if cg < 4:
                    for jj in range(4):
                        j = cg * 4 + jj
                        for k in range(8):
                            MM(pmv[:, j, :], wm.a[:, k, jj * 128:(jj + 1) * 128], s2.a[:, k, :], k == 0, k == 7, [wm.r, s2.r], [pmod.r])
                else:
                    cs = slice((cg - 4) * 512, (cg - 3) * 512)
                    for w in range(2):
                        pg = nb()
                        for k in range(8):
                            MM(pg.a, sbc.a[:, k, w, :], wm.a[:, k, :], k == 0, k == 7, [wm.r, sbc.r], [pg.r])
                        TT("dve", grow[w].a[:, cs], pg.a, bgrow.a[:, cs], ALU.add, [pg.r, bgrow.r], [grow[w].r])
                        TT("pool", grow[w].a[:, cs], grow[w].a[:, cs], pgrow.a[:, cs], ALU.mult, [grow[w].r, pgrow.r], [grow[w].r])
            TT("dve", modc.a, pmv, bc(bmc.a[:, 0:16], [128, 16, 2], 2), ALU.add, [pmod.r, bmc.r], [modc.r])
            STT("dve", acol.a, modc.a[:, 8:16, :], 1.0, bc(colp.a[:, 0:8], [128, 8, 2], 2), ALU.add, ALU.mult, [modc.r, colp.r], [acol.r])
            arena.reset(m0)
            P.barrier()
            m0 = arena.mark()
            if "B" not in phases:
                og = P.group(dedicated=True)
                DMA("sp", out_d[0:128, :], grow[0].a, [grow[0].r], [])
                continue
            xb = [arena.alloc([128, D], F32, "xb%d" % i) for i in range(2)]
            junk = arena.alloc([128, D], BF16, "junk")
            xn = [arena.alloc([128, D], BF16, "xn%d" % i) for i in range(2)]
            tmpf = arena.alloc([128, 8, 128], F32, "tmpf")
            for c in range(NCH):
                xt = xb[c % 2]
                if from_x:
                    src = x_d[c * 128:(c + 1) * 128, :] if c < 16 else ctx_d[(c - 16) * 128:(c - 15) * 128, :]
                else:
                    src = xs_d[c * 128:(c + 1) * 128, :]
                DMA("sp", xt.a, src, [], [xt.r])
                ACT(junk.a, xt.a, AF.Square, [xt.r], [junk.r, ss.r], accum=ss.a[:, c:c + 1])
                ACT(rstd.a[:, c:c + 1], ss.a[:, c:c + 1], AF.Sqrt, [ss.r, cst.r], [rstd.r], bias=cst.a[:, 0:1], scale=1.0 / D)
                P.op("dve", lambda e, o=rstd.a[:, c:c + 1]: e.reciprocal(out=o, in_=o), [rstd.r], [rstd.r])
                xnt = xn[c % 2]
                TS("dve", xnt.a, xt.a, rstd.a[:, c:c + 1], ALU.mult, [xt.r, rstd.r], [xnt.r])
                pt = nb()
                ptb = pt.a.bitcast(BF16).rearrange("p (j t) -> p j t", j=8)
                for j in range(8):
                    TR(ptb[:, j, :], xnt.a[:, j * 128:(j + 1) * 128], ident_b.a, [xnt.r, ident_b.r], [pt.r])
                w = 0 if c < 16 else 1
                TT("dve", tmpf.a, ptb, bc(acol.a[:, :, w], [128, 8, 128], 2), ALU.mult, [pt.r, acol.r], [tmpf.r])
                TT("pool", hT.a[:, :, c * 128:(c + 1) * 128], tmpf.a, bc(modc.a[:, 0:8, w], [128, 8, 128], 2), ALU.add, [tmpf.r, modc.r], [hT.r])
            arena.reset(m0)
            P.barrier()
            if debug and l == n_layers - 1:
                DMA("sp", dbg_h.rearrange("k p t -> p k t"), hT.a, [hT.r], [])

            m_mix = arena.mark()
            if mixers != "ABCD":
                zt = arena.alloc([128, NT], BF16, "zt")
                MSET("pool", zt.a, 0.0, [zt.r])
                for i in range(8):
                    DMA("sp", ycat_d[i], zt.a, [zt.r], [ycat_res])
                arena.reset(m_mix)
                P.barrier()
            wbuf = arena.alloc([128, 8, 1280], BF16, "wbuf")
            wstage = [arena.alloc([128, 1280], F32, "wst%d" % i) for i in range(2)]
            m_mix2 = arena.mark()

            def load_w(coloff, ncols, dst):
                for k in range(8):
                    load_cast(dst.a[:, k, 0:ncols], dst.r, win_d[l, k * 128:(k + 1) * 128, coloff:coloff + ncols], ncols, wstage[k % 2], wres[l]["in"])

            def inproj_fm(wt, col0, t0, n, M=128):
                b = nb()
                for k in range(8):
                    MM(b.a[0:M, 0:n], wt.a[:, k, col0:col0 + M], hT.a[:, k, t0:t0 + n], k == 0, k == 7, [wt.r, hT.r], [b.r])
                return b

            def inproj_tm(wt, col0, ncols, c):
                b = nb()
                for k in range(8):
                    MM(b.a[:, 0:ncols], hT.a[:, k, c * 128:(c + 1) * 128], wt.a[:, k, col0:col0 + ncols], k == 0, k == 7, [wt.r, hT.r], [b.r])
                return b

            def store_ycat(yt, tile_idx):
                DMA("sp", ycat_d[tile_idx], yt.a, [yt.r], [])

            if "A" in mixers:
                load_w(WA, 512, wbuf)
                dg = arena.alloc([128, 8, 128], BF16, "dg")
                for i in range(8):
                    TS("pool", dg.a[:, i, :], ident_f.a, colp.a[:, 8 + i:9 + i], ALU.mult, [ident_f.r, colp.r], [dg.r])
                wgb = arena.alloc([128, 8, 128], BF16, "wgb")
                wgs = arena.alloc([128, 8, 128], F32, "wgs")
                MSET("pool", wgs.a, 0.0, [wgs.r])
                for d_ in range(2):
                    for gi_ in range(2):
                        for blk_ in range(4):
                            t_, o_ = blk_ // 2, (blk_ % 2) * 64
                            DMA("sp", wgs.a[o_:o_ + 64, d_ * 4 + gi_ * 2 + t_, o_:o_ + 64], lruw_d[l, gi_, d_, blk_], [], [wgs.r])
                CP("pool", wgb.a, wgs.a, [wgs.r], [wgb.r])
                kcol = arena.alloc([128, 4], F32, "kcol")
                ACT(kcol.a, colp.a[:, 26:30], AF.Exp, [colp.r], [kcol.r], scale=-1.0)
                ACT(kcol.a, kcol.a, AF.Ln, [kcol.r, cst.r], [kcol.r], bias=cst.a[:, 1:2])
                TS("dve", kcol.a, kcol.a, -8.0, ALU.mult, [kcol.r], [kcol.r])
                xpad = arena.alloc([128, 2, 2310], BF16, "xpad")
                MSET("pool", xpad.a, 0.0, [xpad.r])
                ub = arena.alloc([128, 2, NT], BF16, "ub")
                for t in range(2):
                    for (t0, n) in BLKS:
                        b = inproj_fm(wbuf, t * 128, t0, n)
                        o0 = t0 + 2 if t0 < T else 2053
                        CP("act", xpad.a[:, t, o0:o0 + n], b.a[:, 0:n], [b.r], [xpad.r])
                for t in range(2):
                    for (t0, n) in BLKS:
                        j0 = t0 if t0 < T else 2051
                        b = nb()
                        for k in range(4):
                            MM(b.a[:, 0:n], dg.a[:, k * 2 + t, :], xpad.a[:, t, j0 + k:j0 + k + n], k == 0, k == 3, [dg.r, xpad.r], [b.r])
                        ACT(ub.a[:, t, t0:t0 + n], b.a[:, 0:n], AF.Identity, [b.r, colp.r], [ub.r], bias=colp.a[:, 16 + t:17 + t])
                a_buf = arena.alloc([128, NT], F32, "a_buf")
                b_buf = arena.alloc([128, NT], F32, "b_buf")
                hbuf = [arena.alloc([128, NT], F32, "h%d" % i) for i in range(2)]
                rr = [arena.alloc([128, 512], F32, "rr%d" % i) for i in range(2)]
                ii = [arena.alloc([128, 512], F32, "ii%d" % i) for i in range(2)]
                tq = [arena.alloc([128, 512], F32, "tq%d" % i) for i in range(2)]
                yt = arena.alloc([128, NT], BF16, "yt")
                for t in range(2):
                    for d in range(2):
                        for bi, (t0, n) in enumerate(BLKS):
                            r_, i_, q_ = rr[bi % 2], ii[bi % 2], tq[bi % 2]
                            b1 = nb()
                            MM(b1.a[:, 0:n], wgb.a[:, d * 4 + 0 + t, :], ub.a[:, t, t0:t0 + n], True, True, [wgb.r, ub.r], [b1.r])
                            ACT(r_.a[:, 0:n], b1.a[:, 0:n], AF.Sigmoid, [b1.r, colp.r], [r_.r], bias=colp.a[:, 18 + d * 2 + t:19 + d * 2 + t])
                            b2 = nb()
                            MM(b2.a[:, 0:n], wgb.a[:, d * 4 + 2 + t, :], ub.a[:, t, t0:t0 + n], True, True, [wgb.r, ub.r], [b2.r])
                            ACT(i_.a[:, 0:n], b2.a[:, 0:n], AF.Sigmoid, [b2.r, colp.r], [i_.r], bias=colp.a[:, 22 + d * 2 + t:23 + d * 2 + t])
                            ACT(a_buf.a[:, t0:t0 + n], r_.a[:, 0:n], AF.Exp, [r_.r, kcol.r], [a_buf.r], scale=kcol.a[:, d * 2 + t:d * 2 + t + 1])
                            TT("dve", q_.a[:, 0:n], a_buf.a[:, t0:t0 + n], a_buf.a[:, t0:t0 + n], ALU.mult, [a_buf.r], [q_.r])
                            ACT(q_.a[:, 0:n], q_.a[:, 0:n], AF.Sqrt, [q_.r, cst.r], [q_.r], bias=cst.a[:, 1:2], scale=-1.0)
                            TT("dve", q_.a[:, 0:n], q_.a[:, 0:n], i_.a[:, 0:n], ALU.mult, [q_.r, i_.r], [q_.r])
                            TT("pool", b_buf.a[:, t0:t0 + n], q_.a[:, 0:n], ub.a[:, t, t0:t0 + n], ALU.mult, [q_.r, ub.r], [b_buf.r])
                        h = hbuf[d]
                        if d == 0:
                            SCAN(h.a[:, T:NT], a_buf.a[:, T:NT], b_buf.a[:, T:NT], 0.0, [a_buf.r, b_buf.r], [h.r])
                            SCAN(h.a[:, 0:T], a_buf.a[:, 0:T], b_buf.a[:, 0:T], h.a[:, NT - 1:NT], [a_buf.r, b_buf.r, h.r], [h.r])
                        else:
                            SCAN(rev(h.a[:, T:NT]), rev(a_buf.a[:, T:NT]), rev(b_buf.a[:, T:NT]), 0.0, [a_buf.r, b_buf.r], [h.r])
                            SCAN(rev(h.a[:, 0:T]), rev(a_buf.a[:, 0:T]), rev(b_buf.a[:, 0:T]), h.a[:, T:T + 1], [a_buf.r, b_buf.r, h.r], [h.r])
                    for bi, (t0, n) in enumerate(BLKS):
                        b = inproj_fm(wbuf, 256 + t * 128, t0, n)
                        z_ = rr[bi % 2]
                        ACT(z_.a[:, 0:n], b.a[:, 0:n], AF.Silu, [b.r], [z_.r])
                        q_ = tq[bi % 2]
                        TT("dve", q_.a[:, 0:n], hbuf[0].a[:, t0:t0 + n], hbuf[1].a[:, t0:t0 + n], ALU.add, [hbuf[0].r, hbuf[1].r], [q_.r])
                        TT("pool", yt.a[:, t0:t0 + n], q_.a[:, 0:n], z_.a[:, 0:n], ALU.mult, [q_.r, z_.r], [yt.r])
                    store_ycat(yt, 0 + t)
                arena.reset(m_mix2)
                P.barrier()

            if "B" in mixers:
                load_w(WB, 784, wbuf)
                wz = arena.alloc([128, 8, 256], BF16, "wz")
                load_w(WBZ, 256, wz)
                qT = arena.alloc([128, 2, NT], BF16, "qT")
                kT = arena.alloc([128, 2, NT], BF16, "kT")
                k_tok = arena.alloc([128, NCH, 256], BF16, "k_tok")
                v_tok = arena.alloc([128, NCH, 256], BF16, "v_tok")
                g_tok = arena.alloc([128, NCH, 8], F32, "g_tok")
                nbeta = arena.alloc([128, NCH, 8], F32, "nbeta")
                beta = arena.alloc([128, NCH, 8], F32, "beta")
                nega = arena.alloc([128, 8], F32, "nega")
                mB = arena.mark()
                vT = arena.alloc([128, 2, NT], BF16, "vT")
                dgB = arena.alloc([128, 24, 128], BF16, "dgB")
                for i in range(24):
                    TS("pool", dgB.a[:, i, :], ident_f.a, colp.a[:, 30 + i:31 + i], ALU.mult, [ident_f.r, colp.r], [dgB.r])
                xpads = [arena.alloc([128, 2310], BF16, "xpad%d" % i) for i in range(2)]
                for xp in xpads:
                    MSET("pool", xp.a, 0.0, [xp.r])
                sl = [arena.alloc([128, 512], F32, "sl%d" % i) for i in range(2)]
                sqb = [arena.alloc([128, 512], BF16, "sqb%d" % i) for i in range(2)]
                rn = [arena.alloc([128, 512], F32, "rn%d" % i) for i in range(2)]
                cnt = 0
                for ti in range(6):
                    xp = xpads[ti % 2]
                    for (t0, n) in BLKS:
                        b = inproj_fm(wbuf, ti * 128, t0, n)
                        o0 = t0 + 2 if t0 < T else 2053
                        CP("act", xp.a[:, o0:o0 + n], b.a[:, 0:n], [b.r], [xp.r])
                    for (t0, n) in BLKS:
                        j0 = t0 if t0 < T else 2051
                        b = nb()
                        for k in range(4):
                            MM(b.a[:, 0:n], dgB.a[:, k * 6 + ti, :], xp.a[:, j0 + k:j0 + k + n], k == 0, k == 3, [dgB.r, xp.r], [b.r])
                        if ti >= 4:
                            ACT(vT.a[:, ti - 4, t0:t0 + n], b.a[:, 0:n], AF.Silu, [b.r], [vT.r])
                            continue
                        s_, q_, r_ = sl[cnt % 2], sqb[cnt % 2], rn[cnt % 2]
                        cnt += 1
                        ACT(s_.a[:, 0:n], b.a[:, 0:n], AF.Silu, [b.r], [s_.r])
                        TT("pool", q_.a[:, 0:n], s_.a[:, 0:n], s_.a[:, 0:n], ALU.mult, [s_.r], [q_.r])
                        b2 = nb()
                        MM(b2.a[:, 0:n], blockones_b.a, q_.a[:, 0:n], True, True, [blockones_b.r, q_.r], [b2.r])
                        ACT(r_.a[:, 0:n], b2.a[:, 0:n], AF.Sqrt, [b2.r, cst.r], [r_.r], bias=cst.a[:, 0:1])
                        P.op("dve", lambda e, o=r_.a[:, 0:n]: e.reciprocal(out=o, in_=o), [r_.r], [r_.r])
                        dst = qT if ti < 2 else kT
                        STT("dve", dst.a[:, ti % 2, t0:t0 + n], s_.a[:, 0:n], 0.125 if ti < 2 else 1.0, r_.a[:, 0:n], ALU.mult, ALU.mult, [s_.r, r_.r], [dst.r])
                ab = arena.alloc([128, NCH, 16], F32, "ab")
                for c in range(NCH):
                    for src, dstt in ((kT, k_tok), (vT, v_tok)):
                        pt = nb()
                        ptb = pt.a.bitcast(BF16)[:, 0:256]
                        for t in range(2):
                            TR(ptb[:, t * 128:(t + 1) * 128], src.a[:, t, c * 128:(c + 1) * 128], ident_b.a, [src.r, ident_b.r], [pt.r])
                        CP("act" if dstt is k_tok else "dve", dstt.a[:, c, :], ptb, [pt.r], [dstt.r])
                    b = inproj_tm(wbuf, 768, 16, c)
                    CP("dve", ab.a[:, c, :], b.a[:, 0:16], [b.r], [ab.r])
                ACT(nega.a, rowt.a[:, 0:8], AF.Exp, [rowt.r], [nega.r])
                TS("dve", nega.a, nega.a, -1.0, ALU.mult, [nega.r], [nega.r])
                TT("dve", g_tok.a, ab.a[:, :, 0:8], bc(rowt.a[:, 8:16], [128, NCH, 8], 1), ALU.add, [ab.r, rowt.r], [g_tok.r])
                ACT(g_tok.a, g_tok.a, AF.Exp, [g_tok.r], [g_tok.r])
                ACT(g_tok.a, g_tok.a, AF.Ln, [g_tok.r, cst.r], [g_tok.r], bias=cst.a[:, 1:2])
                TT("dve", g_tok.a, g_tok.a, bc(nega.a, [128, NCH, 8], 1), ALU.mult, [g_tok.r, nega.r], [g_tok.r])
                ACT(beta.a, ab.a[:, :, 8:16], AF.Sigmoid, [ab.r], [beta.r])
                TS("dve", nbeta.a, beta.a, -1.0, ALU.mult, [beta.r], [nbeta.r])
                P.barrier()
                arena.reset(mB)
                o_acc = arena.alloc([128, NCH, 256], F32, "o_acc")
                class WS:
                    pass
                wss = []
                for d in range(2):
                    w_ = WS()
                    stg = wstage[d]
                    w_.gTri = Tl(stg.a[:, 0:512].rearrange("p (h i) -> p h i", h=4), stg.r)
                    w_.DT = Tl(stg.a[:, 512:1024].rearrange("p (h i) -> p h i", h=4), stg.r)
                    w_.t1 = arena.alloc([128, 4, 128], BF16, "t1_%d" % d)
                    w_.NB = arena.alloc([128, 4, 128], BF16, "NB%d" % d)
                    w_.M = [arena.alloc([128, 4, 128], BF16, "M%d_%d" % (d, i)) for i in range(2)]
                    w_.N = [arena.alloc([128, 4, 128], BF16, "N%d_%d" % (d, i)) for i in range(2)]
                    w_.PT = [arena.alloc([128, 4, 128], BF16, "PT%d_%d" % (d, i)) for i in range(2)]
                    w_.Mb = w_.M[1]
                    w_.Nb = w_.N[1]
                    w_.U = w_.PT[0]
                    w_.Tm = w_.PT[1]
                    w_.Y1 = arena.alloc([128, 4, 128], BF16, "Y1_%d" % d)
                    w_.Y2 = arena.alloc([128, 4, 128], BF16, "Y2_%d" % d)
                    w_.att = arena.alloc([128, 4, 128], BF16, "att%d" % d)
                    w_.kz = arena.alloc([128, 4, 128], BF16, "kz%d" % d)
                    MSET("pool", w_.kz.a, 0.0, [w_.kz.r])
                    w_.kg = arena.alloc([128, 4, 64], BF16, "kg%d" % d)
                    w_.ktl = arena.alloc([128, 4, 64], BF16, "ktl%d" % d)
                    w_.negWt = arena.alloc([128, 2, 128], BF16, "negWt%d" % d)
                    w_.vnew = arena.alloc([128, 4, 64], BF16, "vnew%d" % d)
                    w_.to = arena.alloc([128, 256], F32, "to%d" % d)
                    w_.sm = arena.alloc([128, 32], F32, "sm%d" % d)
                    w_.S32 = arena.alloc([128, 2, 128], F32, "S32_%d" % d)
                    w_.Sbd = arena.alloc([128, 2, 128], BF16, "Sbd%d" % d)
                    MSET("pool", w_.S32.a, 0.0, [w_.S32.r])
                    MSET("pool", w_.Sbd.a, 0.0, [w_.Sbd.r])
                    wss.append(w_)

                orderF = [16, 17] + list(range(16))
                orderB = [17, 16] + list(range(15, -1, -1))

                def gdn_pre(c, d):
                    w_ = wss[d]
                    cs = slice(c * 128, (c + 1) * 128)
                    tri = maskF if d == 0 else maskB
                    g4 = g_tok.a[:, c, d * 4:(d + 1) * 4]
                    nb4 = nbeta.a[:, c, d * 4:(d + 1) * 4]
                    sm = w_.sm
                    gcb = nb()
                    MM(gcb.a[:, 0:4], tri.a, g4, True, True, [tri.r, g_tok.r], [gcb.r])
                    MM(gcb.a[:, 4:8], ones_f.a, g4, True, True, [ones_f.r, g_tok.r], [gcb.r])
                    CP("dve", sm.a[:, 0:8], gcb.a[:, 0:8], [gcb.r], [sm.r])
                    TS("dve", sm.a[:, 8:12], sm.a[:, 0:4], -1.0, ALU.mult, [sm.r], [sm.r])
                    TT("dve", sm.a[:, 16:20], sm.a[:, 4:8], sm.a[:, 0:4], ALU.subtract, [sm.r], [sm.r])
                    ACT(sm.a[:, 12:16], sm.a[:, 0:4], AF.Exp, [sm.r], [sm.r])
                    ACT(sm.a[:, 16:20], sm.a[:, 16:20], AF.Exp, [sm.r], [sm.r])
                    ACT(sm.a[:, 20:24], sm.a[:, 4:8], AF.Exp, [sm.r], [sm.r])
                    for t in range(2):
                        CP("dve", sm.a[0:64, 24 + t:25 + t], sm.a[0:64, 20 + 2 * t:21 + 2 * t], [sm.r], [sm.r])
                        CP("dve", sm.a[64:128, 24 + t:25 + t], sm.a[64:128, 21 + 2 * t:22 + 2 * t], [sm.r], [sm.r])
                    TT("pool", w_.gTri.a, bc(tri.a, [128, 4, 128], 1), bc(g4, [128, 4, 128], 2), ALU.mult, [tri.r, g_tok.r], [w_.gTri.r])
                    GB = nb()
                    MM(GB.a, ones_f.a, w_.gTri.a.rearrange("p h i -> p (h i)"), True, False, [ones_f.r, w_.gTri.r], [GB.r])
                    MM(GB.a, ident_f.a, maskneg.a[:, d].rearrange("p h i -> p (h i)"), False, True, [ident_f.r, maskneg.r], [GB.r])
                    for h in range(4):
                        ACT(w_.DT.a[:, h, :], GB.a[:, h * 128:(h + 1) * 128], AF.Exp, [GB.r, sm.r], [w_.DT.r], bias=sm.a[:, 8 + h:9 + h])
                    for t in range(2):
                        CP("pool", w_.kz.a[0:64, 2 * t, :], kT.a[0:64, t, cs], [kT.r], [w_.kz.r])
                        CP("pool", w_.kz.a[64:128, 2 * t + 1, :], kT.a[64:128, t, cs], [kT.r], [w_.kz.r])
                    KK = nb()
                    QK = nb()
                    for h in range(4):
                        MM(KK.a[:, h * 128:(h + 1) * 128], w_.kz.a[:, h, :], kT.a[:, h // 2, cs], True, True, [w_.kz.r, kT.r], [KK.r])
                    for h in range(4):
                        MM(QK.a[:, h * 128:(h + 1) * 128], w_.kz.a[:, h, :], qT.a[:, h // 2, cs], True, True, [w_.kz.r, qT.r], [QK.r])
                    TT("dve", w_.t1.a, KK.a.rearrange("p (h i) -> p h i", h=4), w_.DT.a, ALU.mult, [KK.r, w_.DT.r], [w_.t1.r])
                    TT("dve", w_.att.a, QK.a.rearrange("p (h i) -> p h i", h=4), w_.DT.a, ALU.mult, [QK.r, w_.DT.r], [w_.att.r])
                    TT("pool", w_.NB.a, bc(offdiag.a, [128, 4, 128], 1), bc(nb4, [128, 4, 128], 2), ALU.mult, [offdiag.r, nbeta.r], [w_.NB.r])
                    M, N, PT = w_.M, w_.N, w_.PT
                    TT("pool", M[0].a, w_.t1.a, w_.NB.a, ALU.mult, [w_.t1.r, w_.NB.r], [M[0].r])
                    pt = nb()
                    ptb = pt.a.bitcast(BF16)[:, 0:512]
                    for h in range(4):
                        TR(ptb[:, h * 128:(h + 1) * 128], M[0].a[:, h, :], ident_b.a, [M[0].r, ident_b.r], [pt.r])
                    CP("act", N[0].a.rearrange("p h i -> p (h i)"), ptb, [pt.r], [N[0].r])
                    M0_, N0_ = M[0], N[0]
                    Mb, Nb, U, Tm, Y1, Y2 = w_.Mb, w_.Nb, w_.U, w_.Tm, w_.Y1, w_.Y2
                    f4 = lambda tl: tl.a.rearrange("p h i -> p (h i)")
                    i4 = bc(ident_b.a, [128, 4, 128], 1)

                    def mm4(lhs, rhs):
                        b_ = nb()
                        for h in range(4):
                            MM(b_.a[:, h * 128:(h + 1) * 128], lhs.a[:, h, :], rhs.a[:, h, :], True, True, [lhs.r, rhs.r], [b_.r])
                        return b_

                    m8 = bc(bdm[0].a, [128, 4, 128], 1)
                    TT("pool", Mb.a, M0_.a, m8, ALU.mult, [M0_.r, bdm[0].r], [Mb.r])
                    TT("pool", Nb.a, N0_.a, m8, ALU.mult, [N0_.r, bdm[0].r], [Nb.r])
                    TT("pool", U.a, Mb.a, i4, ALU.add, [Mb.r, ident_b.r], [U.r])
                    TT("pool", Tm.a, Nb.a, i4, ALU.add, [Nb.r, ident_b.r], [Tm.r])
                    for lev in range(2):
                        bM = mm4(Nb, Mb)
                        bN = mm4(Mb, Nb)
                        CP("act", f4(Y1), bM.a, [bM.r], [Y1.r])
                        CP("dve", f4(Y2), bN.a, [bN.r], [Y2.r])
                        bU = mm4(Y2, U)
                        bT = mm4(Y1, Tm)
                        TT("dve", f4(U), f4(U), bU.a, ALU.add, [U.r, bU.r], [U.r])
                        TT("dve", f4(Tm), f4(Tm), bT.a, ALU.add, [Tm.r, bT.r], [Tm.r])
                        if lev == 0:
                            CP("pool", Mb.a, Y1.a, [Y1.r], [Mb.r])
                            CP("pool", Nb.a, Y2.a, [Y2.r], [Nb.r])
                    for li in range(4):
                        mo = bc(offm[li].a, [128, 4, 128], 1)
                        TT("pool", Mb.a, M0_.a, mo, ALU.mult, [M0_.r, offm[li].r], [Mb.r])
                        TT("pool", Nb.a, N0_.a, mo, ALU.mult, [N0_.r, offm[li].r], [Nb.r])
                        bY = mm4(Nb, U)
                        CP("act", f4(Y1), bY.a, [bY.r], [Y1.r])
                        if li < 3:
                            bY2 = mm4(Mb, Tm)
                            CP("dve", f4(Y2), bY2.a, [bY2.r], [Y2.r])
                        bZ = mm4(Tm, Y1)
                        if li < 3:
                            bZ2 = mm4(U, Y2)
                        TT("dve", f4(U), f4(U), bZ.a, ALU.add, [U.r, bZ.r], [U.r])
                        if li < 3:
                            TT("dve", f4(Tm), f4(Tm), bZ2.a, ALU.add, [Tm.r, bZ2.r], [Tm.r])
                    PT = [U]
                    cur = 0
                    w_.ptf = PT[cur]
                    k4 = k_tok.a[:, c, :].rearrange("p (h e) -> p h e", h=4)
                    TT("pool", w_.kg.a, k4, bc(sm.a[:, 12:16], [128, 4, 64], 2), ALU.mult, [k_tok.r, sm.r], [w_.kg.r])
                    TT("pool", w_.ktl.a, k4, bc(sm.a[:, 16:20], [128, 4, 64], 2), ALU.mult, [k_tok.r, sm.r], [w_.ktl.r])
                    WtP = nb()
                    for h in range(4):
                        t = h // 2
                        MM(WtP.a[:, h * 128:(h + 1) * 128], w_.kg.a[:, 2 * t:2 * t + 2, :].rearrange("p h e -> p (h e)"), w_.ptf.a[:, h, :], True, True, [w_.kg.r, w_.ptf.r], [WtP.r])
                    w4 = WtP.a.rearrange("p (t hh i) -> p t hh i", t=2, hh=2)
                    ACT(w_.negWt.a[0:64, :, :], w4[0:64, :, 0, :], AF.Copy, [WtP.r], [w_.negWt.r], scale=-1.0)
                    TS("dve", w_.negWt.a[64:128, :, :], w4[64:128, :, 1, :], -1.0, ALU.mult, [WtP.r], [w_.negWt.r])

                def gdn_seq(c, d):
                    w_ = wss[d]
                    cs = slice(c * 128, (c + 1) * 128)
                    sm = w_.sm
                    Vn = nb()
                    for t in range(2):
                        MM(Vn.a[:, t * 128:(t + 1) * 128], w_.negWt.a[:, t, :], w_.Sbd.a[:, t, :], True, False, [w_.negWt.r, w_.Sbd.r], [Vn.r])
                        for hh in range(2):
                            h = 2 * t + hh
                            MM(Vn.a[:, h * 64:(h + 1) * 64], w_.ptf.a[:, h, :], v_tok.a[:, c, h * 64:(h + 1) * 64], False, hh == 1, [w_.ptf.r, v_tok.r], [Vn.r])
                    TT("dve", w_.vnew.a, Vn.a[:, 0:256].rearrange("p (h e) -> p h e", h=4), bc(beta.a[:, c, d * 4:(d + 1) * 4], [128, 4, 64], 2), ALU.mult, [Vn.r, beta.r], [w_.vnew.r])
                    Oi = nb()
                    for t in range(2):
                        MM(Oi.a[:, t * 128:(t + 1) * 128], qT.a[:, t, cs], w_.Sbd.a[:, t, :], True, True, [qT.r, w_.Sbd.r], [Oi.r])
                    Oa = nb()
                    for h in range(4):
                        MM(Oa.a[:, h * 64:(h + 1) * 64], w_.att.a[:, h, :], w_.vnew.a[:, h, :], True, True, [w_.att.r, w_.vnew.r], [Oa.r])
                    TT("dve", w_.to.a.rearrange("p (h e) -> p h e", h=4), Oi.a[:, 0:256].rearrange("p (h e) -> p h e", h=4), bc(sm.a[:, 12:16], [128, 4, 64], 2), ALU.mult, [Oi.r, sm.r], [w_.to.r])
                    first = (orderF.index(c) <= orderB.index(c)) == (d == 0) and orderF.index(c) != orderB.index(c)
                    import os
                    if first or len(os.environ.get("GDN_DIRS", "01")) == 1:
                        TT("dve", o_acc.a[:, c, :], w_.to.a, Oa.a[:, 0:256], ALU.add, [w_.to.r, Oa.r], [o_acc.r])
                    else:
                        TT("dve", w_.to.a, w_.to.a, Oa.a[:, 0:256], ALU.add, [w_.to.r, Oa.r], [w_.to.r])
                        TT("pool", o_acc.a[:, c, :], o_acc.a[:, c, :], w_.to.a, ALU.add, [w_.to.r, o_acc.r], [o_acc.r])
                    for t in range(2):
                        sp_ = nb()
                        MM(sp_.a[:, 0:128], w_.ktl.a[:, 2 * t:2 * t + 2, :].rearrange("p h e -> p (h e)"), w_.vnew.a[:, 2 * t:2 * t + 2, :].rearrange("p h e -> p (h e)"), True, True, [w_.ktl.r, w_.vnew.r], [sp_.r])
                        STT("dve", w_.S32.a[:, t, :], w_.S32.a[:, t, :], sm.a[:, 24 + t:25 + t], sp_.a[:, 0:128], ALU.mult, ALU.add, [w_.S32.r, sm.r, sp_.r], [w_.S32.r])
                        TT("pool", w_.Sbd.a[:, t, :], w_.S32.a[:, t, :], blockmask.a, ALU.mult, [w_.S32.r, blockmask.r], [w_.Sbd.r])

                import os
                gdirs = os.environ.get("GDN_DIRS", "01")
                for s_i in range(NCH):
                    if "0" in gdirs:
                        gdn_pre(orderF[s_i], 0)
                    if "1" in gdirs:
                        gdn_pre(orderB[s_i], 1)
                    if "0" in gdirs:
                        gdn_seq(orderF[s_i], 0)
                    if "1" in gdirs:
                        gdn_seq(orderB[s_i], 1)
                ycT = qT
                sqi = [arena.alloc([128, 4, 64], F32, "sqi%d" % i) for i in range(2)]
                dti = [arena.alloc([128, 4, 64], F32, "dti%d" % i) for i in range(2)]
                zsi = [arena.alloc([128, 256], F32, "zsi%d" % i) for i in range(2)]
                yti = [arena.alloc([128, 256], BF16, "yti%d" % i) for i in range(2)]
                stt = [arena.alloc([128, 8], F32, "stt%d" % i) for i in range(2)]
                for c in range(NCH):
                    zb = inproj_tm(wz, 0, 256, c)
                    zs, dt_, sq_, yt_, s_ = zsi[c % 2], dti[c % 2], sqi[c % 2], yti[c % 2], stt[c % 2]
                    ACT(zs.a, zb.a[:, 0:256], AF.Silu, [zb.r], [zs.r])
                    o4 = o_acc.a[:, c, :].rearrange("p (h e) -> p h e", h=4)
                    TT("pool", sq_.a, o4, o4, ALU.mult, [o_acc.r], [sq_.r])
                    P.op("dve", lambda e, o=s_.a[:, 0:4], i=sq_.a: e.tensor_reduce(out=o, in_=i, axis=AX.X, op=ALU.add), [sq_.r], [s_.r])
                    ACT(s_.a[:, 0:4], s_.a[:, 0:4], AF.Sqrt, [s_.r, cst.r], [s_.r], bias=cst.a[:, 0:1], scale=1.0 / 64)
                    P.op("dve", lambda e, o=s_.a[:, 0:4]: e.reciprocal(out=o, in_=o), [s_.r], [s_.r])
                    TT("dve", dt_.a, o4, bc(s_.a[:, 0:4], [128, 4, 64], 2), ALU.mult, [o_acc.r, s_.r], [dt_.r])
                    TT("pool", dt_.a, dt_.a, bc(rowt.a[:, 16:80], [128, 4, 64], 1), ALU.mult, [dt_.r, rowt.r], [dt_.r])
                    TT("pool", yt_.a, dt_.a.rearrange("p h e -> p (h e)"), zs.a, ALU.mult, [dt_.r, zs.r], [yt_.r])
                    pt = nb()
                    ptb = pt.a.bitcast(BF16)[:, 0:256]
                    for t in range(2):
                        TR(ptb[:, t * 128:(t + 1) * 128], yt_.a[:, t * 128:(t + 1) * 128], ident_b.a, [yt_.r, ident_b.r], [pt.r])
                    CP("act", ycT.a[:, :, c * 128:(c + 1) * 128], ptb.rearrange("p (t i) -> p t i", t=2), [pt.r], [ycT.r])
                for t in range(2):
                    DMA("sp", ycat_d[2 + t], ycT.a[:, t, :], [ycT.r], [])
                arena.reset(m_mix2)
                P.barrier()

            if "C" in mixers:
                load_w(WC, 1280, wbuf)
                wz = arena.alloc([128, 8, 256], BF16, "wz")
                load_w(WCZ, 256, wz)
                lg = arena.alloc([128, 8], F32, "lg")
                ACT(lg.a, rowt.a[:, 80:88], AF.Sigmoid, [rowt.r], [lg.r])
                ACT(lg.a, lg.a, AF.Ln, [lg.r], [lg.r])
                nlg = arena.alloc([128, 8], F32, "nlg")
                TS("dve", nlg.a, lg.a, -1.0, ALU.mult, [lg.r], [nlg.r])
                DT = arena.alloc([128, 2, 4, 128], F32, "DT")
                for h in range(4):
                    ACT(DT.a[:, 0, h, :], iota_ij.a, AF.Exp, [iota_ij.r, lg.r], [DT.r], scale=lg.a[:, h:h + 1])
                    ACT(DT.a[:, 1, h, :], iota_ij.a, AF.Exp, [iota_ij.r, nlg.r], [DT.r], scale=nlg.a[:, 4 + h:5 + h])
                TT("pool", DT.a[:, 0], DT.a[:, 0], bc(maskF.a, [128, 4, 128], 1), ALU.mult, [DT.r, maskF.r], [DT.r])
                TT("pool", DT.a[:, 1], DT.a[:, 1], bc(maskB.a, [128, 4, 128], 1), ALU.mult, [DT.r, maskB.r], [DT.r])
                pc = arena.alloc([128, 4], F32, "pc")
                TS("dve", pc.a[:, 0:1], pidx.a, 1.0, ALU.add, [pidx.r], [pc.r])
                TS("dve", pc.a[:, 1:2], pidx.a, -1.0, ALU.mult, [pidx.r], [pc.r], s2=128.0, op1=ALU.add)
                TS("dve", pc.a[:, 2:3], pidx.a, -1.0, ALU.mult, [pidx.r], [pc.r], s2=127.0, op1=ALU.add)
                CP("dve", pc.a[:, 3:4], pidx.a, [pidx.r], [pc.r])
                qdec = arena.alloc([128, 2, 4], F32, "qdec")
                kdec = arena.alloc([128, 2, 4], F32, "kdec")
                ACT(qdec.a[:, 0, :], lg.a[:, 0:4], AF.Exp, [lg.r, pc.r], [qdec.r], scale=pc.a[:, 0:1])
                ACT(qdec.a[:, 1, :], lg.a[:, 4:8], AF.Exp, [lg.r, pc.r], [qdec.r], scale=pc.a[:, 1:2])
                ACT(kdec.a[:, 0, :], lg.a[:, 0:4], AF.Exp, [lg.r, pc.r], [kdec.r], scale=pc.a[:, 2:3])
                ACT(kdec.a[:, 1, :], lg.a[:, 4:8], AF.Exp, [lg.r, pc.r], [kdec.r], scale=pc.a[:, 3:4])
                lgc = arena.alloc([128, 8], F32, "lgc")
                ACT(lgc.a, lg.a, AF.Exp, [lg.r], [lgc.r], scale=128.0)
                cdcol = arena.alloc([128, 4], F32, "cdcol")
                for d in range(2):
                    for t in range(2):
                        CP("dve", cdcol.a[0:64, d * 2 + t:d * 2 + t + 1], lgc.a[0:64, d * 4 + 2 * t:d * 4 + 2 * t + 1], [lgc.r], [cdcol.r])
                        CP("dve", cdcol.a[64:128, d * 2 + t:d * 2 + t + 1], lgc.a[64:128, d * 4 + 2 * t + 1:d * 4 + 2 * t + 2], [lgc.r], [cdcol.r])
                qT = arena.alloc([128, 2, NT], BF16, "qT")
                kT = arena.alloc([128, 2, NT], BF16, "kT")
                rt = [arena.alloc([128, 512], F32, "rt%d" % i) for i in range(4)]
                cnt = 0
                for which, dst, sc in ((0, qT, 1.0), (1, kT, 0.125)):
                    for t in range(2):
                        for (t0, n) in BLKS:
                            b1 = inproj_fm(wbuf, which * 512 + t * 128, t0, n)
                            if t0 < T:
                                b2 = inproj_fm(wbuf, which * 512 + 256 + t * 128, t0, n)
                                r1, r2 = rt[(cnt * 2) % 4], rt[(cnt * 2 + 1) % 4]
                                cnt += 1
                                STT("dve", r1.a[:, 0:n], b1.a[:, 0:n], sc, rope.a[:, 0, t0:t0 + n], ALU.mult, ALU.mult, [b1.r, rope.r], [r1.r])
                                STT("dve", r2.a[:, 0:n], b2.a[:, 0:n], sc, rope.a[:, 1, t0:t0 + n], ALU.mult, ALU.mult, [b2.r, rope.r], [r2.r])
                                TT("pool", dst.a[:, t, t0:t0 + n], r1.a[:, 0:n], r2.a[:, 0:n], ALU.add, [r1.r, r2.r], [dst.r])
                            else:
                                ACT(dst.a[:, t, t0:t0 + n], b1.a[:, 0:n], AF.Copy, [b1.r], [dst.r], scale=sc)
                v_tok = arena.alloc([128, NCH, 256], BF16, "v_tok")
                kdt = [arena.alloc([128, NCH, 4, 64], BF16, "kdt%d" % i) for i in range(2)]
                for c in range(NCH):
                    b = inproj_tm(wbuf, 1024, 256, c)
                    CP("act" if c % 2 else "dve", v_tok.a[:, c, :], b.a[:, 0:256], [b.r], [v_tok.r])
                    pt = nb()
                    ptb = pt.a.bitcast(BF16)[:, 0:256]
                    for t in range(2):
                        TR(ptb[:, t * 128:(t + 1) * 128], kT.a[:, t, c * 128:(c + 1) * 128], ident_b.a, [kT.r, ident_b.r], [pt.r])
                    for d in range(2):
                        TT("dve", kdt[d].a[:, c], ptb.rearrange("p (h e) -> p h e", h=4), bc(kdec.a[:, d, :], [128, 4, 64], 2), ALU.mult, [pt.r, kdec.r], [kdt[d].r])
                o_acc = arena.alloc([128, NCH, 256], F32, "o_acc")
                kz = [arena.alloc([128, 4, 128], BF16, "kz%d" % i) for i in range(2)]
                att = [arena.alloc([128, 4, 128], BF16, "att%d" % i) for i in range(2)]
                S32 = arena.alloc([128, 2, 128], F32, "S32")
                Sbd = arena.alloc([128, 2, 128], BF16, "Sbd")
                tmpo = [arena.alloc([128, 256], F32, "tmpo%d" % i) for i in range(2)]
                for z_ in kz:
                    MSET("pool", z_.a, 0.0, [z_.r])
                step = 0
                for d in range(2):
                    order = [16, 17] + list(range(16)) if d == 0 else [17, 16] + list(range(15, -1, -1))
                    MSET("pool", S32.a, 0.0, [S32.r])
                    MSET("pool", Sbd.a, 0.0, [Sbd.r])
                    for c in order:
                        cs = slice(c * 128, (c + 1) * 128)
                        kz_, at_, to_ = kz[step % 2], att[step % 2], tmpo[step % 2]
                        step += 1
                        for t in range(2):
                            CP("pool", kz_.a[0:64, 2 * t, :], kT.a[0:64, t, cs], [kT.r], [kz_.r])
                            CP("pool", kz_.a[64:128, 2 * t + 1, :], kT.a[64:128, t, cs], [kT.r], [kz_.r])
                        sb_ = nb()
                        for h in range(4):
                            MM(sb_.a[:, h * 128:(h + 1) * 128], kz_.a[:, h, :], qT.a[:, h // 2, cs], True, True, [kz_.r, qT.r], [sb_.r])
                        TT("dve", at_.a, sb_.a.rearrange("p (h i) -> p h i", h=4), DT.a[:, d], ALU.mult, [sb_.r, DT.r], [at_.r])
                        oi = nb()
                        for t in range(2):
                            MM(oi.a[:, t * 128:(t + 1) * 128], qT.a[:, t, cs], Sbd.a[:, t, :], True, True, [qT.r, Sbd.r], [oi.r])
                        oa = nb()
                        for h in range(4):
                            MM(oa.a[:, h * 64:(h + 1) * 64], at_.a[:, h, :], v_tok.a[:, c, h * 64:(h + 1) * 64], True, True, [at_.r, v_tok.r], [oa.r])
                        TT("dve", to_.a.rearrange("p (h e) -> p h e", h=4), oi.a[:, 0:256].rearrange("p (h e) -> p h e", h=4), bc(qdec.a[:, d, :], [128, 4, 64], 2), ALU.mult, [oi.r, qdec.r], [to_.r])
                        if d == 0:
                            TT("dve", o_acc.a[:, c, :], to_.a, oa.a[:, 0:256], ALU.add, [to_.r, oa.r], [o_acc.r])
                        else:
                            TT("dve", to_.a, to_.a, oa.a[:, 0:256], ALU.add, [to_.r, oa.r], [to_.r])
                            TT("pool", o_acc.a[:, c, :], o_acc.a[:, c, :], to_.a, ALU.add, [to_.r, o_acc.r], [o_acc.r])
                        for t in range(2):
                            sp_ = nb()
                            MM(sp_.a[:, 0:128], kdt[d].a[:, c, 2 * t:2 * t + 2, :].rearrange("p h e -> p (h e)"), v_tok.a[:, c, t * 128:(t + 1) * 128], True, True, [kdt[d].r, v_tok.r], [sp_.r])
                            STT("dve", S32.a[:, t, :], S32.a[:, t, :], cdcol.a[:, d * 2 + t:d * 2 + t + 1], sp_.a[:, 0:128], ALU.mult, ALU.add, [S32.r, cdcol.r, sp_.r], [S32.r])
                            TT("pool", Sbd.a[:, t, :], S32.a[:, t, :], blockmask.a, ALU.mult, [S32.r, blockmask.r], [Sbd.r])
                ycT = qT
                st4 = arena.alloc([128, 8], F32, "st4")
                dti = [arena.alloc([128, 4, 64], F32, "dti%d" % i) for i in range(2)]
                sqi = [arena.alloc([128, 4, 64], F32, "sqi%d" % i) for i in range(2)]
                zsi = [arena.alloc([128, 256], F32, "zsi%d" % i) for i in range(2)]
                yti = [arena.alloc([128, 256], BF16, "yti%d" % i) for i in range(2)]
                stt = [arena.alloc([128, 8], F32, "stt%d" % i) for i in range(2)]
                for c in range(NCH):
                    zb = inproj_tm(wz, 0, 256, c)
                    zs, dt_, sq_, yt_, s_ = zsi[c % 2], dti[c % 2], sqi[c % 2], yti[c % 2], stt[c % 2]
                    ACT(zs.a, zb.a[:, 0:256], AF.Silu, [zb.r], [zs.r])
                    o4 = o_acc.a[:, c, :].rearrange("p (h e) -> p h e", h=4)
                    P.op("dve", lambda e, o=s_.a[:, 0:4], i=o4: e.tensor_reduce(out=o, in_=i, axis=AX.X, op=ALU.add), [o_acc.r], [s_.r])
                    TS("dve", s_.a[:, 0:4], s_.a[:, 0:4], -1.0 / 64, ALU.mult, [s_.r], [s_.r])
                    TT("dve", dt_.a, o4, bc(s_.a[:, 0:4], [128, 4, 64], 2), ALU.add, [o_acc.r, s_.r], [dt_.r])
                    TT("pool", sq_.a, dt_.a, dt_.a, ALU.mult, [dt_.r], [sq_.r])
                    P.op("dve", lambda e, o=s_.a[:, 4:8], i=sq_.a: e.tensor_reduce(out=o, in_=i, axis=AX.X, op=ALU.add), [sq_.r], [s_.r])
                    ACT(s_.a[:, 4:8], s_.a[:, 4:8], AF.Sqrt, [s_.r, cst.r], [s_.r], bias=cst.a[:, 0:1], scale=1.0 / 64)
                    P.op("dve", lambda e, o=s_.a[:, 4:8]: e.reciprocal(out=o, in_=o), [s_.r], [s_.r])
                    TT("dve", dt_.a, dt_.a, bc(s_.a[:, 4:8], [128, 4, 64], 2), ALU.mult, [dt_.r, s_.r], [dt_.r])
                    TT("pool", yt_.a, dt_.a.rearrange("p h e -> p (h e)"), zs.a, ALU.mult, [dt_.r, zs.r], [yt_.r])
                    pt = nb()
                    ptb = pt.a.bitcast(BF16)[:, 0:256]
                    for t in range(2):
                        TR(ptb[:, t * 128:(t + 1) * 128], yt_.a[:, t * 128:(t + 1) * 128], ident_b.a, [yt_.r, ident_b.r], [pt.r])
                    CP("act", ycT.a[:, :, c * 128:(c + 1) * 128], ptb.rearrange("p (t i) -> p t i", t=2), [pt.r], [ycT.r])
                for t in range(2):
                    DMA("sp", ycat_d[4 + t], ycT.a[:, t, :], [ycT.r], [])
                arena.reset(m_mix2)
                P.barrier()

            if "D" in mixers:
                load_w(WD, 1152, wbuf)
                wz = arena.alloc([128, 8, 256], BF16, "wz")
                load_w(WDZ, 256, wz)
                esink = arena.alloc([128, 4], F32, "esink")
                ACT(esink.a, rowt.a[:, 88:92], AF.Exp, [rowt.r], [esink.r])
                qT = arena.alloc([128, 2, NT], BF16, "qT")
                kdT = arena.alloc([128, 2, NT], BF16, "kdT")
                rt = [arena.alloc([128, 512], F32, "rt%d" % i) for i in range(4)]
                cnt = 0
                for which, dst, sc in ((0, qT, 0.125), (1, kdT, 1.0)):
                    for t in range(2):
                        for (t0, n) in BLKS:
                            b1 = inproj_fm(wbuf, which * 512 + t * 128, t0, n)
                            if t0 < T:
                                b2 = inproj_fm(wbuf, which * 512 + 256 + t * 128, t0, n)
                                r1, r2 = rt[(cnt * 2) % 4], rt[(cnt * 2 + 1) % 4]
                                cnt += 1
                                STT("dve", r1.a[:, 0:n], b1.a[:, 0:n], sc, rope.a[:, 2, t0:t0 + n], ALU.mult, ALU.mult, [b1.r, rope.r], [r1.r])
                                STT("dve", r2.a[:, 0:n], b2.a[:, 0:n], sc, rope.a[:, 3, t0:t0 + n], ALU.mult, ALU.mult, [b2.r, rope.r], [r2.r])
                                TT("pool", dst.a[:, t, t0:t0 + n], r1.a[:, 0:n], r2.a[:, 0:n], ALU.add, [r1.r, r2.r], [dst.r])
                            else:
                                ACT(dst.a[:, t, t0:t0 + n], b1.a[:, 0:n], AF.Copy, [b1.r], [dst.r], scale=sc)
                v_aug = arena.alloc([128, NCH, 2, 65], BF16, "v_aug")
                MSET("pool", v_aug.a, 1.0, [v_aug.r])
                for c in range(NCH):
                    b = inproj_tm(wbuf, 1024, 128, c)
                    CP("act" if c % 2 else "dve", v_aug.a[:, c, :, 0:64], b.a[:, 0:128].rearrange("p (h e) -> p h e", h=2), [b.r], [v_aug.r])
                ycT = arena.alloc([128, 2, NT], BF16, "ycT")
                qbd = [arena.alloc([128, 2, 2, 128], BF16, "qbd%d" % i) for i in range(2)]
                for q_ in qbd:
                    MSET("pool", q_.a, 0.0, [q_.r])
                PT = [arena.alloc([128, 4, 128], BF16, "PT%d" % i) for i in range(10)]
                den = [arena.alloc([128, 8], F32, "den%d" % i) for i in range(2)]
                yf = [arena.alloc([128, 4, 64], F32, "yf%d" % i) for i in range(2)]
                zsi = [arena.alloc([128, 256], F32, "zsi%d" % i) for i in range(2)]
                yti = [arena.alloc([128, 256], BF16, "yti%d" % i) for i in range(2)]
                for qi, n in enumerate(list(range(NCH))):
                    cs = slice(n * 128, (n + 1) * 128)
                    qb_ = qbd[qi % 2]
                    for kvh in range(2):
                        CP("pool", qb_.a[0:64, kvh, 0, :], qT.a[0:64, kvh, cs], [qT.r], [qb_.r])
                        CP("pool", qb_.a[64:128, kvh, 1, :], qT.a[64:128, kvh, cs], [qT.r], [qb_.r])
                    if n < 16:
                        keys = [m for m in (n - 1, n, n + 1) if 0 <= m <= 15] + [16, 17]
                    else:
                        keys = [16, 17]
                    pts = []
                    for mi, m in enumerate(keys):
                        sb_ = nb()
                        for kvh in range(2):
                            MM(sb_.a[:, kvh * 256:(kvh + 1) * 256], kdT.a[:, kvh, m * 128:(m + 1) * 128], qb_.a[:, kvh].rearrange("p g i -> p (g i)"), True, True, [kdT.r, qb_.r], [sb_.r])
                        pt_ = PT[(qi % 2) * 5 + mi]
                        pts.append(pt_)
                        ACT(pt_.a.rearrange("p h i -> p (h i)"), sb_.a, AF.Exp, [sb_.r], [pt_.r])
                        if n < 16 and m == n - 1:
                            TT("pool", pt_.a, pt_.a, bc(maskB.a, [128, 4, 128], 1), ALU.mult, [pt_.r, maskB.r], [pt_.r])
                        elif n < 15 and m == n + 1:
                            TT("pool", pt_.a, pt_.a, bc(maskF.a, [128, 4, 128], 1), ALU.mult, [pt_.r, maskF.r], [pt_.r])
                    ob = nb()
                    for hq in range(4):
                        for mi, m in enumerate(keys):
                            MM(ob.a[:, hq * 65:(hq + 1) * 65], pts[mi].a[:, hq, :], v_aug.a[:, m, hq // 2, :], mi == 0, mi == len(keys) - 1, [pts[mi].r, v_aug.r], [ob.r])
                    o4 = ob.a[:, 0:260].rearrange("p (h e) -> p h e", h=4)
                    dn, yf_, zs, yt_ = den[qi % 2], yf[qi % 2], zsi[qi % 2], yti[qi % 2]
                    TT("dve", dn.a[:, 0:4], o4[:, :, 64], esink.a, ALU.add, [ob.r, esink.r], [dn.r])
                    P.op("dve", lambda e, o=dn.a[:, 0:4]: e.reciprocal(out=o, in_=o), [dn.r], [dn.r])
                    TT("dve", yf_.a, o4[:, :, 0:64], bc(dn.a[:, 0:4], [128, 4, 64], 2), ALU.mult, [ob.r, dn.r], [yf_.r])
                    zb = inproj_tm(wz, 0, 256, n)
                    ACT(zs.a, zb.a[:, 0:256], AF.Silu, [zb.r], [zs.r])
                    TT("pool", yt_.a, yf_.a.rearrange("p h e -> p (h e)"), zs.a, ALU.mult, [yf_.r, zs.r], [yt_.r])
                    pt = nb()
                    ptb = pt.a.bitcast(BF16)[:, 0:256]
                    for t in range(2):
                        TR(ptb[:, t * 128:(t + 1) * 128], yt_.a[:, t * 128:(t + 1) * 128], ident_b.a, [yt_.r, ident_b.r], [pt.r])
                    CP("act", ycT.a[:, :, cs], ptb.rearrange("p (t i) -> p t i", t=2), [pt.r], [ycT.r])
                for t in range(2):
                    DMA("sp", ycat_d[6 + t], ycT.a[:, t, :], [ycT.r], [])
                arena.reset(m_mix2)
                P.barrier()

            arena.reset(m_mix)
            P.barrier()
            if debug and l == n_layers - 1:
                ydb = arena.alloc([128, NT], BF16, "ydb")
                for i in range(8):
                    DMA("sp", ydb.a, ycat_d[i], [], [ydb.r])
                    DMA("sp", dbg_y[i], ydb.a, [ydb.r], [])
                arena.reset(m_mix)
                P.barrier()
            m0 = arena.mark()
            og = P.group(dedicated=True)
            if "D" not in phases:
                DMA("sp", out_d[0:128, :], grow[0].a, [grow[0].r], [])
                continue
            wout = arena.alloc([128, 8, D], BF16, "wout")
            wst = [arena.alloc([128, D], F32, "wst%d" % i) for i in range(2)]
            for k in range(8):
                load_cast(wout.a[:, k, :], wout.r, wout_d[l, k * 128:(k + 1) * 128, :], D, wst[k % 2], wres[l]["out"])
            ylb = [arena.alloc([128, 8, 128], BF16, "yl%d" % i) for i in range(2)]
            xb = [arena.alloc([128, D], F32, "xb%d" % i) for i in range(2)]
            sqfs = [arena.alloc([128, 512], F32, "sqf%d" % i) for i in range(2)]
            t1 = [arena.alloc([128, D], F32, "t1_%d" % i) for i in range(2)]
            xo = [arena.alloc([128, D], F32, "xo%d" % i) for i in range(2)]
            for c in range(NCH):
                if last and c >= 16:
                    continue
                if "1" in phases:
                    continue
                yl = ylb[c % 2]
                if "5" in phases:
                    DMA("sp", yl.a, ycat_d[:, :, c * 128:(c + 1) * 128].rearrange("k p t -> p k t"), [], [yl.r])
                else:
                    for k_ in range(8):
                        DMA("sp", yl.a[:, k_, :], ycat_d[k_, :, c * 128:(c + 1) * 128], [], [yl.r])
                xt = xb[c % 2]
                if from_x:
                    src = x_d[c * 128:(c + 1) * 128, :] if c < 16 else ctx_d[(c - 16) * 128:(c - 15) * 128, :]
                else:
                    src = xs_d[c * 128:(c + 1) * 128, :]
                DMA("sp", xt.a, src, [], [xt.r])
                w = 0 if c < 16 else 1
                tt_, xo_ = t1[c % 2], xo[c % 2]
                hb = []
                for half in range(2):
                    b = nb()
                    hb.append(b)
                    for k in range(8):
                        MM(b.a, yl.a[:, k, :], wout.a[:, k, half * 512:(half + 1) * 512], k == 0, k == 7, [yl.r, wout.r], [b.r])
                    sqf = sqfs[half]
                    ACT(sqf.a, b.a, AF.Square, [b.r], [sqf.r])
                    P.op("dve", lambda e, o=ss.a[:, 2 * c + half:2 * c + half + 1], i=sqf.a: e.tensor_reduce(out=o, in_=i, axis=AX.X, op=ALU.add), [sqf.r], [ss.r])
                    TT("dve", tt_.a[:, half * 512:(half + 1) * 512], b.a, grow[w].a[:, half * 512:(half + 1) * 512], ALU.mult, [b.r, grow[w].r], [tt_.r])
                TT("dve", rstd.a[:, c:c + 1], ss.a[:, 2 * c:2 * c + 1], ss.a[:, 2 * c + 1:2 * c + 2], ALU.add, [ss.r], [rstd.r])
                ACT(rstd.a[:, c:c + 1], rstd.a[:, c:c + 1], AF.Sqrt, [rstd.r, cst.r], [rstd.r], bias=cst.a[:, 0:1], scale=1.0 / D)
                P.op("dve", lambda e, o=rstd.a[:, c:c + 1]: e.reciprocal(out=o, in_=o), [rstd.r], [rstd.r])
                STT("dve", xo_.a, tt_.a, rstd.a[:, c:c + 1], xt.a, ALU.mult, ALU.add, [tt_.r, rstd.r, xt.r], [xo_.r])
                if "2" in phases:
                    pass
                elif last:
                    DMA("sp", out_d[c * 128:(c + 1) * 128, :], xo_.a, [xo_.r], [])
                else:
                    DMA("sp", xs_d[c * 128:(c + 1) * 128, :], xo_.a, [xo_.r], [])
                if debug and l == n_layers - 1:
                    DMA("sp", dbg_xs[c * 128:(c + 1) * 128, :], xo_.a, [xo_.r], [])
            arena.reset(m0)
            P.barrier()
        P.barrier()
        P.replay()
        nc._arena_log = arena.log
        print("arena peak words", arena.peak, "ops", {k: len(v) for k, v in P.ops.items()})
    return nc


MIXERS_EXTRA = []


def kernel(**inputs):
    maps = _prep_inputs(inputs, sharded=False)
    nc = build_nc(sharded=False)
    res = run_bass_kernel_spmd(nc, maps, core_ids=list(range(8)))
    return np.stack([np.asarray(r["out"], dtype=np.float32) for r in res.results], axis=0)
```

```python
import numpy as np
from contextlib import ExitStack
import ml_dtypes
import concourse.bass as bass
import concourse.mybir as mybir
from concourse.bass_utils import run_bass_kernel_spmd

F32 = mybir.dt.float32
BF16 = mybir.dt.bfloat16
AF = mybir.ActivationFunctionType
ALU = mybir.AluOpType
AX = mybir.AxisListType

D = 1024
T = 2048
LC = 256
NT = T + LC
NCH = NT // 128
DEPTH = 4
EPS = 1e-6
ENGS = ("pe", "act", "dve", "pool", "sp")


class Res:
    __slots__ = ("name", "w", "r")

    def __init__(self, name=""):
        self.name = name
        self.w = None
        self.r = {}


class DmaGroup:
    def __init__(self, sem, idx, base):
        self.sem = sem
        self.n = 0
        self.key = ("g", idx)
        self.base = base

    @property
    def total(self):
        return self.base + 16 * self.n


class Prog:
    def __init__(self, nc, stack, nchan=24):
        self.nc = nc
        self.stack = stack
        self.ops = {e: [] for e in ENGS}
        self.count = {e: 0 for e in ENGS}
        self.waited = {e: {} for e in ENGS}
        self.sem = {e: stack.enter_context(nc.semaphore("sem_" + e)) for e in ENGS}
        self.chan_sem = [stack.enter_context(nc.semaphore("semc%d" % i)) for i in range(nchan)]
        self.chan_last = [None] * nchan
        self.chan_rr = 0
        self.groups = []
        self.gmap = {}
        self.open_groups = []

    def group(self, dedicated=False):
        if dedicated:
            sem = self.stack.enter_context(self.nc.semaphore("semd%d" % len(self.groups)))
            g = DmaGroup(sem, len(self.groups), 0)
            g.prev = None
            g.issued = set()
            self.groups.append(g)
            self.gmap[g.key] = g
            self.open_groups.append(g)
            return g
        ch = self.chan_rr
        self.chan_rr = (self.chan_rr + 1) % len(self.chan_sem)
        prev = self.chan_last[ch]
        base = prev.total if prev is not None else 0
        g = DmaGroup(self.chan_sem[ch], len(self.groups), base)
        g.prev = prev
        g.issued = set()
        self.chan_last[ch] = g
        self.groups.append(g)
        self.gmap[g.key] = g
        self.open_groups.append(g)
        return g

    def _deps(self, eng, reads, writes):
        deps = {}

        def add(k, v, same_ok):
            if k == eng and not same_ok and eng == "pe":
                return
            if k in deps:
                if isinstance(v, int):
                    deps[k] = max(deps[k], v)
            else:
                deps[k] = v

        for r in reads:
            if r.w is not None:
                add(r.w[0], r.w[1], True)
        for r in writes:
            if r.w is not None:
                add(r.w[0], r.w[1], False)
            for k, v in r.r.items():
                add(k, v, False)
        out = []
        wd = self.waited[eng]
        for k, v in deps.items():
            if isinstance(k, tuple):
                if wd.get(k):
                    continue
                wd[k] = True
                out.append((k, None))
            else:
                if wd.get(k, 0) >= v:
                    continue
                wd[k] = v
                out.append((k, v))
        return out

    def op(self, eng, fn, reads=(), writes=()):
        waits = self._deps(eng, reads, writes)
        self.count[eng] += 1
        idx = self.count[eng]
        self.ops[eng].append((fn, waits, None))
        for r in reads:
            r.r[eng] = idx
        for r in writes:
            r.w = (eng, idx)
            r.r = {}
        return idx

    def dma(self, eng, group, fn, reads=(), writes=()):
        waits = self._deps(eng, reads, writes)
        if eng not in group.issued:
            group.issued.add(eng)
            if group.prev is not None and not self.waited[eng].get(group.prev.key):
                self.waited[eng][group.prev.key] = True
                waits.append((group.prev.key, None))
        group.n += 1
        self.ops[eng].append((fn, waits, group))
        for r in reads:
            r.r[group.key] = None
        for r in writes:
            r.w = (group.key, None)
            r.r = {}

    def wait_group(self, eng, group):
        self.ops[eng].append((None, [(group.key, None)], None))

    def barrier(self):
        comp = ("pe", "act", "dve", "pool")
        for e in ENGS:
            waits = []
            for f in comp:
                if f != e and self.count[f] > self.waited[e].get(f, 0):
                    self.waited[e][f] = self.count[f]
                    waits.append((f, self.count[f]))
            for g in self.open_groups:
                if g.n > 0 and not self.waited[e].get(g.key):
                    self.waited[e][g.key] = True
                    waits.append((g.key, None))
            if waits:
                self.ops[e].append((None, waits, None))
        self.open_groups = [g for g in self.open_groups if g.n == 0]

    def replay(self):
        nc = self.nc

        def run(name, e):
            own = self.sem[name]
            for fn, waits, group in self.ops[name]:
                for k, v in waits:
                    if isinstance(k, tuple):
                        g = self.gmap[k]
                        e.wait_ge(g.sem, g.total)
                    else:
                        e.wait_ge(self.sem[k], v)
                if fn is None:
                    continue
                ins = fn(e)
                if group is not None:
                    ins.then_inc(group.sem, 16)
                else:
                    ins.then_inc(own, 1)

        with nc.Block() as block:
            @block.tensor
            def _(e):
                run("pe", e)

            @block.scalar
            def _(e):
                run("act", e)

            @block.vector
            def _(e):
                run("dve", e)

            @block.gpsimd
            def _(e):
                run("pool", e)

            @block.sync
            def _(e):
                run("sp", e)


def _sw(cols, nheads):
    c = np.asarray(cols).reshape(nheads, 2, 32)
    return c[:, ::-1, :].reshape(-1)


O_LRUX, O_LRUZ, O_GQKV, O_GZ, O_GA, O_GB = 0, 256, 512, 1280, 1536, 1544
O_RQ, O_RK, O_RV, O_RZ = 1552, 1808, 2064, 2320
O_SQ, O_SK, O_SV, O_SZ = 2576, 2832, 2960, 3088
AR = np.arange


def _colperm():
    A = np.concatenate([AR(O_LRUX, O_LRUX + 256), AR(O_LRUZ, O_LRUZ + 256)])
    B = np.concatenate([AR(O_GQKV, O_GQKV + 768), AR(O_GA, O_GA + 16)])
    Bz = AR(O_GZ, O_GZ + 256)
    rq, rk = AR(O_RQ, O_RQ + 256), AR(O_RK, O_RK + 256)
    C = np.concatenate([rq, _sw(rq, 4), rk, _sw(rk, 4), AR(O_RV, O_RV + 256)])
    Cz = AR(O_RZ, O_RZ + 256)
    sq = AR(O_SQ, O_SQ + 256)
    sk = AR(O_SK, O_SK + 128)
    kd = np.concatenate([sk[0:64], sk[0:64], sk[64:128], sk[64:128]])
    Dm = np.concatenate([sq, _sw(sq, 4), kd, _sw(kd, 4), AR(O_SV, O_SV + 128)])
    Dz = AR(O_SZ, O_SZ + 256)
    parts = [A, B, Bz, C, Cz, Dm, Dz]
    offs = np.cumsum([0] + [len(p) for p in parts])
    return np.concatenate(parts), offs


COLPERM, COLOFF = _colperm()
NCOL = int(COLOFF[-1])
WA, WB, WBZ, WC, WCZ, WD, WDZ = [int(v) for v in COLOFF[:7]]
NCP = 54
NRS = 92


def _rope_tables():
    inv64 = 10000.0 ** (-np.arange(0, 64, 2, dtype=np.float32) / 64)
    ang1 = np.arange(T, dtype=np.float32)[:, None] * inv64[None, :]
    inv32 = 10000.0 ** (-np.arange(0, 32, 2, dtype=np.float32) / 32)
    row = np.repeat(np.arange(T // 64, dtype=np.float32), 64)
    col = np.tile(np.arange(64, dtype=np.float32), T // 64)
    ang2 = np.concatenate([row[:, None] * inv32[None, :], col[:, None] * inv32[None, :]], axis=-1)
    out = np.zeros((4, 128, T), np.float32)
    for i, ang in enumerate((ang1, ang2)):
        c = np.cos(ang).T
        s = np.sin(ang).T
        ctab = np.concatenate([c, c, c, c], axis=0)
        stab = np.concatenate([-s, s, -s, s], axis=0)
        out[2 * i] = ctab
        out[2 * i + 1] = stab
    return out.astype(ml_dtypes.bfloat16)


def _prep_inputs(inp, sharded=False):
    L = DEPTH
    f = lambda a: np.ascontiguousarray(np.asarray(a, dtype=np.float32))
    w_in_p = f(inp["w_in"][:, :, COLPERM])
    colp = np.zeros((L, 128, NCP), np.float32)
    fm = lambda v: np.asarray(v).reshape(-1, 128).T
    for l in range(L):
        c = 0
        colp[l, :, c:c + 8] = fm(inp["pre_norm_g"][l]); c += 8
        for k in range(4):
            colp[l, :, c:c + 2] = fm(inp["lru_conv_w"][l, k]); c += 2
        colp[l, :, c:c + 2] = fm(inp["lru_conv_b"][l]); c += 2
        for nm in ("lru_b_r", "lru_b_i", "lru_lambda"):
            for d in range(2):
                colp[l, :, c:c + 2] = fm(inp[nm][l, d]); c += 2
        for k in range(4):
            colp[l, :, c:c + 6] = fm(inp["gdn_conv_w"][l, k]); c += 6
        assert c == NCP
    rows = np.zeros((L, NRS), np.float32)
    for l in range(L):
        rows[l, 0:8] = np.asarray(inp["gdn_a_log"][l]).reshape(-1)
        rows[l, 8:16] = np.asarray(inp["gdn_dt_bias"][l]).reshape(-1)
        rows[l, 16:80] = np.asarray(inp["gdn_norm_g"][l])
        rows[l, 80:88] = np.asarray(inp["ret_decay_logit"][l]).reshape(-1)
        rows[l, 88:92] = np.asarray(inp["swa_sink"][l])
    wg = np.ascontiguousarray(np.stack([f(inp["lru_w_r"]), f(inp["lru_w_i"])], axis=1))
    shared = {
        "w_mod": f(inp["w_mod"]), "b_mod": f(inp["b_mod"]), "post_g": f(inp["post_norm_g"]),
        "w_in": w_in_p, "w_out": f(inp["w_out"]), "colp": colp, "rows": rows, "lruw": wg,
        "rope": _rope_tables(),
        "bmodc": np.ascontiguousarray(f(inp["b_mod"]).reshape(L, 24, 128).transpose(0, 2, 1)),
    }
    maps = []
    for b in range(8):
        sv = np.zeros((128, 16), np.float32)
        sv[:, 0:8] = fm(inp["c"][b])
        sv[:, 8:16] = fm(inp["c_ctx"])
        m = dict(shared)
        m["x"] = f(inp["x"][b])
        m["ctx"] = f(inp["ctx"][b])
        m["svec"] = sv
        if sharded:
            for k in ("w_mod", "w_in", "w_out"):
                m[k] = np.ascontiguousarray(shared[k][:, b * 128:(b + 1) * 128, :])
        maps.append(m)
    return maps


class Tl:
    __slots__ = ("a", "r")

    def __init__(self, a, r):
        self.a = a
        self.r = r


def _prod(s):
    n = 1
    for v in s:
        n *= v
    return n


class Arena:
    def __init__(self, nc, st, nwords, name="arena"):
        self.t = st.enter_context(nc.sbuf_tensor(name, [128, nwords], F32))
        self.nwords = nwords
        self.off = 0
        self.peak = 0

    def alloc(self, shape, dtype, name=""):
        n = _prod(shape[1:])
        words = n if dtype == F32 else (n + 1) // 2
        words = (words + 7) // 8 * 8
        assert self.off + words <= self.nwords, ("arena overflow", name, self.off, words, self.nwords)
        ap = self.t[:, self.off:self.off + words]
        if dtype != F32:
            ap = ap.bitcast(dtype)
        ap = ap[:, 0:n]
        if len(shape) == 3:
            ap = ap.rearrange("p (a b) -> p a b", a=shape[1])
        elif len(shape) == 4:
            ap = ap.rearrange("p (a b c) -> p a b c", a=shape[1], b=shape[2])
        if not hasattr(self, "log"):
            self.log = {}
        self.log[name] = (self.off, tuple(shape), "f32" if dtype == F32 else "bf16")
        self.off += words
        self.peak = max(self.peak, self.off)
        return Tl(ap, Res(name))

    def mark(self):
        return self.off

    def reset(self, m):
        self.off = m


BLKS = [(0, 512), (512, 512), (1024, 512), (1536, 512), (2048, 256)]


def build_nc(n_layers=DEPTH, mixers="ABCD", debug=False, first_from_scratch=False, final_layer=True, phases="ABD", slim=False, sharded=False):
    nc = bass.Bass("TRN2", target_bir_lowering=False)
    DEPTH = n_layers if slim else 4
    dt_in = lambda name, shape, dt=F32: nc.dram_tensor(name, shape, dt, kind="ExternalInput").ap()
    x_d = dt_in("x", [T, D])
    ctx_d = dt_in("ctx", [LC, D])
    svec_d = dt_in("svec", [128, 16])
    KS = 128 if sharded else D
    wmod_in = dt_in("w_mod", [DEPTH, KS, 3 * D])
    bmod_d = dt_in("b_mod", [DEPTH, 3 * D])
    bmodc_d = dt_in("bmodc", [DEPTH, 128, 24])
    postg_d = dt_in("post_g", [DEPTH, D])
    win_in = dt_in("w_in", [DEPTH, KS, NCOL])
    wout_in = dt_in("w_out", [DEPTH, KS, D])
    if sharded:
        wsh = {k: nc.dram_tensor("wsh_" + k, [DEPTH, 128, n], F32, kind="Internal").ap() for k, n in (("mod", 3 * D), ("in", NCOL), ("out", D))}
        wfull = {k: nc.dram_tensor("wfull_" + k, [DEPTH, D, n], F32, kind="Internal").ap() for k, n in (("mod", 3 * D), ("in", NCOL), ("out", D))}
        wmod_d, win_d, wout_d = wfull["mod"], wfull["in"], wfull["out"]
    else:
        wmod_d, win_d, wout_d = wmod_in, win_in, wout_in
    colp_d = dt_in("colp", [DEPTH, 128, NCP])
    rows_d = dt_in("rows", [DEPTH, NRS])
    lruw_d = dt_in("lruw", [DEPTH, 2, 2, 4, 64, 64])
    rope_d = dt_in("rope", [4, 128, T], BF16)
    out_d = nc.dram_tensor("out", [T, D], F32, kind="ExternalOutput").ap()
    xs_d = nc.dram_tensor("xs", [NT, D], F32, kind="Internal").ap()
    ycat_d = nc.dram_tensor("ycat_s", [8, 128, NT], BF16, kind="Internal").ap()
    if debug:
        dbg_h = nc.dram_tensor("dbg_h", [8, 128, NT], BF16, kind="ExternalOutput").ap()
        dbg_y = nc.dram_tensor("dbg_y", [8, 128, NT], BF16, kind="ExternalOutput").ap()
        dbg_xs = nc.dram_tensor("dbg_xs", [NT, D], F32, kind="ExternalOutput").ap()

    with ExitStack() as st:
        P = Prog(nc, st)
        sbt = lambda name, shape, dt=F32: Tl(st.enter_context(nc.sbuf_tensor("sb_" + name, shape, dt))[:], Res(name))
        banks = [Tl(st.enter_context(nc.psum_tensor("bank%d" % i, [128, 512], F32))[:], Res("bank%d" % i)) for i in range(8)]
        bank_rr = [0]

        def nb():
            b = banks[bank_rr[0]]
            bank_rr[0] = (bank_rr[0] + 1) % 8
            return b

        def ACT(out, in_, func, R, W, bias=None, scale=None, accum=None):
            kw = {}
            if bias is not None:
                kw["bias"] = bias
            if scale is not None:
                kw["scale"] = scale
            if accum is not None:
                kw["accum_out"] = accum
            P.op("act", lambda e: e.activation(out=out, in_=in_, func=func, **kw), R, W)

        def TT(eng, out, in0, in1, op, R, W):
            P.op(eng, lambda e: e.tensor_tensor(out=out, in0=in0, in1=in1, op=op), R, W)

        def TS(eng, out, in0, s1, op0, R, W, s2=None, op1=None):
            if op1 is None:
                P.op(eng, lambda e: e.tensor_scalar(out=out, in0=in0, scalar1=s1, scalar2=None, op0=op0), R, W)
            else:
                P.op(eng, lambda e: e.tensor_scalar(out=out, in0=in0, scalar1=s1, scalar2=s2, op0=op0, op1=op1), R, W)

        def STT(eng, out, in0, scalar, in1, op0, op1, R, W):
            P.op(eng, lambda e: e.scalar_tensor_tensor(out=out, in0=in0, scalar=scalar, in1=in1, op0=op0, op1=op1), R, W)

        def CP(eng, out, in_, R, W):
            if eng == "act":
                P.op("act", lambda e: e.activation(out=out, in_=in_, func=AF.Copy), R, W)
            else:
                P.op(eng, lambda e: e.tensor_copy(out=out, in_=in_), R, W)

        def MSET(eng, out, val, W):
            P.op(eng, lambda e: e.memset(out, val), [], W)

        def MM(out, lhsT, rhs, start, stop, R, W):
            P.op("pe", lambda e: e.matmul(out, lhsT=lhsT, rhs=rhs, start=start, stop=stop), R, W)

        def TR(out, in_, ident, R, W):
            P.op("pe", lambda e: e.transpose(out=out, in_=in_, identity=ident), R, W)

        def DMA(eng, out, in_, R, W, g=None):
            if g is None:
                g = P.group()
            P.dma(eng, g, lambda e: e.dma_start(out=out, in_=in_), R, W)
            return g

        def SCAN(out, d0, d1, init, R, W):
            P.op("dve", lambda e: e.tensor_tensor_scan(out=out, data0=d0, data1=d1, initial=init, op0=ALU.mult, op1=ALU.add), R, W)

        def rev(ap2d):
            n = ap2d.shape[1]
            return bass.AP(ap2d.tensor, ap2d.offset + (n - 1), [list(ap2d.ap[0]), [-1, n]])

        def bc(ap, shape, axis):
            return ap.unsqueeze(axis).to_broadcast(shape)

        wres = [{k: Res("w_%s_%d" % (k, l_)) for k in ("mod", "in", "out")} for l_ in range(DEPTH)]
        if sharded:
            srcs = {"mod": wmod_in, "in": win_in, "out": wout_in}
            shres = {}
            for l_ in range(n_layers):
                for k in ("mod", "in", "out"):
                    shres[(l_, k)] = Res("sh")
                    DMA("sp", wsh[k][l_], srcs[k][l_], [], [shres[(l_, k)]])
            for l_ in range(n_layers):
                for k in ("mod", "in", "out"):
                    g_ = P.group()
                    P.dma("pool", g_, lambda e, i_=wsh[k][l_], o_=wfull[k][l_]: e.collective_compute(
                        "AllGather", op=ALU.bypass, replica_groups=[list(range(8))], ins=[i_], outs=[o_]),
                        [shres[(l_, k)]], [wres[l_][k]])

        ones_f = sbt("ones_f", [128, 128])
        ident_f = sbt("ident_f", [128, 128])
        ident_b = sbt("ident_b", [128, 128], BF16)
        cst = sbt("cst", [128, 4])
        MSET("pool", ones_f.a, 1.0, [ones_f.r])
        MSET("pool", cst.a[:, 0:1], EPS, [cst.r])
        MSET("pool", cst.a[:, 1:2], 1.0, [cst.r])
        MSET("pool", cst.a[:, 2:3], -1.0, [cst.r])
        MSET("pool", cst.a[:, 3:4], 0.0, [cst.r])

        def aff(out_t, pattern, cm, op, base=0, fill=0.0, src=None):
            src = src or ones_f
            P.op("pool", lambda e: e.affine_select(out=out_t.a, in_=src.a, pattern=pattern, compare_op=op, fill=fill, base=base, channel_multiplier=cm), [src.r], [out_t.r])

        aff(ident_f, [[-1, 128]], 1, ALU.is_equal)
        CP("dve", ident_b.a, ident_f.a, [ident_f.r], [ident_b.r])
        maskF = sbt("maskF", [128, 128])
        maskB = sbt("maskB", [128, 128])
        blockmask = sbt("blockmask", [128, 128])
        aff(maskF, [[1, 128]], -1, ALU.is_ge)
        aff(maskB, [[-1, 128]], 1, ALU.is_ge)
        MSET("pool", blockmask.a, 0.0, [blockmask.r])
        MSET("pool", blockmask.a[0:64, 0:64], 1.0, [blockmask.r])
        MSET("pool", blockmask.a[64:128, 64:128], 1.0, [blockmask.r])
        blockones_b = sbt("blockones_b", [128, 128], BF16)
        CP("dve", blockones_b.a, blockmask.a, [blockmask.r], [blockones_b.r])
        offdiag = sbt("offdiag", [128, 128])
        TT("dve", offdiag.a, ones_f.a, ident_f.a, ALU.subtract, [ones_f.r, ident_f.r], [offdiag.r])
        maskneg = sbt("maskneg", [128, 2, 4, 128])
        for d_, mk_ in ((0, maskF), (1, maskB)):
            TS("dve", maskneg.a[:, d_], bc(mk_.a, [128, 4, 128], 1), -1.0, ALU.add, [mk_.r], [maskneg.r], s2=30000.0, op1=ALU.mult)
        bd = []
        Ebuf = sbt("Ebuf", [128, 128])
        for si, sz in enumerate((8, 16, 32, 64)):
            E = Ebuf
            aff(E, [[1, 128]], -sz, ALU.is_ge)
            P.op("pool", lambda e, E=E, sz=sz: e.affine_select(out=E.a, in_=E.a, pattern=[[-1, 128]], compare_op=ALU.is_ge, fill=0.0, base=sz - 1, channel_multiplier=sz), [E.r], [E.r])
            ng = 128 // sz
            bb = nb()
            MM(bb.a[:, 0:128], E.a[0:ng, :], E.a[0:ng, :], True, True, [E.r], [bb.r])
            m_ = sbt("bd%d" % sz, [128, 128])
            CP("dve", m_.a, bb.a[:, 0:128], [bb.r], [m_.r])
            bd.append(m_)
        bdm = [bd[0]]
        offm = []
        for si in range(4):
            o_ = sbt("off%d" % si, [128, 128])
            hi = bd[si + 1] if si < 3 else ones_f
            TT("dve", o_.a, hi.a, bd[si].a, ALU.subtract, [hi.r, bd[si].r], [o_.r])
            offm.append(o_)
        iota_i = sbt("iota_i", [128, 128], mybir.dt.int32)
        iota_ij = sbt("iota_ij", [128, 128])
        P.op("pool", lambda e: e.iota(out=iota_i.a, pattern=[[1, 128]], base=0, channel_multiplier=-1), [], [iota_i.r])
        CP("dve", iota_ij.a, iota_i.a, [iota_i.r], [iota_ij.r])
        pidx_i = sbt("pidx_i", [128, 1], mybir.dt.int32)
        pidx = sbt("pidx", [128, 1])
        P.op("pool", lambda e: e.iota(out=pidx_i.a, pattern=[[0, 1]], base=0, channel_multiplier=1), [], [pidx_i.r])
        CP("dve", pidx.a, pidx_i.a, [pidx_i.r], [pidx.r])
        rowt = sbt("rowt", [128, NRS])
        s2 = sbt("s2", [128, 8, 2])
        svt = sbt("svt", [128, 16])
        DMA("sp", svt.a, svec_d[:, :], [], [svt.r])
        ACT(s2.a.rearrange("p k w -> p w k"), svt.a.rearrange("p (w k) -> p w k", w=2), AF.Silu, [svt.r], [s2.r])
        modc = sbt("modc", [128, 16, 2])
        acol = sbt("acol", [128, 8, 2])
        grow = [sbt("grow%d" % w, [128, D]) for w in range(2)]
        colp = sbt("colp", [128, NCP])
        ss = sbt("ss", [128, 2 * NCH])
        rstd = sbt("rstd", [128, 2 * NCH])
        rope = sbt("rope", [128, 4, T], BF16)
        for i in range(4):
            DMA("sp", rope.a[:, i, :], rope_d[i], [], [rope.r])

        ycat_res = Res("ycat_dram")
        arena = Arena(nc, st, 43200)
        hT = arena.alloc([128, 8, NT], BF16, "hT")
        base_mark = arena.mark()

        def load_cast(dst_ap, dst_res, src_rows_ap, ncols, stage, wr=None):
            DMA("sp", stage.a[:, :ncols], src_rows_ap, [wr] if wr is not None else [], [stage.r])
            CP("pool", dst_ap, stage.a[:, :ncols], [stage.r], [dst_res])

        for l in range(n_layers):
            last = final_layer and (l == n_layers - 1)
            from_x = (l == 0) and not first_from_scratch
            arena.reset(base_mark)
            P.barrier()
            DMA("sp", colp.a, colp_d[l], [], [colp.r])
            DMA("sp", rowt.a, rows_d[l, :].partition_broadcast(128), [], [rowt.r])
            m0 = arena.mark()
            wmb = [arena.alloc([128, 8, 512], F32, "wm%d" % i) for i in range(2)]
            bgrow = arena.alloc([128, D], F32, "bgrow")
            pgrow = arena.alloc([128, D], F32, "pgrow")
            bmc = arena.alloc([128, 24], F32, "bmc")
            sbc = arena.alloc([128, 8, 2, 128], F32, "sbc")
            CP("dve", sbc.a, bc(s2.a, [128, 8, 2, 128], 3), [s2.r], [sbc.r])
            DMA("sp", bgrow.a, bmod_d[l, 2 * D:3 * D].partition_broadcast(128), [], [bgrow.r])
            DMA("sp", pgrow.a, postg_d[l, :].partition_broadcast(128), [], [pgrow.r])
            DMA("sp", bmc.a, bmodc_d[l], [], [bmc.r])
            pmod = nb()
            pmv = pmod.a[:, 0:32].rearrange("p (j w) -> p j w", w=2)
            wsrc = wmod_d[l].rearrange("(k p) n -> p k n", p=128)
            for cg in range(6):
                wm = wmb[cg % 2]
                DMA("sp", wm.a, wsrc[:, :, cg * 512:(cg + 1) * 512], [wres[l]["mod"]], [wm.r])
                if cg < 4:
                    for jj in range(4):
                        j = cg * 4 + jj
                        for k in range(8):
                            MM(pmv[:, j, :], wm.a[:, k, jj * 128:(jj + 1) * 128], s2.a[:, k, :], k == 0, k == 7, [wm.r, s2.r], [pmod.r])
                else:
                    cs = slice((cg - 4) * 512, (cg - 3) * 512)
                    for w in range(2):
                        pg = nb()
                        for k in range(8):
                            MM(pg.a, sbc.a[:, k, w, :], wm.a[:, k, :], k == 0, k == 7, [wm.r, sbc.r], [pg.r])
                        TT("dve", grow[w].a[:, cs], pg.a, bgrow.a[:, cs], ALU.add, [pg.r, bgrow.r], [grow[w].r])
                        TT("pool", grow[w].a[:, cs], grow[w].a[:, cs], pgrow.a[:, cs], ALU.mult, [grow[w].r, pgrow.r], [grow[w].r])
            TT("dve", modc.a, pmv, bc(bmc.a[:, 0:16], [128, 16, 2], 2), ALU.add, [pmod.r, bmc.r], [modc.r])
            STT("dve", acol.a, modc.a[:, 8:16, :], 1.0, bc(colp.a[:, 0:8], [128, 8, 2], 2), ALU.add, ALU.mult, [modc.r, colp.r], [acol.r])
            arena.reset(m0)
            P.barrier()
            m0 = arena.mark()
            if "B" not in phases:
                og = P.group(dedicated=True)
                DMA("sp", out_d[0:128, :], grow[0].a, [grow[0].r], [])
                continue
            xb = [arena.alloc([128, D], F32, "xb%d" % i) for i in range(2)]
            junk = arena.alloc([128, D], BF16, "junk")
            xn = [arena.alloc([128, D], BF16, "xn%d" % i) for i in range(2)]
            tmpf = arena.alloc([128, 8, 128], F32, "tmpf")
            for c in range(NCH):
                xt = xb[c % 2]
                if from_x:
                    src = x_d[c * 128:(c + 1) * 128, :] if c < 16 else ctx_d[(c - 16) * 128:(c - 15) * 128, :]
                else:
                    src = xs_d[c * 128:(c + 1) * 128, :]
                DMA("sp", xt.a, src, [], [xt.r])
                ACT(junk.a, xt.a, AF.Square, [xt.r], [junk.r, ss.r], accum=ss.a[:, c:c + 1])
                ACT(rstd.a[:, c:c + 1], ss.a[:, c:c + 1], AF.Sqrt, [ss.r, cst.r], [rstd.r], bias=cst.a[:, 0:1], scale=1.0 / D)
                P.op("dve", lambda e, o=rstd.a[:, c:c + 1]: e.reciprocal(out=o, in_=o), [rstd.r], [rstd.r])
                xnt = xn[c % 2]
                TS("dve", xnt.a, xt.a, rstd.a[:, c:c + 1], ALU.mult, [xt.r, rstd.r], [xnt.r])
                pt = nb()
                ptb = pt.a.bitcast(BF16).rearrange("p (j t) -> p j t", j=8)
                for j in range(8):
                    TR(ptb[:, j, :], xnt.a[:, j * 128:(j + 1) * 128], ident_b.a, [xnt.r, ident_b.r], [pt.r])
                w = 0 if c < 16 else 1
                TT("dve", tmpf.a, ptb, bc(acol.a[:, :, w], [128, 8, 128], 2), ALU.mult, [pt.r, acol.r], [tmpf.r])
                TT("pool", hT.a[:, :, c * 128:(c + 1) * 128], tmpf.a, bc(modc.a[:, 0:8, w], [128, 8, 128], 2), ALU.add, [tmpf.r, modc.r], [hT.r])
            arena.reset(m0)
            P.barrier()
            if debug and l == n_layers - 1:
                DMA("sp", dbg_h.rearrange("k p t -> p k t"), hT.a, [hT.r], [])

            m_mix = arena.mark()
            if mixers != "ABCD":
                zt = arena.alloc([128, NT], BF16, "zt")
                MSET("pool", zt.a, 0.0, [zt.r])
                for i in range(8):
                    DMA("sp", ycat_d[i], zt.a, [zt.r], [ycat_res])
                arena.reset(m_mix)
                P.barrier()
            wbuf = arena.alloc([128, 8, 1280], BF16, "wbuf")
            wstage = [arena.alloc([128, 1280], F32, "wst%d" % i) for i in range(2)]
            m_mix2 = arena.mark()

            def load_w(coloff, ncols, dst):
                for k in range(8):
                    load_cast(dst.a[:, k, 0:ncols], dst.r, win_d[l, k * 128:(k + 1) * 128, coloff:coloff + ncols], ncols, wstage[k % 2], wres[l]["in"])

            def inproj_fm(wt, col0, t0, n, M=128):
                b = nb()
                for k in range(8):
                    MM(b.a[0:M, 0:n], wt.a[:, k, col0:col0 + M], hT.a[:, k, t0:t0 + n], k == 0, k == 7, [wt.r, hT.r], [b.r])
                return b

            def inproj_tm(wt, col0, ncols, c):
                b = nb()
                for k in range(8):
                    MM(b.a[:, 0:ncols], hT.a[:, k, c * 128:(c + 1) * 128], wt.a[:, k, col0:col0 + ncols], k == 0, k == 7, [wt.r, hT.r], [b.r])
                return b

            def store_ycat(yt, tile_idx):
                DMA("sp", ycat_d[tile_idx], yt.a, [yt.r], [])

            if "A" in mixers:
                load_w(WA, 512, wbuf)
                dg = arena.alloc([128, 8, 128], BF16, "dg")
                for i in range(8):
                    TS("pool", dg.a[:, i, :], ident_f.a, colp.a[:, 8 + i:9 + i], ALU.mult, [ident_f.r, colp.r], [dg.r])
                wgb = arena.alloc([128, 8, 128], BF16, "wgb")
                wgs = arena.alloc([128, 8, 128], F32, "wgs")
                MSET("pool", wgs.a, 0.0, [wgs.r])
                for d_ in range(2):
                    for gi_ in range(2):
                        for blk_ in range(4):
                            t_, o_ = blk_ // 2, (blk_ % 2) * 64
                            DMA("sp", wgs.a[o_:o_ + 64, d_ * 4 + gi_ * 2 + t_, o_:o_ + 64], lruw_d[l, gi_, d_, blk_], [], [wgs.r])
                CP("pool", wgb.a, wgs.a, [wgs.r], [wgb.r])
                kcol = arena.alloc([128, 4], F32, "kcol")
                ACT(kcol.a, colp.a[:, 26:30], AF.Exp, [colp.r], [kcol.r], scale=-1.0)
                ACT(kcol.a, kcol.a, AF.Ln, [kcol.r, cst.r], [kcol.r], bias=cst.a[:, 1:2])
                TS("dve", kcol.a, kcol.a, -8.0, ALU.mult, [kcol.r], [kcol.r])
                xpad = arena.alloc([128, 2, 2310], BF16, "xpad")
                MSET("pool", xpad.a, 0.0, [xpad.r])
                ub = arena.alloc([128, 2, NT], BF16, "ub")
                for t in range(2):
                    for (t0, n) in BLKS:
                        b = inproj_fm(wbuf, t * 128, t0, n)
                        o0 = t0 + 2 if t0 < T else 2053
                        CP("act", xpad.a[:, t, o0:o0 + n], b.a[:, 0:n], [b.r], [xpad.r])
                for t in range(2):
                    for (t0, n) in BLKS:
                        j0 = t0 if t0 < T else 2051
                        b = nb()
                        for k in range(4):
                            MM(b.a[:, 0:n], dg.a[:, k * 2 + t, :], xpad.a[:, t, j0 + k:j0 + k + n], k == 0, k == 3, [dg.r, xpad.r], [b.r])
                        ACT(ub.a[:, t, t0:t0 + n], b.a[:, 0:n], AF.Identity, [b.r, colp.r], [ub.r], bias=colp.a[:, 16 + t:17 + t])
                a_buf = arena.alloc([128, NT], F32, "a_buf")
                b_buf = arena.alloc([128, NT], F32, "b_buf")
                hbuf = [arena.alloc([128, NT], F32, "h%d" % i) for i in range(2)]
                rr = [arena.alloc([128, 512], F32, "rr%d" % i) for i in range(2)]
                ii = [arena.alloc([128, 512], F32, "ii%d" % i) for i in range(2)]
                tq = [arena.alloc([128, 512], F32, "tq%d" % i) for i in range(2)]
                yt = arena.alloc([128, NT], BF16, "yt")
                for t in range(2):
                    for d in range(2):
                        for bi, (t0, n) in enumerate(BLKS):
                            r_, i_, q_ = rr[bi % 2], ii[bi % 2], tq[bi % 2]
                            b1 = nb()
                            MM(b1.a[:, 0:n], wgb.a[:, d * 4 + 0 + t, :], ub.a[:, t, t0:t0 + n], True, True, [wgb.r, ub.r], [b1.r])
                            ACT(r_.a[:, 0:n], b1.a[:, 0:n], AF.Sigmoid, [b1.r, colp.r], [r_.r], bias=colp.a[:, 18 + d * 2 + t:19 + d * 2 + t])
                            b2 = nb()
                            MM(b2.a[:, 0:n], wgb.a[:, d * 4 + 2 + t, :], ub.a[:, t, t0:t0 + n], True, True, [wgb.r, ub.r], [b2.r])
                            ACT(i_.a[:, 0:n], b2.a[:, 0:n], AF.Sigmoid, [b2.r, colp.r], [i_.r], bias=colp.a[:, 22 + d * 2 + t:23 + d * 2 + t])
                            ACT(a_buf.a[:, t0:t0 + n], r_.a[:, 0:n], AF.Exp, [r_.r, kcol.r], [a_buf.r], scale=kcol.a[:, d * 2 + t:d * 2 + t + 1])
                            TT("dve", q_.a[:, 0:n], a_buf.a[:, t0:t0 + n], a_buf.a[:, t0:t0 + n], ALU.mult, [a_buf.r], [q_.r])
                            ACT(q_.a[:, 0:n], q_.a[:, 0:n], AF.Sqrt, [q_.r, cst.r], [q_.r], bias=cst.a[:, 1:2], scale=-1.0)
                            TT("dve", q_.a[:, 0:n], q_.a[:, 0:n], i_.a[:, 0:n], ALU.mult, [q_.r, i_.r], [q_.r])
                            TT("pool", b_buf.a[:, t0:t0 + n], q_.a[:, 0:n], ub.a[:, t, t0:t0 + n], ALU.mult, [q_.r, ub.r], [b_buf.r])
                        h = hbuf[d]
                        if d == 0:
                            SCAN(h.a[:, T:NT], a_buf.a[:, T:NT], b_buf.a[:, T:NT], 0.0, [a_buf.r, b_buf.r], [h.r])
                            SCAN(h.a[:, 0:T], a_buf.a[:, 0:T], b_buf.a[:, 0:T], h.a[:, NT - 1:NT], [a_buf.r, b_buf.r, h.r], [h.r])
                        else:
                            SCAN(rev(h.a[:, T:NT]), rev(a_buf.a[:, T:NT]), rev(b_buf.a[:, T:NT]), 0.0, [a_buf.r, b_buf.r], [h.r])
                            SCAN(rev(h.a[:, 0:T]), rev(a_buf.a[:, 0:T]), rev(b_buf.a[:, 0:T]), h.a[:, T:T + 1], [a_buf.r, b_buf.r, h.r], [h.r])
                    for bi, (t0, n) in enumerate(BLKS):
                        b = inproj_fm(wbuf, 256 + t * 128, t0, n)
                        z_ = rr[bi % 2]
                        ACT(z_.a[:, 0:n], b.a[:, 0:n], AF.Silu, [b.r], [z_.r])
                        q_ = tq[bi % 2]
                        TT("dve", q_.a[:, 0:n], hbuf[0].a[:, t0:t0 + n], hbuf[1].a[:, t0:t0 + n], ALU.add, [hbuf[0].r, hbuf[1].r], [q_.r])
                        TT("pool", yt.a[:, t0:t0 + n], q_.a[:, 0:n], z_.a[:, 0:n], ALU.mult, [q_.r, z_.r], [yt.r])
                    store_ycat(yt, 0 + t)
                arena.reset(m_mix2)
                P.barrier()

            if "B" in mixers:
                load_w(WB, 784, wbuf)
                wz = arena.alloc([128, 8, 256], BF16, "wz")
                load_w(WBZ, 256, wz)
                qT = arena.alloc([128, 2, NT], BF16, "qT")
                kT = arena.alloc([128, 2, NT], BF16, "kT")
                k_tok = arena.alloc([128, NCH, 256], BF16, "k_tok")
                v_tok = arena.alloc([128, NCH, 256], BF16, "v_tok")
                g_tok = arena.alloc([128, NCH, 8], F32, "g_tok")
                nbeta = arena.alloc([128, NCH, 8], F32, "nbeta")
                beta = arena.alloc([128, NCH, 8], F32, "beta")
                nega = arena.alloc([128, 8], F32, "nega")
                mB = arena.mark()
                vT = arena.alloc([128, 2, NT], BF16, "vT")
                dgB = arena.alloc([128, 24, 128], BF16, "dgB")
                for i in range(24):
                    TS("pool", dgB.a[:, i, :], ident_f.a, colp.a[:, 30 + i:31 + i], ALU.mult, [ident_f.r, colp.r], [dgB.r])
                xpads = [arena.alloc([128, 2310], BF16, "xpad%d" % i) for i in range(2)]
                for xp in xpads:
                    MSET("pool", xp.a, 0.0, [xp.r])
                sl = [arena.alloc([128, 512], F32, "sl%d" % i) for i in range(2)]
                sqb = [arena.alloc([128, 512], BF16, "sqb%d" % i) for i in range(2)]
                rn = [arena.alloc([128, 512], F32, "rn%d" % i) for i in range(2)]
                cnt = 0
                for ti in range(6):
                    xp = xpads[ti % 2]
                    for (t0, n) in BLKS:
                        b = inproj_fm(wbuf, ti * 128, t0, n)
                        o0 = t0 + 2 if t0 < T else 2053
                        CP("act", xp.a[:, o0:o0 + n], b.a[:, 0:n], [b.r], [xp.r])
                    for (t0, n) in BLKS:
                        j0 = t0 if t0 < T else 2051
                        b = nb()
                        for k in range(4):
                            MM(b.a[:, 0:n], dgB.a[:, k * 6 + ti, :], xp.a[:, j0 + k:j0 + k + n], k == 0, k == 3, [dgB.r, xp.r], [b.r])
                        if ti >= 4:
                            ACT(vT.a[:, ti - 4, t0:t0 + n], b.a[:, 0:n], AF.Silu, [b.r], [vT.r])
                            continue
                        s_, q_, r_ = sl[cnt % 2], sqb[cnt % 2], rn[cnt % 2]
                        cnt += 1
                        ACT(s_.a[:, 0:n], b.a[:, 0:n], AF.Silu, [b.r], [s_.r])
                        TT("pool", q_.a[:, 0:n], s_.a[:, 0:n], s_.a[:, 0:n], ALU.mult, [s_.r], [q_.r])
                        b2 = nb()
                        MM(b2.a[:, 0:n], blockones_b.a, q_.a[:, 0:n], True, True, [blockones_b.r, q_.r], [b2.r])
                        ACT(r_.a[:, 0:n], b2.a[:, 0:n], AF.Sqrt, [b2.r, cst.r], [r_.r], bias=cst.a[:, 0:1])
                        P.op("dve", lambda e, o=r_.a[:, 0:n]: e.reciprocal(out=o, in_=o), [r_.r], [r_.r])
                        dst = qT if ti < 2 else kT
                        STT("dve", dst.a[:, ti % 2, t0:t0 + n], s_.a[:, 0:n], 0.125 if ti < 2 else 1.0, r_.a[:, 0:n], ALU.mult, ALU.mult, [s_.r, r_.r], [dst.r])
                ab = arena.alloc([128, NCH, 16], F32, "ab")
                for c in range(NCH):
                    for src, dstt in ((kT, k_tok), (vT, v_tok)):
                        pt = nb()
                        ptb = pt.a.bitcast(BF16)[:, 0:256]
                        for t in range(2):
                            TR(ptb[:, t * 128:(t + 1) * 128], src.a[:, t, c * 128:(c + 1) * 128], ident_b.a, [src.r, ident_b.r], [pt.r])
                        CP("act" if dstt is k_tok else "dve", dstt.a[:, c, :], ptb, [pt.r], [dstt.r])
                    b = inproj_tm(wbuf, 768, 16, c)
                    CP("dve", ab.a[:, c, :], b.a[:, 0:16], [b.r], [ab.r])
                ACT(nega.a, rowt.a[:, 0:8], AF.Exp, [rowt.r], [nega.r])
                TS("dve", nega.a, nega.a, -1.0, ALU.mult, [nega.r], [nega.r])
                TT("dve", g_tok.a, ab.a[:, :, 0:8], bc(rowt.a[:, 8:16], [128, NCH, 8], 1), ALU.add, [ab.r, rowt.r], [g_tok.r])
                ACT(g_tok.a, g_tok.a, AF.Exp, [g_tok.r], [g_tok.r])
                ACT(g_tok.a, g_tok.a, AF.Ln, [g_tok.r, cst.r], [g_tok.r], bias=cst.a[:, 1:2])
                TT("dve", g_tok.a, g_tok.a, bc(nega.a, [128, NCH, 8], 1), ALU.mult, [g_tok.r, nega.r], [g_tok.r])
                ACT(beta.a, ab.a[:, :, 8:16], AF.Sigmoid, [ab.r], [beta.r])
                TS("dve", nbeta.a, beta.a, -1.0, ALU.mult, [beta.r], [nbeta.r])
                P.barrier()
                arena.reset(mB)
                o_acc = arena.alloc([128, NCH, 256], F32, "o_acc")
                class WS:
                    pass
                wss = []
                for d in range(2):
                    w_ = WS()
                    stg = wstage[d]
                    w_.gTri = Tl(stg.a[:, 0:512].rearrange("p (h i) -> p h i", h=4), stg.r)
                    w_.DT = Tl(stg.a[:, 512:1024].rearrange("p (h i) -> p h i", h=4), stg.r)
                    w_.t1 = arena.alloc([128, 4, 128], BF16, "t1_%d" % d)
                    w_.NB = arena.alloc([128, 4, 128], BF16, "NB%d" % d)
                    w_.M = [arena.alloc([128, 4, 128], BF16, "M%d_%d" % (d, i)) for i in range(2)]
                    w_.N = [arena.alloc([128, 4, 128], BF16, "N%d_%d" % (d, i)) for i in range(2)]
                    w_.PT = [arena.alloc([128, 4, 128], BF16, "PT%d_%d" % (d, i)) for i in range(2)]
                    w_.Mb = w_.M[1]
                    w_.Nb = w_.N[1]
                    w_.U = w_.PT[0]
                    w_.Tm = w_.PT[1]
                    w_.Y1 = arena.alloc([128, 4, 128], BF16, "Y1_%d" % d)
                    w_.Y2 = arena.alloc([128, 4, 128], BF16, "Y2_%d" % d)
                    w_.att = arena.alloc([128, 4, 128], BF16, "att%d" % d)
                    w_.kz = arena.alloc([128, 4, 128], BF16, "kz%d" % d)
                    MSET("pool", w_.kz.a, 0.0, [w_.kz.r])
                    w_.kg = arena.alloc([128, 4, 64], BF16, "kg%d" % d)
                    w_.ktl = arena.alloc([128, 4, 64], BF16, "ktl%d" % d)
                    w_.negWt = arena.alloc([128, 2, 128], BF16, "negWt%d" % d)
                    w_.vnew = arena.alloc([128, 4, 64], BF16, "vnew%d" % d)
                    w_.to = arena.alloc([128, 256], F32, "to%d" % d)
                    w_.sm = arena.alloc([128, 32], F32, "sm%d" % d)
                    w_.S32 = arena.alloc([128, 2, 128], F32, "S32_%d" % d)
                    w_.Sbd = arena.alloc([128, 2, 128], BF16, "Sbd%d" % d)
                    MSET("pool", w_.S32.a, 0.0, [w_.S32.r])
                    MSET("pool", w_.Sbd.a, 0.0, [w_.Sbd.r])
                    wss.append(w_)

                orderF = [16, 17] + list(range(16))
                orderB = [17, 16] + list(range(15, -1, -1))

                def gdn_pre(c, d):
                    w_ = wss[d]
                    cs = slice(c * 128, (c + 1) * 128)
                    tri = maskF if d == 0 else maskB
                    g4 = g_tok.a[:, c, d * 4:(d + 1) * 4]
                    nb4 = nbeta.a[:, c, d * 4:(d + 1) * 4]
                    sm = w_.sm
                    gcb = nb()
                    MM(gcb.a[:, 0:4], tri.a, g4, True, True, [tri.r, g_tok.r], [gcb.r])
                    MM(gcb.a[:, 4:8], ones_f.a, g4, True, True, [ones_f.r, g_tok.r], [gcb.r])
                    CP("dve", sm.a[:, 0:8], gcb.a[:, 0:8], [gcb.r], [sm.r])
                    yield
                    TS("dve", sm.a[:, 8:12], sm.a[:, 0:4], -1.0, ALU.mult, [sm.r], [sm.r])
                    TT("dve", sm.a[:, 16:20], sm.a[:, 4:8], sm.a[:, 0:4], ALU.subtract, [sm.r], [sm.r])
                    ACT(sm.a[:, 12:16], sm.a[:, 0:4], AF.Exp, [sm.r], [sm.r])
                    ACT(sm.a[:, 16:20], sm.a[:, 16:20], AF.Exp, [sm.r], [sm.r])
                    ACT(sm.a[:, 20:24], sm.a[:, 4:8], AF.Exp, [sm.r], [sm.r])
                    for t in range(2):
                        CP("dve", sm.a[0:64, 24 + t:25 + t], sm.a[0:64, 20 + 2 * t:21 + 2 * t], [sm.r], [sm.r])
                        CP("dve", sm.a[64:128, 24 + t:25 + t], sm.a[64:128, 21 + 2 * t:22 + 2 * t], [sm.r], [sm.r])
                    TT("pool", w_.gTri.a, bc(tri.a, [128, 4, 128], 1), bc(g4, [128, 4, 128], 2), ALU.mult, [tri.r, g_tok.r], [w_.gTri.r])
                    yield
                    GB = nb()
                    MM(GB.a, ones_f.a, w_.gTri.a.rearrange("p h i -> p (h i)"), True, False, [ones_f.r, w_.gTri.r], [GB.r])
                    MM(GB.a, ident_f.a, maskneg.a[:, d].rearrange("p h i -> p (h i)"), False, True, [ident_f.r, maskneg.r], [GB.r])
                    yield
                    for h in range(4):
                        ACT(w_.DT.a[:, h, :], GB.a[:, h * 128:(h + 1) * 128], AF.Exp, [GB.r, sm.r], [w_.DT.r], bias=sm.a[:, 8 + h:9 + h])
                    for t in range(2):
                        CP("pool", w_.kz.a[0:64, 2 * t, :], kT.a[0:64, t, cs], [kT.r], [w_.kz.r])
                        CP("pool", w_.kz.a[64:128, 2 * t + 1, :], kT.a[64:128, t, cs], [kT.r], [w_.kz.r])
                    yield
                    KK = nb()
                    QK = nb()
                    for h in range(4):
                        MM(KK.a[:, h * 128:(h + 1) * 128], w_.kz.a[:, h, :], kT.a[:, h // 2, cs], True, True, [w_.kz.r, kT.r], [KK.r])
                    for h in range(4):
                        MM(QK.a[:, h * 128:(h + 1) * 128], w_.kz.a[:, h, :], qT.a[:, h // 2, cs], True, True, [w_.kz.r, qT.r], [QK.r])
                    TT("dve", w_.t1.a, KK.a.rearrange("p (h i) -> p h i", h=4), w_.DT.a, ALU.mult, [KK.r, w_.DT.r], [w_.t1.r])
                    TT("dve", w_.att.a, QK.a.rearrange("p (h i) -> p h i", h=4), w_.DT.a, ALU.mult, [QK.r, w_.DT.r], [w_.att.r])
                    TT("pool", w_.NB.a, bc(offdiag.a, [128, 4, 128], 1), bc(nb4, [128, 4, 128], 2), ALU.mult, [offdiag.r, nbeta.r], [w_.NB.r])
                    M, N, PT = w_.M, w_.N, w_.PT
                    TT("pool", M[0].a, w_.t1.a, w_.NB.a, ALU.mult, [w_.t1.r, w_.NB.r], [M[0].r])
                    yield
                    pt = nb()
                    ptb = pt.a.bitcast(BF16)[:, 0:512]
                    for h in range(4):
                        TR(ptb[:, h * 128:(h + 1) * 128], M[0].a[:, h, :], ident_b.a, [M[0].r, ident_b.r], [pt.r])
                    CP("act", N[0].a.rearrange("p h i -> p (h i)"), ptb, [pt.r], [N[0].r])
                    yield
                    M0_, N0_ = M[0], N[0]
                    Mb, Nb, U, Tm, Y1, Y2 = w_.Mb, w_.Nb, w_.U, w_.Tm, w_.Y1, w_.Y2
                    f4 = lambda tl: tl.a.rearrange("p h i -> p (h i)")
                    i4 = bc(ident_b.a, [128, 4, 128], 1)

                    def mm4(lhs, rhs):
                        b_ = nb()
                        for h in range(4):
                            MM(b_.a[:, h * 128:(h + 1) * 128], lhs.a[:, h, :], rhs.a[:, h, :], True, True, [lhs.r, rhs.r], [b_.r])
                        return b_

                    m8 = bc(bdm[0].a, [128, 4, 128], 1)
                    TT("pool", Mb.a, M0_.a, m8, ALU.mult, [M0_.r, bdm[0].r], [Mb.r])
                    TT("pool", Nb.a, N0_.a, m8, ALU.mult, [N0_.r, bdm[0].r], [Nb.r])
                    TT("pool", U.a, Mb.a, i4, ALU.add, [Mb.r, ident_b.r], [U.r])
                    TT("pool", Tm.a, Nb.a, i4, ALU.add, [Nb.r, ident_b.r], [Tm.r])
                    yield
                    for lev in range(2):
                        bM = mm4(Nb, Mb)
                        bN = mm4(Mb, Nb)
                        CP("act", f4(Y1), bM.a, [bM.r], [Y1.r])
                        CP("dve", f4(Y2), bN.a, [bN.r], [Y2.r])
                        yield
                        bU = mm4(Y2, U)
                        bT = mm4(Y1, Tm)
                        TT("dve", f4(U), f4(U), bU.a, ALU.add, [U.r, bU.r], [U.r])
                        TT("dve", f4(Tm), f4(Tm), bT.a, ALU.add, [Tm.r, bT.r], [Tm.r])
                        yield
                        if lev == 0:
                            CP("pool", Mb.a, Y1.a, [Y1.r], [Mb.r])
                            CP("pool", Nb.a, Y2.a, [Y2.r], [Nb.r])
                    for li in range(4):
                        mo = bc(offm[li].a, [128, 4, 128], 1)
                        TT("pool", Mb.a, M0_.a, mo, ALU.mult, [M0_.r, offm[li].r], [Mb.r])
                        TT("pool", Nb.a, N0_.a, mo, ALU.mult, [N0_.r, offm[li].r], [Nb.r])
                        bY = mm4(Nb, U)
                        CP("act", f4(Y1), bY.a, [bY.r], [Y1.r])
                        if li < 3:
                            bY2 = mm4(Mb, Tm)
                            CP("dve", f4(Y2), bY2.a, [bY2.r], [Y2.r])
                        yield
                        bZ = mm4(Tm, Y1)
                        if li < 3:
                            bZ2 = mm4(U, Y2)
                        TT("dve", f4(U), f4(U), bZ.a, ALU.add, [U.r, bZ.r], [U.r])
                        if li < 3:
                            TT("dve", f4(Tm), f4(Tm), bZ2.a, ALU.add, [Tm.r, bZ2.r], [Tm.r])
                        yield
                    PT = [U]
                    cur = 0
                    w_.ptf = PT[cur]
                    k4 = k_tok.a[:, c, :].rearrange("p (h e) -> p h e", h=4)
                    TT("pool", w_.kg.a, k4, bc(sm.a[:, 12:16], [128, 4, 64], 2), ALU.mult, [k_tok.r, sm.r], [w_.kg.r])
                    TT("pool", w_.ktl.a, k4, bc(sm.a[:, 16:20], [128, 4, 64], 2), ALU.mult, [k_tok.r, sm.r], [w_.ktl.r])
                    yield
                    WtP = nb()
                    for h in range(4):
                        t = h // 2
                        MM(WtP.a[:, h * 128:(h + 1) * 128], w_.kg.a[:, 2 * t:2 * t + 2, :].rearrange("p h e -> p (h e)"), w_.ptf.a[:, h, :], True, True, [w_.kg.r, w_.ptf.r], [WtP.r])
                    w4 = WtP.a.rearrange("p (t hh i) -> p t hh i", t=2, hh=2)
                    ACT(w_.negWt.a[0:64, :, :], w4[0:64, :, 0, :], AF.Copy, [WtP.r], [w_.negWt.r], scale=-1.0)
                    TS("dve", w_.negWt.a[64:128, :, :], w4[64:128, :, 1, :], -1.0, ALU.mult, [WtP.r], [w_.negWt.r])

                def gdn_seq(c, d):
                    w_ = wss[d]
                    cs = slice(c * 128, (c + 1) * 128)
                    sm = w_.sm
                    Vn = nb()
                    for t in range(2):
                        MM(Vn.a[:, t * 128:(t + 1) * 128], w_.negWt.a[:, t, :], w_.Sbd.a[:, t, :], True, False, [w_.negWt.r, w_.Sbd.r], [Vn.r])
                        for hh in range(2):
                            h = 2 * t + hh
                            MM(Vn.a[:, h * 64:(h + 1) * 64], w_.ptf.a[:, h, :], v_tok.a[:, c, h * 64:(h + 1) * 64], False, hh == 1, [w_.ptf.r, v_tok.r], [Vn.r])
                    TT("dve", w_.vnew.a, Vn.a[:, 0:256].rearrange("p (h e) -> p h e", h=4), bc(beta.a[:, c, d * 4:(d + 1) * 4], [128, 4, 64], 2), ALU.mult, [Vn.r, beta.r], [w_.vnew.r])
                    yield
                    Oi = nb()
                    for t in range(2):
                        MM(Oi.a[:, t * 128:(t + 1) * 128], qT.a[:, t, cs], w_.Sbd.a[:, t, :], True, True, [qT.r, w_.Sbd.r], [Oi.r])
                    Oa = nb()
                    for h in range(4):
                        MM(Oa.a[:, h * 64:(h + 1) * 64], w_.att.a[:, h, :], w_.vnew.a[:, h, :], True, True, [w_.att.r, w_.vnew.r], [Oa.r])
                    TT("dve", w_.to.a.rearrange("p (h e) -> p h e", h=4), Oi.a[:, 0:256].rearrange("p (h e) -> p h e", h=4), bc(sm.a[:, 12:16], [128, 4, 64], 2), ALU.mult, [Oi.r, sm.r], [w_.to.r])
                    first = (orderF.index(c) <= orderB.index(c)) == (d == 0) and orderF.index(c) != orderB.index(c)
                    import os
                    if first or len(os.environ.get("GDN_DIRS", "01")) == 1:
                        TT("dve", o_acc.a[:, c, :], w_.to.a, Oa.a[:, 0:256], ALU.add, [w_.to.r, Oa.r], [o_acc.r])
                    else:
                        TT("dve", w_.to.a, w_.to.a, Oa.a[:, 0:256], ALU.add, [w_.to.r, Oa.r], [w_.to.r])
                        TT("pool", o_acc.a[:, c, :], o_acc.a[:, c, :], w_.to.a, ALU.add, [w_.to.r, o_acc.r], [o_acc.r])
                    yield
                    for t in range(2):
                        sp_ = nb()
                        MM(sp_.a[:, 0:128], w_.ktl.a[:, 2 * t:2 * t + 2, :].rearrange("p h e -> p (h e)"), w_.vnew.a[:, 2 * t:2 * t + 2, :].rearrange("p h e -> p (h e)"), True, True, [w_.ktl.r, w_.vnew.r], [sp_.r])
                        STT("dve", w_.S32.a[:, t, :], w_.S32.a[:, t, :], sm.a[:, 24 + t:25 + t], sp_.a[:, 0:128], ALU.mult, ALU.add, [w_.S32.r, sm.r, sp_.r], [w_.S32.r])
                        TT("pool", w_.Sbd.a[:, t, :], w_.S32.a[:, t, :], blockmask.a, ALU.mult, [w_.S32.r, blockmask.r], [w_.Sbd.r])

                import os
                gdirs = os.environ.get("GDN_DIRS", "01")
                def run_il(gens):
                    gens = list(gens)
                    while gens:
                        for g_ in list(gens):
                            try:
                                next(g_)
                            except StopIteration:
                                gens.remove(g_)

                for s_i in range(NCH):
                    run_il([gdn_pre(orderF[s_i], 0), gdn_pre(orderB[s_i], 1)])
                    run_il([gdn_seq(orderF[s_i], 0), gdn_seq(orderB[s_i], 1)])
                ycT = qT
                sqi = [arena.alloc([128, 4, 64], F32, "sqi%d" % i) for i in range(2)]
                dti = [arena.alloc([128, 4, 64], F32, "dti%d" % i) for i in range(2)]
                zsi = [arena.alloc([128, 256], F32, "zsi%d" % i) for i in range(2)]
                yti = [arena.alloc([128, 256], BF16, "yti%d" % i) for i in range(2)]
                stt = [arena.alloc([128, 8], F32, "stt%d" % i) for i in range(2)]
                for c in range(NCH):
                    zb = inproj_tm(wz, 0, 256, c)
                    zs, dt_, sq_, yt_, s_ = zsi[c % 2], dti[c % 2], sqi[c % 2], yti[c % 2], stt[c % 2]
                    ACT(zs.a, zb.a[:, 0:256], AF.Silu, [zb.r], [zs.r])
                    o4 = o_acc.a[:, c, :].rearrange("p (h e) -> p h e", h=4)
                    TT("pool", sq_.a, o4, o4, ALU.mult, [o_acc.r], [sq_.r])
                    P.op("dve", lambda e, o=s_.a[:, 0:4], i=sq_.a: e.tensor_reduce(out=o, in_=i, axis=AX.X, op=ALU.add), [sq_.r], [s_.r])
                    ACT(s_.a[:, 0:4], s_.a[:, 0:4], AF.Sqrt, [s_.r, cst.r], [s_.r], bias=cst.a[:, 0:1], scale=1.0 / 64)
                    P.op("dve", lambda e, o=s_.a[:, 0:4]: e.reciprocal(out=o, in_=o), [s_.r], [s_.r])
                    TT("dve", dt_.a, o4, bc(s_.a[:, 0:4], [128, 4, 64], 2), ALU.mult, [o_acc.r, s_.r], [dt_.r])
                    TT("pool", dt_.a, dt_.a, bc(rowt.a[:, 16:80], [128, 4, 64], 1), ALU.mult, [dt_.r, rowt.r], [dt_.r])
                    TT("pool", yt_.a, dt_.a.rearrange("p h e -> p (h e)"), zs.a, ALU.mult, [dt_.r, zs.r], [yt_.r])
                    pt = nb()
                    ptb = pt.a.bitcast(BF16)[:, 0:256]
                    for t in range(2):
                        TR(ptb[:, t * 128:(t + 1) * 128], yt_.a[:, t * 128:(t + 1) * 128], ident_b.a, [yt_.r, ident_b.r], [pt.r])
                    CP("act", ycT.a[:, :, c * 128:(c + 1) * 128], ptb.rearrange("p (t i) -> p t i", t=2), [pt.r], [ycT.r])
                for t in range(2):
                    DMA("sp", ycat_d[2 + t], ycT.a[:, t, :], [ycT.r], [])
                arena.reset(m_mix2)
                P.barrier()

            if "C" in mixers:
                load_w(WC, 1280, wbuf)
                wz = arena.alloc([128, 8, 256], BF16, "wz")
                load_w(WCZ, 256, wz)
                lg = arena.alloc([128, 8], F32, "lg")
                ACT(lg.a, rowt.a[:, 80:88], AF.Sigmoid, [rowt.r], [lg.r])
                ACT(lg.a, lg.a, AF.Ln, [lg.r], [lg.r])
                nlg = arena.alloc([128, 8], F32, "nlg")
                TS("dve", nlg.a, lg.a, -1.0, ALU.mult, [lg.r], [nlg.r])
                DT = arena.alloc([128, 2, 4, 128], F32, "DT")
                for h in range(4):
                    ACT(DT.a[:, 0, h, :], iota_ij.a, AF.Exp, [iota_ij.r, lg.r], [DT.r], scale=lg.a[:, h:h + 1])
                    ACT(DT.a[:, 1, h, :], iota_ij.a, AF.Exp, [iota_ij.r, nlg.r], [DT.r], scale=nlg.a[:, 4 + h:5 + h])
                TT("pool", DT.a[:, 0], DT.a[:, 0], bc(maskF.a, [128, 4, 128], 1), ALU.mult, [DT.r, maskF.r], [DT.r])
                TT("pool", DT.a[:, 1], DT.a[:, 1], bc(maskB.a, [128, 4, 128], 1), ALU.mult, [DT.r, maskB.r], [DT.r])
                pc = arena.alloc([128, 4], F32, "pc")
                TS("dve", pc.a[:, 0:1], pidx.a, 1.0, ALU.add, [pidx.r], [pc.r])
                TS("dve", pc.a[:, 1:2], pidx.a, -1.0, ALU.mult, [pidx.r], [pc.r], s2=128.0, op1=ALU.add)
                TS("dve", pc.a[:, 2:3], pidx.a, -1.0, ALU.mult, [pidx.r], [pc.r], s2=127.0, op1=ALU.add)
                CP("dve", pc.a[:, 3:4], pidx.a, [pidx.r], [pc.r])
                qdec = arena.alloc([128, 2, 4], F32, "qdec")
                kdec = arena.alloc([128, 2, 4], F32, "kdec")
                ACT(qdec.a[:, 0, :], lg.a[:, 0:4], AF.Exp, [lg.r, pc.r], [qdec.r], scale=pc.a[:, 0:1])
                ACT(qdec.a[:, 1, :], lg.a[:, 4:8], AF.Exp, [lg.r, pc.r], [qdec.r], scale=pc.a[:, 1:2])
                ACT(kdec.a[:, 0, :], lg.a[:, 0:4], AF.Exp, [lg.r, pc.r], [kdec.r], scale=pc.a[:, 2:3])
                ACT(kdec.a[:, 1, :], lg.a[:, 4:8], AF.Exp, [lg.r, pc.r], [kdec.r], scale=pc.a[:, 3:4])
                lgc = arena.alloc([128, 8], F32, "lgc")
                ACT(lgc.a, lg.a, AF.Exp, [lg.r], [lgc.r], scale=128.0)
                cdcol = arena.alloc([128, 4], F32, "cdcol")
                for d in range(2):
                    for t in range(2):
                        CP("dve", cdcol.a[0:64, d * 2 + t:d * 2 + t + 1], lgc.a[0:64, d * 4 + 2 * t:d * 4 + 2 * t + 1], [lgc.r], [cdcol.r])
                        CP("dve", cdcol.a[64:128, d * 2 + t:d * 2 + t + 1], lgc.a[64:128, d * 4 + 2 * t + 1:d * 4 + 2 * t + 2], [lgc.r], [cdcol.r])
                qT = arena.alloc([128, 2, NT], BF16, "qT")
                kT = arena.alloc([128, 2, NT], BF16, "kT")
                rt = [arena.alloc([128, 512], F32, "rt%d" % i) for i in range(4)]
                cnt = 0
                for which, dst, sc in ((0, qT, 1.0), (1, kT, 0.125)):
                    for t in range(2):
                        for (t0, n) in BLKS:
                            b1 = inproj_fm(wbuf, which * 512 + t * 128, t0, n)
                            if t0 < T:
                                b2 = inproj_fm(wbuf, which * 512 + 256 + t * 128, t0, n)
                                r1, r2 = rt[(cnt * 2) % 4], rt[(cnt * 2 + 1) % 4]
                                cnt += 1
                                STT("dve", r1.a[:, 0:n], b1.a[:, 0:n], sc, rope.a[:, 0, t0:t0 + n], ALU.mult, ALU.mult, [b1.r, rope.r], [r1.r])
                                STT("dve", r2.a[:, 0:n], b2.a[:, 0:n], sc, rope.a[:, 1, t0:t0 + n], ALU.mult, ALU.mult, [b2.r, rope.r], [r2.r])
                                TT("pool", dst.a[:, t, t0:t0 + n], r1.a[:, 0:n], r2.a[:, 0:n], ALU.add, [r1.r, r2.r], [dst.r])
                            else:
                                ACT(dst.a[:, t, t0:t0 + n], b1.a[:, 0:n], AF.Copy, [b1.r], [dst.r], scale=sc)
                v_tok = arena.alloc([128, NCH, 256], BF16, "v_tok")
                kdt = [arena.alloc([128, NCH, 4, 64], BF16, "kdt%d" % i) for i in range(2)]
                for c in range(NCH):
                    b = inproj_tm(wbuf, 1024, 256, c)
                    CP("act" if c % 2 else "dve", v_tok.a[:, c, :], b.a[:, 0:256], [b.r], [v_tok.r])
                    pt = nb()
                    ptb = pt.a.bitcast(BF16)[:, 0:256]
                    for t in range(2):
                        TR(ptb[:, t * 128:(t + 1) * 128], kT.a[:, t, c * 128:(c + 1) * 128], ident_b.a, [kT.r, ident_b.r], [pt.r])
                    for d in range(2):
                        TT("dve", kdt[d].a[:, c], ptb.rearrange("p (h e) -> p h e", h=4), bc(kdec.a[:, d, :], [128, 4, 64], 2), ALU.mult, [pt.r, kdec.r], [kdt[d].r])
                o_acc = arena.alloc([128, NCH, 256], F32, "o_acc")
                kz = [arena.alloc([128, 4, 128], BF16, "kz%d" % i) for i in range(2)]
                att = [arena.alloc([128, 4, 128], BF16, "att%d" % i) for i in range(2)]
                S32 = arena.alloc([128, 2, 128], F32, "S32")
                Sbd = arena.alloc([128, 2, 128], BF16, "Sbd")
                tmpo = [arena.alloc([128, 256], F32, "tmpo%d" % i) for i in range(2)]
                for z_ in kz:
                    MSET("pool", z_.a, 0.0, [z_.r])
                step = 0
                for d in range(2):
                    order = [16, 17] + list(range(16)) if d == 0 else [17, 16] + list(range(15, -1, -1))
                    MSET("pool", S32.a, 0.0, [S32.r])
                    MSET("pool", Sbd.a, 0.0, [Sbd.r])
                    for c in order:
                        cs = slice(c * 128, (c + 1) * 128)
                        kz_, at_, to_ = kz[step % 2], att[step % 2], tmpo[step % 2]
                        step += 1
                        for t in range(2):
                            CP("pool", kz_.a[0:64, 2 * t, :], kT.a[0:64, t, cs], [kT.r], [kz_.r])
                            CP("pool", kz_.a[64:128, 2 * t + 1, :], kT.a[64:128, t, cs], [kT.r], [kz_.r])
                        sb_ = nb()
                        for h in range(4):
                            MM(sb_.a[:, h * 128:(h + 1) * 128], kz_.a[:, h, :], qT.a[:, h // 2, cs], True, True, [kz_.r, qT.r], [sb_.r])
                        TT("dve", at_.a, sb_.a.rearrange("p (h i) -> p h i", h=4), DT.a[:, d], ALU.mult, [sb_.r, DT.r], [at_.r])
                        oi = nb()
                        for t in range(2):
                            MM(oi.a[:, t * 128:(t + 1) * 128], qT.a[:, t, cs], Sbd.a[:, t, :], True, True, [qT.r, Sbd.r], [oi.r])
                        oa = nb()
                        for h in range(4):
                            MM(oa.a[:, h * 64:(h + 1) * 64], at_.a[:, h, :], v_tok.a[:, c, h * 64:(h + 1) * 64], True, True, [at_.r, v_tok.r], [oa.r])
                        TT("dve", to_.a.rearrange("p (h e) -> p h e", h=4), oi.a[:, 0:256].rearrange("p (h e) -> p h e", h=4), bc(qdec.a[:, d, :], [128, 4, 64], 2), ALU.mult, [oi.r, qdec.r], [to_.r])
                        if d == 0:
                            TT("dve", o_acc.a[:, c, :], to_.a, oa.a[:, 0:256], ALU.add, [to_.r, oa.r], [o_acc.r])
                        else:
                            TT("dve", to_.a, to_.a, oa.a[:, 0:256], ALU.add, [to_.r, oa.r], [to_.r])
                            TT("pool", o_acc.a[:, c, :], o_acc.a[:, c, :], to_.a, ALU.add, [to_.r, o_acc.r], [o_acc.r])
                        for t in range(2):
                            sp_ = nb()
                            MM(sp_.a[:, 0:128], kdt[d].a[:, c, 2 * t:2 * t + 2, :].rearrange("p h e -> p (h e)"), v_tok.a[:, c, t * 128:(t + 1) * 128], True, True, [kdt[d].r, v_tok.r], [sp_.r])
                            STT("dve", S32.a[:, t, :], S32.a[:, t, :], cdcol.a[:, d * 2 + t:d * 2 + t + 1], sp_.a[:, 0:128], ALU.mult, ALU.add, [S32.r, cdcol.r, sp_.r], [S32.r])
                            TT("pool", Sbd.a[:, t, :], S32.a[:, t, :], blockmask.a, ALU.mult, [S32.r, blockmask.r], [Sbd.r])
                ycT = qT
                st4 = arena.alloc([128, 8], F32, "st4")
                dti = [arena.alloc([128, 4, 64], F32, "dti%d" % i) for i in range(2)]
                sqi = [arena.alloc([128, 4, 64], F32, "sqi%d" % i) for i in range(2)]
                zsi = [arena.alloc([128, 256], F32, "zsi%d" % i) for i in range(2)]
                yti = [arena.alloc([128, 256], BF16, "yti%d" % i) for i in range(2)]
                stt = [arena.alloc([128, 8], F32, "stt%d" % i) for i in range(2)]
                for c in range(NCH):
                    zb = inproj_tm(wz, 0, 256, c)
                    zs, dt_, sq_, yt_, s_ = zsi[c % 2], dti[c % 2], sqi[c % 2], yti[c % 2], stt[c % 2]
                    ACT(zs.a, zb.a[:, 0:256], AF.Silu, [zb.r], [zs.r])
                    o4 = o_acc.a[:, c, :].rearrange("p (h e) -> p h e", h=4)
                    P.op("dve", lambda e, o=s_.a[:, 0:4], i=o4: e.tensor_reduce(out=o, in_=i, axis=AX.X, op=ALU.add), [o_acc.r], [s_.r])
                    TS("dve", s_.a[:, 0:4], s_.a[:, 0:4], -1.0 / 64, ALU.mult, [s_.r], [s_.r])
                    TT("dve", dt_.a, o4, bc(s_.a[:, 0:4], [128, 4, 64], 2), ALU.add, [o_acc.r, s_.r], [dt_.r])
                    TT("pool", sq_.a, dt_.a, dt_.a, ALU.mult, [dt_.r], [sq_.r])
                    P.op("dve", lambda e, o=s_.a[:, 4:8], i=sq_.a: e.tensor_reduce(out=o, in_=i, axis=AX.X, op=ALU.add), [sq_.r], [s_.r])
                    ACT(s_.a[:, 4:8], s_.a[:, 4:8], AF.Sqrt, [s_.r, cst.r], [s_.r], bias=cst.a[:, 0:1], scale=1.0 / 64)
                    P.op("dve", lambda e, o=s_.a[:, 4:8]: e.reciprocal(out=o, in_=o), [s_.r], [s_.r])
                    TT("dve", dt_.a, dt_.a, bc(s_.a[:, 4:8], [128, 4, 64], 2), ALU.mult, [dt_.r, s_.r], [dt_.r])
                    TT("pool", yt_.a, dt_.a.rearrange("p h e -> p (h e)"), zs.a, ALU.mult, [dt_.r, zs.r], [yt_.r])
                    pt = nb()
                    ptb = pt.a.bitcast(BF16)[:, 0:256]
                    for t in range(2):
                        TR(ptb[:, t * 128:(t + 1) * 128], yt_.a[:, t * 128:(t + 1) * 128], ident_b.a, [yt_.r, ident_b.r], [pt.r])
                    CP("act", ycT.a[:, :, c * 128:(c + 1) * 128], ptb.rearrange("p (t i) -> p t i", t=2), [pt.r], [ycT.r])
                for t in range(2):
                    DMA("sp", ycat_d[4 + t], ycT.a[:, t, :], [ycT.r], [])
                arena.reset(m_mix2)
                P.barrier()

            if "D" in mixers:
                load_w(WD, 1152, wbuf)
                wz = arena.alloc([128, 8, 256], BF16, "wz")
                load_w(WDZ, 256, wz)
                esink = arena.alloc([128, 4], F32, "esink")
                ACT(esink.a, rowt.a[:, 88:92], AF.Exp, [rowt.r], [esink.r])
                qT = arena.alloc([128, 2, NT], BF16, "qT")
                kdT = arena.alloc([128, 2, NT], BF16, "kdT")
                rt = [arena.alloc([128, 512], F32, "rt%d" % i) for i in range(4)]
                cnt = 0
                for which, dst, sc in ((0, qT, 0.125), (1, kdT, 1.0)):
                    for t in range(2):
                        for (t0, n) in BLKS:
                            b1 = inproj_fm(wbuf, which * 512 + t * 128, t0, n)
                            if t0 < T:
                                b2 = inproj_fm(wbuf, which * 512 + 256 + t * 128, t0, n)
                                r1, r2 = rt[(cnt * 2) % 4], rt[(cnt * 2 + 1) % 4]
                                cnt += 1
                                STT("dve", r1.a[:, 0:n], b1.a[:, 0:n], sc, rope.a[:, 2, t0:t0 + n], ALU.mult, ALU.mult, [b1.r, rope.r], [r1.r])
                                STT("dve", r2.a[:, 0:n], b2.a[:, 0:n], sc, rope.a[:, 3, t0:t0 + n], ALU.mult, ALU.mult, [b2.r, rope.r], [r2.r])
                                TT("pool", dst.a[:, t, t0:t0 + n], r1.a[:, 0:n], r2.a[:, 0:n], ALU.add, [r1.r, r2.r], [dst.r])
                            else:
                                ACT(dst.a[:, t, t0:t0 + n], b1.a[:, 0:n], AF.Copy, [b1.r], [dst.r], scale=sc)
                v_aug = arena.alloc([128, NCH, 2, 65], BF16, "v_aug")
                MSET("pool", v_aug.a, 1.0, [v_aug.r])
                for c in range(NCH):
                    b = inproj_tm(wbuf, 1024, 128, c)
                    CP("act" if c % 2 else "dve", v_aug.a[:, c, :, 0:64], b.a[:, 0:128].rearrange("p (h e) -> p h e", h=2), [b.r], [v_aug.r])
                ycT = arena.alloc([128, 2, NT], BF16, "ycT")
                qbd = [arena.alloc([128, 2, 2, 128], BF16, "qbd%d" % i) for i in range(2)]
                for q_ in qbd:
                    MSET("pool", q_.a, 0.0, [q_.r])
                PT = [arena.alloc([128, 4, 128], BF16, "PT%d" % i) for i in range(10)]
                den = [arena.alloc([128, 8], F32, "den%d" % i) for i in range(2)]
                yf = [arena.alloc([128, 4, 64], F32, "yf%d" % i) for i in range(2)]
                zsi = [arena.alloc([128, 256], F32, "zsi%d" % i) for i in range(2)]
                yti = [arena.alloc([128, 256], BF16, "yti%d" % i) for i in range(2)]
                for qi, n in enumerate(list(range(NCH))):
                    cs = slice(n * 128, (n + 1) * 128)
                    qb_ = qbd[qi % 2]
                    for kvh in range(2):
                        CP("pool", qb_.a[0:64, kvh, 0, :], qT.a[0:64, kvh, cs], [qT.r], [qb_.r])
                        CP("pool", qb_.a[64:128, kvh, 1, :], qT.a[64:128, kvh, cs], [qT.r], [qb_.r])
                    if n < 16:
                        keys = [m for m in (n - 1, n, n + 1) if 0 <= m <= 15] + [16, 17]
                    else:
                        keys = [16, 17]
                    pts = []
                    for mi, m in enumerate(keys):
                        sb_ = nb()
                        for kvh in range(2):
                            MM(sb_.a[:, kvh * 256:(kvh + 1) * 256], kdT.a[:, kvh, m * 128:(m + 1) * 128], qb_.a[:, kvh].rearrange("p g i -> p (g i)"), True, True, [kdT.r, qb_.r], [sb_.r])
                        pt_ = PT[(qi % 2) * 5 + mi]
                        pts.append(pt_)
                        ACT(pt_.a.rearrange("p h i -> p (h i)"), sb_.a, AF.Exp, [sb_.r], [pt_.r])
                        if n < 16 and m == n - 1:
                            TT("pool", pt_.a, pt_.a, bc(maskB.a, [128, 4, 128], 1), ALU.mult, [pt_.r, maskB.r], [pt_.r])
                        elif n < 15 and m == n + 1:
                            TT("pool", pt_.a, pt_.a, bc(maskF.a, [128, 4, 128], 1), ALU.mult, [pt_.r, maskF.r], [pt_.r])
                    ob = nb()
                    for hq in range(4):
                        for mi, m in enumerate(keys):
                            MM(ob.a[:, hq * 65:(hq + 1) * 65], pts[mi].a[:, hq, :], v_aug.a[:, m, hq // 2, :], mi == 0, mi == len(keys) - 1, [pts[mi].r, v_aug.r], [ob.r])
                    o4 = ob.a[:, 0:260].rearrange("p (h e) -> p h e", h=4)
                    dn, yf_, zs, yt_ = den[qi % 2], yf[qi % 2], zsi[qi % 2], yti[qi % 2]
                    TT("dve", dn.a[:, 0:4], o4[:, :, 64], esink.a, ALU.add, [ob.r, esink.r], [dn.r])
                    P.op("dve", lambda e, o=dn.a[:, 0:4]: e.reciprocal(out=o, in_=o), [dn.r], [dn.r])
                    TT("dve", yf_.a, o4[:, :, 0:64], bc(dn.a[:, 0:4], [128, 4, 64], 2), ALU.mult, [ob.r, dn.r], [yf_.r])
                    zb = inproj_tm(wz, 0, 256, n)
                    ACT(zs.a, zb.a[:, 0:256], AF.Silu, [zb.r], [zs.r])
                    TT("pool", yt_.a, yf_.a.rearrange("p h e -> p (h e)"), zs.a, ALU.mult, [yf_.r, zs.r], [yt_.r])
                    pt = nb()
                    ptb = pt.a.bitcast(BF16)[:, 0:256]
                    for t in range(2):
                        TR(ptb[:, t * 128:(t + 1) * 128], yt_.a[:, t * 128:(t + 1) * 128], ident_b.a, [yt_.r, ident_b.r], [pt.r])
                    CP("act", ycT.a[:, :, cs], ptb.rearrange("p (t i) -> p t i", t=2), [pt.r], [ycT.r])
                for t in range(2):
                    DMA("sp", ycat_d[6 + t], ycT.a[:, t, :], [ycT.r], [])
                arena.reset(m_mix2)
                P.barrier()

            arena.reset(m_mix)
            P.barrier()
            if debug and l == n_layers - 1:
                ydb = arena.alloc([128, NT], BF16, "ydb")
                for i in range(8):
                    DMA("sp", ydb.a, ycat_d[i], [], [ydb.r])
                    DMA("sp", dbg_y[i], ydb.a, [ydb.r], [])
                arena.reset(m_mix)
                P.barrier()
            m0 = arena.mark()
            og = P.group(dedicated=True)
            if "D" not in phases:
                DMA("sp", out_d[0:128, :], grow[0].a, [grow[0].r], [])
                continue
            wout = arena.alloc([128, 8, D], BF16, "wout")
            wst = [arena.alloc([128, D], F32, "wst%d" % i) for i in range(2)]
            for k in range(8):
                load_cast(wout.a[:, k, :], wout.r, wout_d[l, k * 128:(k + 1) * 128, :], D, wst[k % 2], wres[l]["out"])
            ylb = [arena.alloc([128, 8, 128], BF16, "yl%d" % i) for i in range(2)]
            xb = [arena.alloc([128, D], F32, "xb%d" % i) for i in range(2)]
            sqfs = [arena.alloc([128, 512], F32, "sqf%d" % i) for i in range(2)]
            t1 = [arena.alloc([128, D], F32, "t1_%d" % i) for i in range(2)]
            xo = [arena.alloc([128, D], F32, "xo%d" % i) for i in range(2)]
            for c in range(NCH):
                if last and c >= 16:
                    continue
                if "1" in phases:
                    continue
                yl = ylb[c % 2]
                if "5" in phases:
                    DMA("sp", yl.a, ycat_d[:, :, c * 128:(c + 1) * 128].rearrange("k p t -> p k t"), [], [yl.r])
                else:
                    for k_ in range(8):
                        DMA("sp", yl.a[:, k_, :], ycat_d[k_, :, c * 128:(c + 1) * 128], [], [yl.r])
                xt = xb[c % 2]
                if from_x:
                    src = x_d[c * 128:(c + 1) * 128, :] if c < 16 else ctx_d[(c - 16) * 128:(c - 15) * 128, :]
                else:
                    src = xs_d[c * 128:(c + 1) * 128, :]
                DMA("sp", xt.a, src, [], [xt.r])
                w = 0 if c < 16 else 1
                tt_, xo_ = t1[c % 2], xo[c % 2]
                hb = []
                for half in range(2):
                    b = nb()
                    hb.append(b)
                    for k in range(8):
                        MM(b.a, yl.a[:, k, :], wout.a[:, k, half * 512:(half + 1) * 512], k == 0, k == 7, [yl.r, wout.r], [b.r])
                    sqf = sqfs[half]
                    ACT(sqf.a, b.a, AF.Square, [b.r], [sqf.r])
                    P.op("dve", lambda e, o=ss.a[:, 2 * c + half:2 * c + half + 1], i=sqf.a: e.tensor_reduce(out=o, in_=i, axis=AX.X, op=ALU.add), [sqf.r], [ss.r])
                    TT("dve", tt_.a[:, half * 512:(half + 1) * 512], b.a, grow[w].a[:, half * 512:(half + 1) * 512], ALU.mult, [b.r, grow[w].r], [tt_.r])
                TT("dve", rstd.a[:, c:c + 1], ss.a[:, 2 * c:2 * c + 1], ss.a[:, 2 * c + 1:2 * c + 2], ALU.add, [ss.r], [rstd.r])
                ACT(rstd.a[:, c:c + 1], rstd.a[:, c:c + 1], AF.Sqrt, [rstd.r, cst.r], [rstd.r], bias=cst.a[:, 0:1], scale=1.0 / D)
                P.op("dve", lambda e, o=rstd.a[:, c:c + 1]: e.reciprocal(out=o, in_=o), [rstd.r], [rstd.r])
                STT("dve", xo_.a, tt_.a, rstd.a[:, c:c + 1], xt.a, ALU.mult, ALU.add, [tt_.r, rstd.r, xt.r], [xo_.r])
                if "2" in phases:
                    pass
                elif last:
                    DMA("sp", out_d[c * 128:(c + 1) * 128, :], xo_.a, [xo_.r], [])
                else:
                    DMA("sp", xs_d[c * 128:(c + 1) * 128, :], xo_.a, [xo_.r], [])
                if debug and l == n_layers - 1:
                    DMA("sp", dbg_xs[c * 128:(c + 1) * 128, :], xo_.a, [xo_.r], [])
            arena.reset(m0)
            P.barrier()
        P.barrier()
        P.replay()
        nc._arena_log = arena.log
        print("arena peak words", arena.peak, "ops", {k: len(v) for k, v in P.ops.items()})
    return nc


MIXERS_EXTRA = []


def kernel(**inputs):
    maps = _prep_inputs(inputs, sharded=False)
    nc = build_nc(sharded=False)
    res = run_bass_kernel_spmd(nc, maps, core_ids=list(range(8)))
    return np.stack([np.asarray(r["out"], dtype=np.float32) for r in res.results], axis=0)
```

```python
import numpy as np
from contextlib import ExitStack
import ml_dtypes
import concourse.bass as bass
import concourse.mybir as mybir
from concourse.bass_utils import run_bass_kernel_spmd

F32 = mybir.dt.float32
BF16 = mybir.dt.bfloat16
AF = mybir.ActivationFunctionType
ALU = mybir.AluOpType
AX = mybir.AxisListType

D = 1024
T = 2048
LC = 256
NT = T + LC
NCH = NT // 128
DEPTH = 4
EPS = 1e-6
ENGS = ("pe", "act", "dve", "pool", "sp")


class Res:
    __slots__ = ("name", "w", "r")

    def __init__(self, name=""):
        self.name = name
        self.w = None
        self.r = {}


class DmaGroup:
    def __init__(self, sem, idx, base):
        self.sem = sem
        self.n = 0
        self.key = ("g", idx)
        self.base = base

    @property
    def total(self):
        return self.base + 16 * self.n


class Prog:
    def __init__(self, nc, stack, nchan=24):
        self.nc = nc
        self.stack = stack
        self.ops = {e: [] for e in ENGS}
        self.count = {e: 0 for e in ENGS}
        self.waited = {e: {} for e in ENGS}
        self.sem = {e: stack.enter_context(nc.semaphore("sem_" + e)) for e in ENGS}
        self.chan_sem = [stack.enter_context(nc.semaphore("semc%d" % i)) for i in range(nchan)]
        self.chan_last = [None] * nchan
        self.chan_rr = 0
        self.groups = []
        self.gmap = {}
        self.open_groups = []

    def group(self, dedicated=False):
        if dedicated:
            sem = self.stack.enter_context(self.nc.semaphore("semd%d" % len(self.groups)))
            g = DmaGroup(sem, len(self.groups), 0)
            g.prev = None
            g.issued = set()
            self.groups.append(g)
            self.gmap[g.key] = g
            self.open_groups.append(g)
            return g
        ch = self.chan_rr
        self.chan_rr = (self.chan_rr + 1) % len(self.chan_sem)
        prev = self.chan_last[ch]
        base = prev.total if prev is not None else 0
        g = DmaGroup(self.chan_sem[ch], len(self.groups), base)
        g.prev = prev
        g.issued = set()
        self.chan_last[ch] = g
        self.groups.append(g)
        self.gmap[g.key] = g
        self.open_groups.append(g)
        return g

    def _deps(self, eng, reads, writes):
        deps = {}

        def add(k, v, same_ok):
            if k == eng and not same_ok and eng == "pe":
                return
            if k in deps:
                if isinstance(v, int):
                    deps[k] = max(deps[k], v)
            else:
                deps[k] = v

        for r in reads:
            if r.w is not None:
                add(r.w[0], r.w[1], True)
        for r in writes:
            if r.w is not None:
                add(r.w[0], r.w[1], False)
            for k, v in r.r.items():
                add(k, v, False)
        out = []
        wd = self.waited[eng]
        for k, v in deps.items():
            if isinstance(k, tuple):
                if wd.get(k):
                    continue
                wd[k] = True
                out.append((k, None))
            else:
                if wd.get(k, 0) >= v:
                    continue
                wd[k] = v
                out.append((k, v))
        return out

    def op(self, eng, fn, reads=(), writes=()):
        waits = self._deps(eng, reads, writes)
        self.count[eng] += 1
        idx = self.count[eng]
        self.ops[eng].append((fn, waits, None))
        for r in reads:
            r.r[eng] = idx
        for r in writes:
            r.w = (eng, idx)
            r.r = {}
        return idx

    def dma(self, eng, group, fn, reads=(), writes=()):
        waits = self._deps(eng, reads, writes)
        if eng not in group.issued:
            group.issued.add(eng)
            if group.prev is not None and not self.waited[eng].get(group.prev.key):
                self.waited[eng][group.prev.key] = True
                waits.append((group.prev.key, None))
        group.n += 1
        self.ops[eng].append((fn, waits, group))
        for r in reads:
            r.r[group.key] = None
        for r in writes:
            r.w = (group.key, None)
            r.r = {}

    def wait_group(self, eng, group):
        self.ops[eng].append((None, [(group.key, None)], None))

    def barrier(self):
        comp = ("pe", "act", "dve", "pool")
        for e in ENGS:
            waits = []
            for f in comp:
                if f != e and self.count[f] > self.waited[e].get(f, 0):
                    self.waited[e][f] = self.count[f]
                    waits.append((f, self.count[f]))
            for g in self.open_groups:
                if g.n > 0 and not self.waited[e].get(g.key):
                    self.waited[e][g.key] = True
                    waits.append((g.key, None))
            if waits:
                self.ops[e].append((None, waits, None))
        self.open_groups = [g for g in self.open_groups if g.n == 0]

    def replay(self):
        nc = self.nc

        def run(name, e):
            own = self.sem[name]
            for fn, waits, group in self.ops[name]:
                for k, v in waits:
                    if isinstance(k, tuple):
                        g = self.gmap[k]
                        e.wait_ge(g.sem, g.total)
                    else:
                        e.wait_ge(self.sem[k], v)
                if fn is None:
                    continue
                ins = fn(e)
                if group is not None:
                    ins.then_inc(group.sem, 16)
                else:
                    ins.then_inc(own, 1)

        with nc.Block() as block:
            @block.tensor
            def _(e):
                run("pe", e)

            @block.scalar
            def _(e):
                run("act", e)

            @block.vector
            def _(e):
                run("dve", e)

            @block.gpsimd
            def _(e):
                run("pool", e)

            @block.sync
            def _(e):
                run("sp", e)


def _sw(cols, nheads):
    c = np.asarray(cols).reshape(nheads, 2, 32)
    return c[:, ::-1, :].reshape(-1)


O_LRUX, O_LRUZ, O_GQKV, O_GZ, O_GA, O_GB = 0, 256, 512, 1280, 1536, 1544
O_RQ, O_RK, O_RV, O_RZ = 1552, 1808, 2064, 2320
O_SQ, O_SK, O_SV, O_SZ = 2576, 2832, 2960, 3088
AR = np.arange


def _colperm():
    A = np.concatenate([AR(O_LRUX, O_LRUX + 256), AR(O_LRUZ, O_LRUZ + 256)])
    B = np.concatenate([AR(O_GQKV, O_GQKV + 768), AR(O_GA, O_GA + 16)])
    Bz = AR(O_GZ, O_GZ + 256)
    rq, rk = AR(O_RQ, O_RQ + 256), AR(O_RK, O_RK + 256)
    C = np.concatenate([rq, _sw(rq, 4), rk, _sw(rk, 4), AR(O_RV, O_RV + 256)])
    Cz = AR(O_RZ, O_RZ + 256)
    sq = AR(O_SQ, O_SQ + 256)
    sk = AR(O_SK, O_SK + 128)
    kd = np.concatenate([sk[0:64], sk[0:64], sk[64:128], sk[64:128]])
    Dm = np.concatenate([sq, _sw(sq, 4), kd, _sw(kd, 4), AR(O_SV, O_SV + 128)])
    Dz = AR(O_SZ, O_SZ + 256)
    parts = [A, B, Bz, C, Cz, Dm, Dz]
    offs = np.cumsum([0] + [len(p) for p in parts])
    return np.concatenate(parts), offs


COLPERM, COLOFF = _colperm()
NCOL = int(COLOFF[-1])
WA, WB, WBZ, WC, WCZ, WD, WDZ = [int(v) for v in COLOFF[:7]]
NCP = 54
NRS = 92


def _rope_tables():
    inv64 = 10000.0 ** (-np.arange(0, 64, 2, dtype=np.float32) / 64)
    ang1 = np.arange(T, dtype=np.float32)[:, None] * inv64[None, :]
    inv32 = 10000.0 ** (-np.arange(0, 32, 2, dtype=np.float32) / 32)
    row = np.repeat(np.arange(T // 64, dtype=np.float32), 64)
    col = np.tile(np.arange(64, dtype=np.float32), T // 64)
    ang2 = np.concatenate([row[:, None] * inv32[None, :], col[:, None] * inv32[None, :]], axis=-1)
    out = np.zeros((4, 128, T), np.float32)
    for i, ang in enumerate((ang1, ang2)):
        c = np.cos(ang).T
        s = np.sin(ang).T
        ctab = np.concatenate([c, c, c, c], axis=0)
        stab = np.concatenate([-s, s, -s, s], axis=0)
        out[2 * i] = ctab
        out[2 * i + 1] = stab
    return out.astype(ml_dtypes.bfloat16)


def _prep_inputs(inp, sharded=False):
    L = DEPTH
    f = lambda a: np.ascontiguousarray(np.asarray(a, dtype=np.float32))
    w_in_p = f(inp["w_in"][:, :, COLPERM])
    colp = np.zeros((L, 128, NCP), np.float32)
    fm = lambda v: np.asarray(v).reshape(-1, 128).T
    for l in range(L):
        c = 0
        colp[l, :, c:c + 8] = fm(inp["pre_norm_g"][l]); c += 8
        for k in range(4):
            colp[l, :, c:c + 2] = fm(inp["lru_conv_w"][l, k]); c += 2
        colp[l, :, c:c + 2] = fm(inp["lru_conv_b"][l]); c += 2
        for nm in ("lru_b_r", "lru_b_i", "lru_lambda"):
            for d in range(2):
                colp[l, :, c:c + 2] = fm(inp[nm][l, d]); c += 2
        for k in range(4):
            colp[l, :, c:c + 6] = fm(inp["gdn_conv_w"][l, k]); c += 6
        assert c == NCP
    rows = np.zeros((L, NRS), np.float32)
    for l in range(L):
        rows[l, 0:8] = np.asarray(inp["gdn_a_log"][l]).reshape(-1)
        rows[l, 8:16] = np.asarray(inp["gdn_dt_bias"][l]).reshape(-1)
        rows[l, 16:80] = np.asarray(inp["gdn_norm_g"][l])
        rows[l, 80:88] = np.asarray(inp["ret_decay_logit"][l]).reshape(-1)
        rows[l, 88:92] = np.asarray(inp["swa_sink"][l])
    wg = np.ascontiguousarray(np.stack([f(inp["lru_w_r"]), f(inp["lru_w_i"])], axis=1))
    shared = {
        "w_mod": f(inp["w_mod"]), "b_mod": f(inp["b_mod"]), "post_g": f(inp["post_norm_g"]),
        "w_in": w_in_p, "w_out": f(inp["w_out"]), "colp": colp, "rows": rows, "lruw": wg,
        "rope": _rope_tables(),
        "bmodc": np.ascontiguousarray(f(inp["b_mod"]).reshape(L, 24, 128).transpose(0, 2, 1)),
    }
    maps = []
    for b in range(8):
        sv = np.zeros((128, 16), np.float32)
        sv[:, 0:8] = fm(inp["c"][b])
        sv[:, 8:16] = fm(inp["c_ctx"])
        m = dict(shared)
        m["x"] = f(inp["x"][b])
        m["ctx"] = f(inp["ctx"][b])
        m["svec"] = sv
        if sharded:
            for k in ("w_mod", "w_in", "w_out"):
                m[k] = np.ascontiguousarray(shared[k][:, b * 128:(b + 1) * 128, :])
        maps.append(m)
    return maps


class Tl:
    __slots__ = ("a", "r")

    def __init__(self, a, r):
        self.a = a
        self.r = r


def _prod(s):
    n = 1
    for v in s:
        n *= v
    return n


class Arena:
    def __init__(self, nc, st, nwords, name="arena"):
        self.t = st.enter_context(nc.sbuf_tensor(name, [128, nwords], F32))
        self.nwords = nwords
        self.off = 0
        self.peak = 0

    def alloc(self, shape, dtype, name=""):
        n = _prod(shape[1:])
        words = n if dtype == F32 else (n + 1) // 2
        words = (words + 7) // 8 * 8
        assert self.off + words <= self.nwords, ("arena overflow", name, self.off, words, self.nwords)
        ap = self.t[:, self.off:self.off + words]
        if dtype != F32:
            ap = ap.bitcast(dtype)
        ap = ap[:, 0:n]
        if len(shape) == 3:
            ap = ap.rearrange("p (a b) -> p a b", a=shape[1])
        elif len(shape) == 4:
            ap = ap.rearrange("p (a b c) -> p a b c", a=shape[1], b=shape[2])
        if not hasattr(self, "log"):
            self.log = {}
        self.log[name] = (self.off, tuple(shape), "f32" if dtype == F32 else "bf16")
        self.off += words
        self.peak = max(self.peak, self.off)
        return Tl(ap, Res(name))

    def mark(self):
        return self.off

    def reset(self, m):
        self.off = m


BLKS = [(0, 512), (512, 512), (1024, 512), (1536, 512), (2048, 256)]


def build_nc(n_layers=DEPTH, mixers="ABCD", debug=False, first_from_scratch=False, final_layer=True, phases="ABD", slim=False, sharded=False):
    nc = bass.Bass("TRN2", target_bir_lowering=False)
    DEPTH = n_layers if slim else 4
    dt_in = lambda name, shape, dt=F32: nc.dram_tensor(name, shape, dt, kind="ExternalInput").ap()
    x_d = dt_in("x", [T, D])
    ctx_d = dt_in("ctx", [LC, D])
    svec_d = dt_in("svec", [128, 16])
    KS = 128 if sharded else D
    wmod_in = dt_in("w_mod", [DEPTH, KS, 3 * D])
    bmod_d = dt_in("b_mod", [DEPTH, 3 * D])
    bmodc_d = dt_in("bmodc", [DEPTH, 128, 24])
    postg_d = dt_in("post_g", [DEPTH, D])
    win_in = dt_in("w_in", [DEPTH, KS, NCOL])
    wout_in = dt_in("w_out", [DEPTH, KS, D])
    if sharded:
        wsh = {k: nc.dram_tensor("wsh_" + k, [DEPTH, 128, n], F32, kind="Internal").ap() for k, n in (("mod", 3 * D), ("in", NCOL), ("out", D))}
        wfull = {k: nc.dram_tensor("wfull_" + k, [DEPTH, D, n], F32, kind="Internal").ap() for k, n in (("mod", 3 * D), ("in", NCOL), ("out", D))}
        wmod_d, win_d, wout_d = wfull["mod"], wfull["in"], wfull["out"]
    else:
        wmod_d, win_d, wout_d = wmod_in, win_in, wout_in
    colp_d = dt_in("colp", [DEPTH, 128, NCP])
    rows_d = dt_in("rows", [DEPTH, NRS])
    lruw_d = dt_in("lruw", [DEPTH, 2, 2, 4, 64, 64])
    rope_d = dt_in("rope", [4, 128, T], BF16)
    out_d = nc.dram_tensor("out", [T, D], F32, kind="ExternalOutput").ap()
    xs_d = nc.dram_tensor("xs", [NT, D], F32, kind="Internal").ap()
    ycat_d = nc.dram_tensor("ycat_s", [8, 128, NT], BF16, kind="Internal").ap()
    if debug:
        dbg_h = nc.dram_tensor("dbg_h", [8, 128, NT], BF16, kind="ExternalOutput").ap()
        dbg_y = nc.dram_tensor("dbg_y", [8, 128, NT], BF16, kind="ExternalOutput").ap()
        dbg_xs = nc.dram_tensor("dbg_xs", [NT, D], F32, kind="ExternalOutput").ap()

    with ExitStack() as st:
        P = Prog(nc, st)
        sbt = lambda name, shape, dt=F32: Tl(st.enter_context(nc.sbuf_tensor("sb_" + name, shape, dt))[:], Res(name))
        banks = [Tl(st.enter_context(nc.psum_tensor("bank%d" % i, [128, 512], F32))[:], Res("bank%d" % i)) for i in range(8)]
        bank_rr = [0]

        def nb():
            b = banks[bank_rr[0]]
            bank_rr[0] = (bank_rr[0] + 1) % 8
            return b

        def ACT(out, in_, func, R, W, bias=None, scale=None, accum=None):
            kw = {}
            if bias is not None:
                kw["bias"] = bias
            if scale is not None:
                kw["scale"] = scale
            if accum is not None:
                kw["accum_out"] = accum
            P.op("act", lambda e: e.activation(out=out, in_=in_, func=func, **kw), R, W)

        def TT(eng, out, in0, in1, op, R, W):
            P.op(eng, lambda e: e.tensor_tensor(out=out, in0=in0, in1=in1, op=op), R, W)

        def TS(eng, out, in0, s1, op0, R, W, s2=None, op1=None):
            if op1 is None:
                P.op(eng, lambda e: e.tensor_scalar(out=out, in0=in0, scalar1=s1, scalar2=None, op0=op0), R, W)
            else:
                P.op(eng, lambda e: e.tensor_scalar(out=out, in0=in0, scalar1=s1, scalar2=s2, op0=op0, op1=op1), R, W)

        def STT(eng, out, in0, scalar, in1, op0, op1, R, W):
            P.op(eng, lambda e: e.scalar_tensor_tensor(out=out, in0=in0, scalar=scalar, in1=in1, op0=op0, op1=op1), R, W)

        def CP(eng, out, in_, R, W):
            if eng == "act":
                P.op("act", lambda e: e.activation(out=out, in_=in_, func=AF.Copy), R, W)
            else:
                P.op(eng, lambda e: e.tensor_copy(out=out, in_=in_), R, W)

        def MSET(eng, out, val, W):
            P.op(eng, lambda e: e.memset(out, val), [], W)

        def MM(out, lhsT, rhs, start, stop, R, W):
            P.op("pe", lambda e: e.matmul(out, lhsT=lhsT, rhs=rhs, start=start, stop=stop), R, W)

        def TR(out, in_, ident, R, W):
            P.op("pe", lambda e: e.transpose(out=out, in_=in_, identity=ident), R, W)

        def DMA(eng, out, in_, R, W, g=None):
            if g is None:
                g = P.group()
            P.dma(eng, g, lambda e: e.dma_start(out=out, in_=in_), R, W)
            return g

        def SCAN(out, d0, d1, init, R, W):
            P.op("dve", lambda e: e.tensor_tensor_scan(out=out, data0=d0, data1=d1, initial=init, op0=ALU.mult, op1=ALU.add), R, W)

        def rev(ap2d):
            n = ap2d.shape[1]
            return bass.AP(ap2d.tensor, ap2d.offset + (n - 1), [list(ap2d.ap[0]), [-1, n]])

        def bc(ap, shape, axis):
            return ap.unsqueeze(axis).to_broadcast(shape)

        wres = [{k: Res("w_%s_%d" % (k, l_)) for k in ("mod", "in", "out")} for l_ in range(DEPTH)]
        if sharded:
            srcs = {"mod": wmod_in, "in": win_in, "out": wout_in}
            shres = {}
            for l_ in range(n_layers):
                for k in ("mod", "in", "out"):
                    shres[(l_, k)] = Res("sh")
                    DMA("sp", wsh[k][l_], srcs[k][l_], [], [shres[(l_, k)]])
            for l_ in range(n_layers):
                for k in ("mod", "in", "out"):
                    g_ = P.group()
                    P.dma("pool", g_, lambda e, i_=wsh[k][l_], o_=wfull[k][l_]: e.collective_compute(
                        "AllGather", op=ALU.bypass, replica_groups=[list(range(8))], ins=[i_], outs=[o_]),
                        [shres[(l_, k)]], [wres[l_][k]])

        ones_f = sbt("ones_f", [128, 128])
        ident_f = sbt("ident_f", [128, 128])
        ident_b = sbt("ident_b", [128, 128], BF16)
        cst = sbt("cst", [128, 4])
        MSET("pool", ones_f.a, 1.0, [ones_f.r])
        MSET("pool", cst.a[:, 0:1], EPS, [cst.r])
        MSET("pool", cst.a[:, 1:2], 1.0, [cst.r])
        MSET("pool", cst.a[:, 2:3], -1.0, [cst.r])
        MSET("pool", cst.a[:, 3:4], 0.0, [cst.r])

        def aff(out_t, pattern, cm, op, base=0, fill=0.0, src=None):
            src = src or ones_f
            P.op("pool", lambda e: e.affine_select(out=out_t.a, in_=src.a, pattern=pattern, compare_op=op, fill=fill, base=base, channel_multiplier=cm), [src.r], [out_t.r])

        aff(ident_f, [[-1, 128]], 1, ALU.is_equal)
        CP("dve", ident_b.a, ident_f.a, [ident_f.r], [ident_b.r])
        maskF = sbt("maskF", [128, 128])
        maskB = sbt("maskB", [128, 128])
        blockmask = sbt("blockmask", [128, 128])
        aff(maskF, [[1, 128]], -1, ALU.is_ge)
        aff(maskB, [[-1, 128]], 1, ALU.is_ge)
        MSET("pool", blockmask.a, 0.0, [blockmask.r])
        MSET("pool", blockmask.a[0:64, 0:64], 1.0, [blockmask.r])
        MSET("pool", blockmask.a[64:128, 64:128], 1.0, [blockmask.r])
        blockones_b = sbt("blockones_b", [128, 128], BF16)
        CP("dve", blockones_b.a, blockmask.a, [blockmask.r], [blockones_b.r])
        offdiag = sbt("offdiag", [128, 128])
        TT("dve", offdiag.a, ones_f.a, ident_f.a, ALU.subtract, [ones_f.r, ident_f.r], [offdiag.r])
        maskneg = sbt("maskneg", [128, 2, 4, 128])
        for d_, mk_ in ((0, maskF), (1, maskB)):
            TS("dve", maskneg.a[:, d_], bc(mk_.a, [128, 4, 128], 1), -1.0, ALU.add, [mk_.r], [maskneg.r], s2=30000.0, op1=ALU.mult)
        bd = []
        Ebuf = sbt("Ebuf", [128, 128])
        for si, sz in enumerate((8, 16, 32, 64)):
            E = Ebuf
            aff(E, [[1, 128]], -sz, ALU.is_ge)
            P.op("pool", lambda e, E=E, sz=sz: e.affine_select(out=E.a, in_=E.a, pattern=[[-1, 128]], compare_op=ALU.is_ge, fill=0.0, base=sz - 1, channel_multiplier=sz), [E.r], [E.r])
            ng = 128 // sz
            bb = nb()
            MM(bb.a[:, 0:128], E.a[0:ng, :], E.a[0:ng, :], True, True, [E.r], [bb.r])
            m_ = sbt("bd%d" % sz, [128, 128])
            CP("dve", m_.a, bb.a[:, 0:128], [bb.r], [m_.r])
            bd.append(m_)
        bdm = [bd[0]]
        offm = []
        for si in range(4):
            o_ = sbt("off%d" % si, [128, 128])
            hi = bd[si + 1] if si < 3 else ones_f
            TT("dve", o_.a, hi.a, bd[si].a, ALU.subtract, [hi.r, bd[si].r], [o_.r])
            offm.append(o_)
        iota_i = sbt("iota_i", [128, 128], mybir.dt.int32)
        iota_ij = sbt("iota_ij", [128, 128])
        P.op("pool", lambda e: e.iota(out=iota_i.a, pattern=[[1, 128]], base=0, channel_multiplier=-1), [], [iota_i.r])
        CP("dve", iota_ij.a, iota_i.a, [iota_i.r], [iota_ij.r])
        pidx_i = sbt("pidx_i", [128, 1], mybir.dt.int32)
        pidx = sbt("pidx", [128, 1])
        P.op("pool", lambda e: e.iota(out=pidx_i.a, pattern=[[0, 1]], base=0, channel_multiplier=1), [], [pidx_i.r])
        CP("dve", pidx.a, pidx_i.a, [pidx_i.r], [pidx.r])
        rowt = sbt("rowt", [128, NRS])
        s2 = sbt("s2", [128, 8, 2])
        svt = sbt("svt", [128, 16])
        DMA("sp", svt.a, svec_d[:, :], [], [svt.r])
        ACT(s2.a.rearrange("p k w -> p w k"), svt.a.rearrange("p (w k) -> p w k", w=2), AF.Silu, [svt.r], [s2.r])
        modc = sbt("modc", [128, 16, 2])
        acol = sbt("acol", [128, 8, 2])
        grow = [sbt("grow%d" % w, [128, D]) for w in range(2)]
        colp = sbt("colp", [128, NCP])
        ss = sbt("ss", [128, 2 * NCH])
        rstd = sbt("rstd", [128, 2 * NCH])
        ss_r = [Res("ss%d" % i) for i in range(2 * NCH)]
        rs_r = [Res("rs%d" % i) for i in range(2 * NCH)]
        rope = sbt("rope", [128, 4, T], BF16)
        for i in range(4):
            DMA("sp", rope.a[:, i, :], rope_d[i], [], [rope.r])

        ycat_res = Res("ycat_dram")
        arena = Arena(nc, st, 43200)
        hT = arena.alloc([128, 8, NT], BF16, "hT")
        base_mark = arena.mark()

        def load_cast(dst_ap, dst_res, src_rows_ap, ncols, stage, wr=None):
            DMA("sp", stage.a[:, :ncols], src_rows_ap, [wr] if wr is not None else [], [stage.r])
            CP("pool", dst_ap, stage.a[:, :ncols], [stage.r], [dst_res])

        for l in range(n_layers):
            last = final_layer and (l == n_layers - 1)
            from_x = (l == 0) and not first_from_scratch
            arena.reset(base_mark)
            P.barrier()
            DMA("sp", colp.a, colp_d[l], [], [colp.r])
            DMA("sp", rowt.a, rows_d[l, :].partition_broadcast(128), [], [rowt.r])
            m0 = arena.mark()
            wmb = [arena.alloc([128, 8, 512], F32, "wm%d" % i) for i in range(2)]
            bgrow = arena.alloc([128, D], F32, "bgrow")
            pgrow = arena.alloc([128, D], F32, "pgrow")
            bmc = arena.alloc([128, 24], F32, "bmc")
            sbc = arena.alloc([128, 8, 2, 128], F32, "sbc")
            CP("dve", sbc.a, bc(s2.a, [128, 8, 2, 128], 3), [s2.r], [sbc.r])
            DMA("sp", bgrow.a, bmod_d[l, 2 * D:3 * D].partition_broadcast(128), [], [bgrow.r])
            DMA("sp", pgrow.a, postg_d[l, :].partition_broadcast(128), [], [pgrow.r])
            DMA("sp", bmc.a, bmodc_d[l], [], [bmc.r])
            pmod = nb()
            pmv = pmod.a[:, 0:32].rearrange("p (j w) -> p j w", w=2)
            wsrc = wmod_d[l].rearrange("(k p) n -> p k n", p=128)
            for cg in range(6):
                wm = wmb[cg % 2]
                DMA("sp", wm.a, wsrc[:, :, cg * 512:(cg + 1) * 512], [wres[l]["mod"]], [wm.r])
                if cg < 4:
                    for jj in range(4):
                        j = cg * 4 + jj
                        for k in range(8):
                            MM(pmv[:, j, :], wm.a[:, k, jj * 128:(jj + 1) * 128], s2.a[:, k, :], k == 0, k == 7, [wm.r, s2.r], [pmod.r])
                else:
                    cs = slice((cg - 4) * 512, (cg - 3) * 512)
                    for w in range(2):
                        pg = nb()
                        for k in range(8):
                            MM(pg.a, sbc.a[:, k, w, :], wm.a[:, k, :], k == 0, k == 7, [wm.r, sbc.r], [pg.r])
                        TT("dve", grow[w].a[:, cs], pg.a, bgrow.a[:, cs], ALU.add, [pg.r, bgrow.r], [grow[w].r])
                        TT("pool", grow[w].a[:, cs], grow[w].a[:, cs], pgrow.a[:, cs], ALU.mult, [grow[w].r, pgrow.r], [grow[w].r])
            TT("dve", modc.a, pmv, bc(bmc.a[:, 0:16], [128, 16, 2], 2), ALU.add, [pmod.r, bmc.r], [modc.r])
            STT("dve", acol.a, modc.a[:, 8:16, :], 1.0, bc(colp.a[:, 0:8], [128, 8, 2], 2), ALU.add, ALU.mult, [modc.r, colp.r], [acol.r])
            arena.reset(m0)
            P.barrier()
            m0 = arena.mark()
            if "B" not in phases:
                og = P.group(dedicated=True)
                DMA("sp", out_d[0:128, :], grow[0].a, [grow[0].r], [])
                continue
            xb = [arena.alloc([128, D], F32, "xb%d" % i) for i in range(2)]
            junks = [arena.alloc([128, D], BF16, "junk%d" % i) for i in range(2)]
            xn = [arena.alloc([128, D], BF16, "xn%d" % i) for i in range(2)]
            tmpfs = [arena.alloc([128, 8, 128], F32, "tmpf%d" % i) for i in range(2)]
            for c in range(NCH):
                xt = xb[c % 2]
                if from_x:
                    src = x_d[c * 128:(c + 1) * 128, :] if c < 16 else ctx_d[(c - 16) * 128:(c - 15) * 128, :]
                else:
                    src = xs_d[c * 128:(c + 1) * 128, :]
                DMA("sp", xt.a, src, [], [xt.r])
                junk = junks[c % 2]
                tmpf = tmpfs[c % 2]
                ACT(junk.a, xt.a, AF.Square, [xt.r], [junk.r, ss_r[c]], accum=ss.a[:, c:c + 1])
                ACT(rstd.a[:, c:c + 1], ss.a[:, c:c + 1], AF.Sqrt, [ss_r[c], cst.r], [rs_r[c]], bias=cst.a[:, 0:1], scale=1.0 / D)
                P.op("dve", lambda e, o=rstd.a[:, c:c + 1]: e.reciprocal(out=o, in_=o), [rs_r[c]], [rs_r[c]])
                xnt = xn[c % 2]
                TS("dve", xnt.a, xt.a, rstd.a[:, c:c + 1], ALU.mult, [xt.r, rs_r[c]], [xnt.r])
                pt = nb()
                ptb = pt.a.bitcast(BF16).rearrange("p (j t) -> p j t", j=8)
                for j in range(8):
                    TR(ptb[:, j, :], xnt.a[:, j * 128:(j + 1) * 128], ident_b.a, [xnt.r, ident_b.r], [pt.r])
                w = 0 if c < 16 else 1
                TT("dve", tmpf.a, ptb, bc(acol.a[:, :, w], [128, 8, 128], 2), ALU.mult, [pt.r, acol.r], [tmpf.r])
                TT("pool", hT.a[:, :, c * 128:(c + 1) * 128], tmpf.a, bc(modc.a[:, 0:8, w], [128, 8, 128], 2), ALU.add, [tmpf.r, modc.r], [hT.r])
            arena.reset(m0)
            P.barrier()
            if debug and l == n_layers - 1:
                DMA("sp", dbg_h.rearrange("k p t -> p k t"), hT.a, [hT.r], [])

            m_mix = arena.mark()
            if mixers != "ABCD":
                zt = arena.alloc([128, NT], BF16, "zt")
                MSET("pool", zt.a, 0.0, [zt.r])
                for i in range(8):
                    DMA("sp", ycat_d[i], zt.a, [zt.r], [ycat_res])
                arena.reset(m_mix)
                P.barrier()
            wbuf = arena.alloc([128, 8, 1280], BF16, "wbuf")
            wstage = [arena.alloc([128, 1280], F32, "wst%d" % i) for i in range(2)]
            m_mix2 = arena.mark()

            def load_w(coloff, ncols, dst):
                for k in range(8):
                    load_cast(dst.a[:, k, 0:ncols], dst.r, win_d[l, k * 128:(k + 1) * 128, coloff:coloff + ncols], ncols, wstage[k % 2], wres[l]["in"])

            def inproj_fm(wt, col0, t0, n, M=128):
                b = nb()
                for k in range(8):
                    MM(b.a[0:M, 0:n], wt.a[:, k, col0:col0 + M], hT.a[:, k, t0:t0 + n], k == 0, k == 7, [wt.r, hT.r], [b.r])
                return b

            def inproj_tm(wt, col0, ncols, c):
                b = nb()
                for k in range(8):
                    MM(b.a[:, 0:ncols], hT.a[:, k, c * 128:(c + 1) * 128], wt.a[:, k, col0:col0 + ncols], k == 0, k == 7, [wt.r, hT.r], [b.r])
                return b

            def store_ycat(yt, tile_idx):
                DMA("sp", ycat_d[tile_idx], yt.a, [yt.r], [])

            if "A" in mixers:
                load_w(WA, 512, wbuf)
                dg = arena.alloc([128, 8, 128], BF16, "dg")
                for i in range(8):
                    TS("pool", dg.a[:, i, :], ident_f.a, colp.a[:, 8 + i:9 + i], ALU.mult, [ident_f.r, colp.r], [dg.r])
                wgb = arena.alloc([128, 8, 128], BF16, "wgb")
                wgs = arena.alloc([128, 8, 128], F32, "wgs")
                MSET("pool", wgs.a, 0.0, [wgs.r])
                for d_ in range(2):
                    for gi_ in range(2):
                        for blk_ in range(4):
                            t_, o_ = blk_ // 2, (blk_ % 2) * 64
                            DMA("sp", wgs.a[o_:o_ + 64, d_ * 4 + gi_ * 2 + t_, o_:o_ + 64], lruw_d[l, gi_, d_, blk_], [], [wgs.r])
                CP("pool", wgb.a, wgs.a, [wgs.r], [wgb.r])
                kcol = arena.alloc([128, 4], F32, "kcol")
                ACT(kcol.a, colp.a[:, 26:30], AF.Exp, [colp.r], [kcol.r], scale=-1.0)
                ACT(kcol.a, kcol.a, AF.Ln, [kcol.r, cst.r], [kcol.r], bias=cst.a[:, 1:2])
                TS("dve", kcol.a, kcol.a, -8.0, ALU.mult, [kcol.r], [kcol.r])
                xpad = arena.alloc([128, 2, 2310], BF16, "xpad")
                MSET("pool", xpad.a, 0.0, [xpad.r])
                ub = arena.alloc([128, 2, NT], BF16, "ub")
                for t in range(2):
                    for (t0, n) in BLKS:
                        b = inproj_fm(wbuf, t * 128, t0, n)
                        o0 = t0 + 2 if t0 < T else 2053
                        CP("act", xpad.a[:, t, o0:o0 + n], b.a[:, 0:n], [b.r], [xpad.r])
                for t in range(2):
                    for (t0, n) in BLKS:
                        j0 = t0 if t0 < T else 2051
                        b = nb()
                        for k in range(4):
                            MM(b.a[:, 0:n], dg.a[:, k * 2 + t, :], xpad.a[:, t, j0 + k:j0 + k + n], k == 0, k == 3, [dg.r, xpad.r], [b.r])
                        ACT(ub.a[:, t, t0:t0 + n], b.a[:, 0:n], AF.Identity, [b.r, colp.r], [ub.r], bias=colp.a[:, 16 + t:17 + t])
                a_buf = arena.alloc([128, NT], F32, "a_buf")
                b_buf = arena.alloc([128, NT], F32, "b_buf")
                hbuf = [arena.alloc([128, NT], F32, "h%d" % i) for i in range(2)]
                rr = [arena.alloc([128, 512], F32, "rr%d" % i) for i in range(2)]
                ii = [arena.alloc([128, 512], F32, "ii%d" % i) for i in range(2)]
                tq = [arena.alloc([128, 512], F32, "tq%d" % i) for i in range(2)]
                yt = arena.alloc([128, NT], BF16, "yt")
                for t in range(2):
                    for d in range(2):
                        for bi, (t0, n) in enumerate(BLKS):
                            r_, i_, q_ = rr[bi % 2], ii[bi % 2], tq[bi % 2]
                            b1 = nb()
                            MM(b1.a[:, 0:n], wgb.a[:, d * 4 + 0 + t, :], ub.a[:, t, t0:t0 + n], True, True, [wgb.r, ub.r], [b1.r])
                            ACT(r_.a[:, 0:n], b1.a[:, 0:n], AF.Sigmoid, [b1.r, colp.r], [r_.r], bias=colp.a[:, 18 + d * 2 + t:19 + d * 2 + t])
                            b2 = nb()
                            MM(b2.a[:, 0:n], wgb.a[:, d * 4 + 2 + t, :], ub.a[:, t, t0:t0 + n], True, True, [wgb.r, ub.r], [b2.r])
                            ACT(i_.a[:, 0:n], b2.a[:, 0:n], AF.Sigmoid, [b2.r, colp.r], [i_.r], bias=colp.a[:, 22 + d * 2 + t:23 + d * 2 + t])
                            ACT(a_buf.a[:, t0:t0 + n], r_.a[:, 0:n], AF.Exp, [r_.r, kcol.r], [a_buf.r], scale=kcol.a[:, d * 2 + t:d * 2 + t + 1])
                            TT("dve", q_.a[:, 0:n], a_buf.a[:, t0:t0 + n], a_buf.a[:, t0:t0 + n], ALU.mult, [a_buf.r], [q_.r])
                            ACT(q_.a[:, 0:n], q_.a[:, 0:n], AF.Sqrt, [q_.r, cst.r], [q_.r], bias=cst.a[:, 1:2], scale=-1.0)
                            TT("dve", q_.a[:, 0:n], q_.a[:, 0:n], i_.a[:, 0:n], ALU.mult, [q_.r, i_.r], [q_.r])
                            TT("pool", b_buf.a[:, t0:t0 + n], q_.a[:, 0:n], ub.a[:, t, t0:t0 + n], ALU.mult, [q_.r, ub.r], [b_buf.r])
                        h = hbuf[d]
                        if d == 0:
                            SCAN(h.a[:, T:NT], a_buf.a[:, T:NT], b_buf.a[:, T:NT], 0.0, [a_buf.r, b_buf.r], [h.r])
                            SCAN(h.a[:, 0:T], a_buf.a[:, 0:T], b_buf.a[:, 0:T], h.a[:, NT - 1:NT], [a_buf.r, b_buf.r, h.r], [h.r])
                        else:
                            SCAN(rev(h.a[:, T:NT]), rev(a_buf.a[:, T:NT]), rev(b_buf.a[:, T:NT]), 0.0, [a_buf.r, b_buf.r], [h.r])
                            SCAN(rev(h.a[:, 0:T]), rev(a_buf.a[:, 0:T]), rev(b_buf.a[:, 0:T]), h.a[:, T:T + 1], [a_buf.r, b_buf.r, h.r], [h.r])
                    for bi, (t0, n) in enumerate(BLKS):
                        b = inproj_fm(wbuf, 256 + t * 128, t0, n)
                        z_ = rr[bi % 2]
                        ACT(z_.a[:, 0:n], b.a[:, 0:n], AF.Silu, [b.r], [z_.r])
                        q_ = tq[bi % 2]
                        TT("dve", q_.a[:, 0:n], hbuf[0].a[:, t0:t0 + n], hbuf[1].a[:, t0:t0 + n], ALU.add, [hbuf[0].r, hbuf[1].r], [q_.r])
                        TT("pool", yt.a[:, t0:t0 + n], q_.a[:, 0:n], z_.a[:, 0:n], ALU.mult, [q_.r, z_.r], [yt.r])
                    store_ycat(yt, 0 + t)
                arena.reset(m_mix2)
                P.barrier()

            if "B" in mixers:
                load_w(WB, 784, wbuf)
                wz = arena.alloc([128, 8, 256], BF16, "wz")
                load_w(WBZ, 256, wz)
                qT = arena.alloc([128, 2, NT], BF16, "qT")
                kT = arena.alloc([128, 2, NT], BF16, "kT")
                k_tok = arena.alloc([128, NCH, 256], BF16, "k_tok")
                v_tok = arena.alloc([128, NCH, 256], BF16, "v_tok")
                g_tok = arena.alloc([128, NCH, 8], F32, "g_tok")
                nbeta = arena.alloc([128, NCH, 8], F32, "nbeta")
                beta = arena.alloc([128, NCH, 8], F32, "beta")
                nega = arena.alloc([128, 8], F32, "nega")
                mB = arena.mark()
                vT = arena.alloc([128, 2, NT], BF16, "vT")
                dgB = arena.alloc([128, 24, 128], BF16, "dgB")
                for i in range(24):
                    TS("pool", dgB.a[:, i, :], ident_f.a, colp.a[:, 30 + i:31 + i], ALU.mult, [ident_f.r, colp.r], [dgB.r])
                xpads = [arena.alloc([128, 2310], BF16, "xpad%d" % i) for i in range(2)]
                for xp in xpads:
                    MSET("pool", xp.a, 0.0, [xp.r])
                sl = [arena.alloc([128, 512], F32, "sl%d" % i) for i in range(2)]
                sqb = [arena.alloc([128, 512], BF16, "sqb%d" % i) for i in range(2)]
                rn = [arena.alloc([128, 512], F32, "rn%d" % i) for i in range(2)]
                cnt = 0
                for ti in range(6):
                    xp = xpads[ti % 2]
                    for (t0, n) in BLKS:
                        b = inproj_fm(wbuf, ti * 128, t0, n)
                        o0 = t0 + 2 if t0 < T else 2053
                        CP("act", xp.a[:, o0:o0 + n], b.a[:, 0:n], [b.r], [xp.r])
                    for (t0, n) in BLKS:
                        j0 = t0 if t0 < T else 2051
                        b = nb()
                        for k in range(4):
                            MM(b.a[:, 0:n], dgB.a[:, k * 6 + ti, :], xp.a[:, j0 + k:j0 + k + n], k == 0, k == 3, [dgB.r, xp.r], [b.r])
                        if ti >= 4:
                            ACT(vT.a[:, ti - 4, t0:t0 + n], b.a[:, 0:n], AF.Silu, [b.r], [vT.r])
                            continue
                        s_, q_, r_ = sl[cnt % 2], sqb[cnt % 2], rn[cnt % 2]
                        cnt += 1
                        ACT(s_.a[:, 0:n], b.a[:, 0:n], AF.Silu, [b.r], [s_.r])
                        TT("pool", q_.a[:, 0:n], s_.a[:, 0:n], s_.a[:, 0:n], ALU.mult, [s_.r], [q_.r])
                        b2 = nb()
                        MM(b2.a[:, 0:n], blockones_b.a, q_.a[:, 0:n], True, True, [blockones_b.r, q_.r], [b2.r])
                        ACT(r_.a[:, 0:n], b2.a[:, 0:n], AF.Sqrt, [b2.r, cst.r], [r_.r], bias=cst.a[:, 0:1])
                        P.op("dve", lambda e, o=r_.a[:, 0:n]: e.reciprocal(out=o, in_=o), [r_.r], [r_.r])
                        dst = qT if ti < 2 else kT
                        STT("dve", dst.a[:, ti % 2, t0:t0 + n], s_.a[:, 0:n], 0.125 if ti < 2 else 1.0, r_.a[:, 0:n], ALU.mult, ALU.mult, [s_.r, r_.r], [dst.r])
                ab = arena.alloc([128, NCH, 16], F32, "ab")
                for c in range(NCH):
                    for src, dstt in ((kT, k_tok), (vT, v_tok)):
                        pt = nb()
                        ptb = pt.a.bitcast(BF16)[:, 0:256]
                        for t in range(2):
                            TR(ptb[:, t * 128:(t + 1) * 128], src.a[:, t, c * 128:(c + 1) * 128], ident_b.a, [src.r, ident_b.r], [pt.r])
                        CP("act" if dstt is k_tok else "dve", dstt.a[:, c, :], ptb, [pt.r], [dstt.r])
                    b = inproj_tm(wbuf, 768, 16, c)
                    CP("dve", ab.a[:, c, :], b.a[:, 0:16], [b.r], [ab.r])
                ACT(nega.a, rowt.a[:, 0:8], AF.Exp, [rowt.r], [nega.r])
                TS("dve", nega.a, nega.a, -1.0, ALU.mult, [nega.r], [nega.r])
                TT("dve", g_tok.a, ab.a[:, :, 0:8], bc(rowt.a[:, 8:16], [128, NCH, 8], 1), ALU.add, [ab.r, rowt.r], [g_tok.r])
                ACT(g_tok.a, g_tok.a, AF.Exp, [g_tok.r], [g_tok.r])
                ACT(g_tok.a, g_tok.a, AF.Ln, [g_tok.r, cst.r], [g_tok.r], bias=cst.a[:, 1:2])
                TT("dve", g_tok.a, g_tok.a, bc(nega.a, [128, NCH, 8], 1), ALU.mult, [g_tok.r, nega.r], [g_tok.r])
                ACT(beta.a, ab.a[:, :, 8:16], AF.Sigmoid, [ab.r], [beta.r])
                TS("dve", nbeta.a, beta.a, -1.0, ALU.mult, [beta.r], [nbeta.r])
                P.barrier()
                arena.reset(mB)
                o_acc = arena.alloc([128, NCH, 256], F32, "o_acc")
                class WS:
                    pass
                wss = []
                for d in range(2):
                    w_ = WS()
                    stg = wstage[d]
                    w_.gTri = Tl(stg.a[:, 0:512].rearrange("p (h i) -> p h i", h=4), stg.r)
                    w_.DT = Tl(stg.a[:, 512:1024].rearrange("p (h i) -> p h i", h=4), stg.r)
                    w_.t1 = arena.alloc([128, 4, 128], BF16, "t1_%d" % d)
                    w_.NB = arena.alloc([128, 4, 128], BF16, "NB%d" % d)
                    w_.M = [arena.alloc([128, 4, 128], BF16, "M%d_%d" % (d, i)) for i in range(2)]
                    w_.N = [arena.alloc([128, 4, 128], BF16, "N%d_%d" % (d, i)) for i in range(2)]
                    w_.PT = [arena.alloc([128, 4, 128], BF16, "PT%d_%d" % (d, i)) for i in range(2)]
                    w_.Mb = w_.M[1]
                    w_.Nb = w_.N[1]
                    w_.U = w_.PT[0]
                    w_.Tm = w_.PT[1]
                    w_.Y1 = arena.alloc([128, 4, 128], BF16, "Y1_%d" % d)
                    w_.Y2 = arena.alloc([128, 4, 128], BF16, "Y2_%d" % d)
                    w_.att = arena.alloc([128, 4, 128], BF16, "att%d" % d)
                    w_.kz = arena.alloc([128, 4, 128], BF16, "kz%d" % d)
                    MSET("pool", w_.kz.a, 0.0, [w_.kz.r])
                    w_.kg = arena.alloc([128, 4, 64], BF16, "kg%d" % d)
                    w_.ktl = arena.alloc([128, 4, 64], BF16, "ktl%d" % d)
                    w_.negWt = arena.alloc([128, 2, 128], BF16, "negWt%d" % d)
                    w_.vnew = arena.alloc([128, 4, 64], BF16, "vnew%d" % d)
                    w_.to = arena.alloc([128, 256], F32, "to%d" % d)
                    w_.sm = arena.alloc([128, 32], F32, "sm%d" % d)
                    w_.S32 = arena.alloc([128, 2, 128], F32, "S32_%d" % d)
                    w_.Sbd = arena.alloc([128, 2, 128], BF16, "Sbd%d" % d)
                    MSET("pool", w_.S32.a, 0.0, [w_.S32.r])
                    MSET("pool", w_.Sbd.a, 0.0, [w_.Sbd.r])
                    wss.append(w_)

                orderF = [16, 17] + list(range(16))
                orderB = [17, 16] + list(range(15, -1, -1))

                def gdn_pre(c, d):
                    w_ = wss[d]
                    cs = slice(c * 128, (c + 1) * 128)
                    tri = maskF if d == 0 else maskB
                    g4 = g_tok.a[:, c, d * 4:(d + 1) * 4]
                    nb4 = nbeta.a[:, c, d * 4:(d + 1) * 4]
                    sm = w_.sm
                    gcb = nb()
                    MM(gcb.a[:, 0:4], tri.a, g4, True, True, [tri.r, g_tok.r], [gcb.r])
                    MM(gcb.a[:, 4:8], ones_f.a, g4, True, True, [ones_f.r, g_tok.r], [gcb.r])
                    CP("dve", sm.a[:, 0:8], gcb.a[:, 0:8], [gcb.r], [sm.r])
                    yield
                    TS("dve", sm.a[:, 8:12], sm.a[:, 0:4], -1.0, ALU.mult, [sm.r], [sm.r])
                    TT("dve", sm.a[:, 16:20], sm.a[:, 4:8], sm.a[:, 0:4], ALU.subtract, [sm.r], [sm.r])
                    ACT(sm.a[:, 12:16], sm.a[:, 0:4], AF.Exp, [sm.r], [sm.r])
                    ACT(sm.a[:, 16:20], sm.a[:, 16:20], AF.Exp, [sm.r], [sm.r])
                    ACT(sm.a[:, 20:24], sm.a[:, 4:8], AF.Exp, [sm.r], [sm.r])
                    for t in range(2):
                        CP("dve", sm.a[0:64, 24 + t:25 + t], sm.a[0:64, 20 + 2 * t:21 + 2 * t], [sm.r], [sm.r])
                        CP("dve", sm.a[64:128, 24 + t:25 + t], sm.a[64:128, 21 + 2 * t:22 + 2 * t], [sm.r], [sm.r])
                    TT("pool", w_.gTri.a, bc(tri.a, [128, 4, 128], 1), bc(g4, [128, 4, 128], 2), ALU.mult, [tri.r, g_tok.r], [w_.gTri.r])
                    yield
                    GB = nb()
                    MM(GB.a, ones_f.a, w_.gTri.a.rearrange("p h i -> p (h i)"), True, False, [ones_f.r, w_.gTri.r], [GB.r])
                    MM(GB.a, ident_f.a, maskneg.a[:, d].rearrange("p h i -> p (h i)"), False, True, [ident_f.r, maskneg.r], [GB.r])
                    yield
                    for h in range(4):
                        ACT(w_.DT.a[:, h, :], GB.a[:, h * 128:(h + 1) * 128], AF.Exp, [GB.r, sm.r], [w_.DT.r], bias=sm.a[:, 8 + h:9 + h])
                    for t in range(2):
                        CP("pool", w_.kz.a[0:64, 2 * t, :], kT.a[0:64, t, cs], [kT.r], [w_.kz.r])
                        CP("pool", w_.kz.a[64:128, 2 * t + 1, :], kT.a[64:128, t, cs], [kT.r], [w_.kz.r])
                    yield
                    KK = nb()
                    QK = nb()
                    for h in range(4):
                        MM(KK.a[:, h * 128:(h + 1) * 128], w_.kz.a[:, h, :], kT.a[:, h // 2, cs], True, True, [w_.kz.r, kT.r], [KK.r])
                    for h in range(4):
                        MM(QK.a[:, h * 128:(h + 1) * 128], w_.kz.a[:, h, :], qT.a[:, h // 2, cs], True, True, [w_.kz.r, qT.r], [QK.r])
                    TT("dve", w_.t1.a, KK.a.rearrange("p (h i) -> p h i", h=4), w_.DT.a, ALU.mult, [KK.r, w_.DT.r], [w_.t1.r])
                    TT("dve", w_.att.a, QK.a.rearrange("p (h i) -> p h i", h=4), w_.DT.a, ALU.mult, [QK.r, w_.DT.r], [w_.att.r])
                    TT("pool", w_.NB.a, bc(offdiag.a, [128, 4, 128], 1), bc(nb4, [128, 4, 128], 2), ALU.mult, [offdiag.r, nbeta.r], [w_.NB.r])
                    M, N, PT = w_.M, w_.N, w_.PT
                    TT("pool", M[0].a, w_.t1.a, w_.NB.a, ALU.mult, [w_.t1.r, w_.NB.r], [M[0].r])
                    yield
                    pt = nb()
                    ptb = pt.a.bitcast(BF16)[:, 0:512]
                    for h in range(4):
                        TR(ptb[:, h * 128:(h + 1) * 128], M[0].a[:, h, :], ident_b.a, [M[0].r, ident_b.r], [pt.r])
                    CP("act", N[0].a.rearrange("p h i -> p (h i)"), ptb, [pt.r], [N[0].r])
                    yield
                    M0_, N0_ = M[0], N[0]
                    Mb, Nb, U, Tm, Y1, Y2 = w_.Mb, w_.Nb, w_.U, w_.Tm, w_.Y1, w_.Y2
                    f4 = lambda tl: tl.a.rearrange("p h i -> p (h i)")
                    i4 = bc(ident_b.a, [128, 4, 128], 1)

                    def mm4(lhs, rhs):
                        b_ = nb()
                        for h in range(4):
                            MM(b_.a[:, h * 128:(h + 1) * 128], lhs.a[:, h, :], rhs.a[:, h, :], True, True, [lhs.r, rhs.r], [b_.r])
                        return b_

                    m8 = bc(bdm[0].a, [128, 4, 128], 1)
                    TT("pool", Mb.a, M0_.a, m8, ALU.mult, [M0_.r, bdm[0].r], [Mb.r])
                    TT("pool", Nb.a, N0_.a, m8, ALU.mult, [N0_.r, bdm[0].r], [Nb.r])
                    TT("pool", U.a, Mb.a, i4, ALU.add, [Mb.r, ident_b.r], [U.r])
                    TT("pool", Tm.a, Nb.a, i4, ALU.add, [Nb.r, ident_b.r], [Tm.r])
                    yield
                    for lev in range(2):
                        bM = mm4(Nb, Mb)
                        bN = mm4(Mb, Nb)
                        CP("act", f4(Y1), bM.a, [bM.r], [Y1.r])
                        CP("dve", f4(Y2), bN.a, [bN.r], [Y2.r])
                        yield
                        bU = mm4(Y2, U)
                        bT = mm4(Y1, Tm)
                        TT("dve", f4(U), f4(U), bU.a, ALU.add, [U.r, bU.r], [U.r])
                        TT("dve", f4(Tm), f4(Tm), bT.a, ALU.add, [Tm.r, bT.r], [Tm.r])
                        yield
                        if lev == 0:
                            CP("pool", Mb.a, Y1.a, [Y1.r], [Mb.r])
                            CP("pool", Nb.a, Y2.a, [Y2.r], [Nb.r])
                    for li in range(4):
                        mo = bc(offm[li].a, [128, 4, 128], 1)
                        TT("pool", Mb.a, M0_.a, mo, ALU.mult, [M0_.r, offm[li].r], [Mb.r])
                        TT("pool", Nb.a, N0_.a, mo, ALU.mult, [N0_.r, offm[li].r], [Nb.r])
                        bY = mm4(Nb, U)
                        CP("act", f4(Y1), bY.a, [bY.r], [Y1.r])
                        if li < 3:
                            bY2 = mm4(Mb, Tm)
                            CP("dve", f4(Y2), bY2.a, [bY2.r], [Y2.r])
                        yield
                        bZ = mm4(Tm, Y1)
                        if li < 3:
                            bZ2 = mm4(U, Y2)
                        TT("dve", f4(U), f4(U), bZ.a, ALU.add, [U.r, bZ.r], [U.r])
                        if li < 3:
                            TT("dve", f4(Tm), f4(Tm), bZ2.a, ALU.add, [Tm.r, bZ2.r], [Tm.r])
                        yield
                    PT = [U]
                    cur = 0
                    w_.ptf = PT[cur]
                    k4 = k_tok.a[:, c, :].rearrange("p (h e) -> p h e", h=4)
                    TT("pool", w_.kg.a, k4, bc(sm.a[:, 12:16], [128, 4, 64], 2), ALU.mult, [k_tok.r, sm.r], [w_.kg.r])
                    TT("pool", w_.ktl.a, k4, bc(sm.a[:, 16:20], [128, 4, 64], 2), ALU.mult, [k_tok.r, sm.r], [w_.ktl.r])
                    yield
                    WtP = nb()
                    for h in range(4):
                        t = h // 2
                        MM(WtP.a[:, h * 128:(h + 1) * 128], w_.kg.a[:, 2 * t:2 * t + 2, :].rearrange("p h e -> p (h e)"), w_.ptf.a[:, h, :], True, True, [w_.kg.r, w_.ptf.r], [WtP.r])
                    w4 = WtP.a.rearrange("p (t hh i) -> p t hh i", t=2, hh=2)
                    ACT(w_.negWt.a[0:64, :, :], w4[0:64, :, 0, :], AF.Copy, [WtP.r], [w_.negWt.r], scale=-1.0)
                    TS("dve", w_.negWt.a[64:128, :, :], w4[64:128, :, 1, :], -1.0, ALU.mult, [WtP.r], [w_.negWt.r])

                def gdn_seq(c, d):
                    w_ = wss[d]
                    cs = slice(c * 128, (c + 1) * 128)
                    sm = w_.sm
                    Vn = nb()
                    for t in range(2):
                        MM(Vn.a[:, t * 128:(t + 1) * 128], w_.negWt.a[:, t, :], w_.Sbd.a[:, t, :], True, False, [w_.negWt.r, w_.Sbd.r], [Vn.r])
                        for hh in range(2):
                            h = 2 * t + hh
                            MM(Vn.a[:, h * 64:(h + 1) * 64], w_.ptf.a[:, h, :], v_tok.a[:, c, h * 64:(h + 1) * 64], False, hh == 1, [w_.ptf.r, v_tok.r], [Vn.r])
                    TT("dve", w_.vnew.a, Vn.a[:, 0:256].rearrange("p (h e) -> p h e", h=4), bc(beta.a[:, c, d * 4:(d + 1) * 4], [128, 4, 64], 2), ALU.mult, [Vn.r, beta.r], [w_.vnew.r])
                    yield
                    Oi = nb()
                    for t in range(2):
                        MM(Oi.a[:, t * 128:(t + 1) * 128], qT.a[:, t, cs], w_.Sbd.a[:, t, :], True, True, [qT.r, w_.Sbd.r], [Oi.r])
                    Oa = nb()
                    for h in range(4):
                        MM(Oa.a[:, h * 64:(h + 1) * 64], w_.att.a[:, h, :], w_.vnew.a[:, h, :], True, True, [w_.att.r, w_.vnew.r], [Oa.r])
                    TT("dve", w_.to.a.rearrange("p (h e) -> p h e", h=4), Oi.a[:, 0:256].rearrange("p (h e) -> p h e", h=4), bc(sm.a[:, 12:16], [128, 4, 64], 2), ALU.mult, [Oi.r, sm.r], [w_.to.r])
                    first = (orderF.index(c) <= orderB.index(c)) == (d == 0) and orderF.index(c) != orderB.index(c)
                    import os
                    if first or len(os.environ.get("GDN_DIRS", "01")) == 1:
                        TT("dve", o_acc.a[:, c, :], w_.to.a, Oa.a[:, 0:256], ALU.add, [w_.to.r, Oa.r], [o_acc.r])
                    else:
                        TT("dve", w_.to.a, w_.to.a, Oa.a[:, 0:256], ALU.add, [w_.to.r, Oa.r], [w_.to.r])
                        TT("pool", o_acc.a[:, c, :], o_acc.a[:, c, :], w_.to.a, ALU.add, [w_.to.r, o_acc.r], [o_acc.r])
                    yield
                    for t in range(2):
                        sp_ = nb()
                        MM(sp_.a[:, 0:128], w_.ktl.a[:, 2 * t:2 * t + 2, :].rearrange("p h e -> p (h e)"), w_.vnew.a[:, 2 * t:2 * t + 2, :].rearrange("p h e -> p (h e)"), True, True, [w_.ktl.r, w_.vnew.r], [sp_.r])
                        STT("dve", w_.S32.a[:, t, :], w_.S32.a[:, t, :], sm.a[:, 24 + t:25 + t], sp_.a[:, 0:128], ALU.mult, ALU.add, [w_.S32.r, sm.r, sp_.r], [w_.S32.r])
                        TT("pool", w_.Sbd.a[:, t, :], w_.S32.a[:, t, :], blockmask.a, ALU.mult, [w_.S32.r, blockmask.r], [w_.Sbd.r])

                import os
                gdirs = os.environ.get("GDN_DIRS", "01")
                def run_il(gens):
                    gens = list(gens)
                    while gens:
                        for g_ in list(gens):
                            try:
                                next(g_)
                            except StopIteration:
                                gens.remove(g_)

                for s_i in range(NCH):
                    run_il([gdn_pre(orderF[s_i], 0), gdn_pre(orderB[s_i], 1)])
                    run_il([gdn_seq(orderF[s_i], 0), gdn_seq(orderB[s_i], 1)])
                ycT = qT
                sqi = [arena.alloc([128, 4, 64], F32, "sqi%d" % i) for i in range(2)]
                dti = [arena.alloc([128, 4, 64], F32, "dti%d" % i) for i in range(2)]
                zsi = [arena.alloc([128, 256], F32, "zsi%d" % i) for i in range(2)]
                yti = [arena.alloc([128, 256], BF16, "yti%d" % i) for i in range(2)]
                stt = [arena.alloc([128, 8], F32, "stt%d" % i) for i in range(2)]
                for c in range(NCH):
                    zb = inproj_tm(wz, 0, 256, c)
                    zs, dt_, sq_, yt_, s_ = zsi[c % 2], dti[c % 2], sqi[c % 2], yti[c % 2], stt[c % 2]
                    ACT(zs.a, zb.a[:, 0:256], AF.Silu, [zb.r], [zs.r])
                    o4 = o_acc.a[:, c, :].rearrange("p (h e) -> p h e", h=4)
                    TT("pool", sq_.a, o4, o4, ALU.mult, [o_acc.r], [sq_.r])
                    P.op("dve", lambda e, o=s_.a[:, 0:4], i=sq_.a: e.tensor_reduce(out=o, in_=i, axis=AX.X, op=ALU.add), [sq_.r], [s_.r])
                    ACT(s_.a[:, 0:4], s_.a[:, 0:4], AF.Sqrt, [s_.r, cst.r], [s_.r], bias=cst.a[:, 0:1], scale=1.0 / 64)
                    P.op("dve", lambda e, o=s_.a[:, 0:4]: e.reciprocal(out=o, in_=o), [s_.r], [s_.r])
                    TT("dve", dt_.a, o4, bc(s_.a[:, 0:4], [128, 4, 64], 2), ALU.mult, [o_acc.r, s_.r], [dt_.r])
                    TT("pool", dt_.a, dt_.a, bc(rowt.a[:, 16:80], [128, 4, 64], 1), ALU.mult, [dt_.r, rowt.r], [dt_.r])
                    TT("pool", yt_.a, dt_.a.rearrange("p h e -> p (h e)"), zs.a, ALU.mult, [dt_.r, zs.r], [yt_.r])
                    pt = nb()
                    ptb = pt.a.bitcast(BF16)[:, 0:256]
                    for t in range(2):
                        TR(ptb[:, t * 128:(t + 1) * 128], yt_.a[:, t * 128:(t + 1) * 128], ident_b.a, [yt_.r, ident_b.r], [pt.r])
                    CP("act", ycT.a[:, :, c * 128:(c + 1) * 128], ptb.rearrange("p (t i) -> p t i", t=2), [pt.r], [ycT.r])
                for t in range(2):
                    DMA("sp", ycat_d[2 + t], ycT.a[:, t, :], [ycT.r], [])
                arena.reset(m_mix2)
                P.barrier()

            if "C" in mixers:
                load_w(WC, 1280, wbuf)
                wz = arena.alloc([128, 8, 256], BF16, "wz")
                load_w(WCZ, 256, wz)
                lg = arena.alloc([128, 8], F32, "lg")
                ACT(lg.a, rowt.a[:, 80:88], AF.Sigmoid, [rowt.r], [lg.r])
                ACT(lg.a, lg.a, AF.Ln, [lg.r], [lg.r])
                nlg = arena.alloc([128, 8], F32, "nlg")
                TS("dve", nlg.a, lg.a, -1.0, ALU.mult, [lg.r], [nlg.r])
                DT = arena.alloc([128, 2, 4, 128], F32, "DT")
                for h in range(4):
                    ACT(DT.a[:, 0, h, :], iota_ij.a, AF.Exp, [iota_ij.r, lg.r], [DT.r], scale=lg.a[:, h:h + 1])
                    ACT(DT.a[:, 1, h, :], iota_ij.a, AF.Exp, [iota_ij.r, nlg.r], [DT.r], scale=nlg.a[:, 4 + h:5 + h])
                TT("pool", DT.a[:, 0], DT.a[:, 0], bc(maskF.a, [128, 4, 128], 1), ALU.mult, [DT.r, maskF.r], [DT.r])
                TT("pool", DT.a[:, 1], DT.a[:, 1], bc(maskB.a, [128, 4, 128], 1), ALU.mult, [DT.r, maskB.r], [DT.r])
                pc = arena.alloc([128, 4], F32, "pc")
                TS("dve", pc.a[:, 0:1], pidx.a, 1.0, ALU.add, [pidx.r], [pc.r])
                TS("dve", pc.a[:, 1:2], pidx.a, -1.0, ALU.mult, [pidx.r], [pc.r], s2=128.0, op1=ALU.add)
                TS("dve", pc.a[:, 2:3], pidx.a, -1.0, ALU.mult, [pidx.r], [pc.r], s2=127.0, op1=ALU.add)
                CP("dve", pc.a[:, 3:4], pidx.a, [pidx.r], [pc.r])
                qdec = arena.alloc([128, 2, 4], F32, "qdec")
                kdec = arena.alloc([128, 2, 4], F32, "kdec")
                ACT(qdec.a[:, 0, :], lg.a[:, 0:4], AF.Exp, [lg.r, pc.r], [qdec.r], scale=pc.a[:, 0:1])
                ACT(qdec.a[:, 1, :], lg.a[:, 4:8], AF.Exp, [lg.r, pc.r], [qdec.r], scale=pc.a[:, 1:2])
                ACT(kdec.a[:, 0, :], lg.a[:, 0:4], AF.Exp, [lg.r, pc.r], [kdec.r], scale=pc.a[:, 2:3])
                ACT(kdec.a[:, 1, :], lg.a[:, 4:8], AF.Exp, [lg.r, pc.r], [kdec.r], scale=pc.a[:, 3:4])
                lgc = arena.alloc([128, 8], F32, "lgc")
                ACT(lgc.a, lg.a, AF.Exp, [lg.r], [lgc.r], scale=128.0)
                cdcol = arena.alloc([128, 4], F32, "cdcol")
                for d in range(2):
                    for t in range(2):
                        CP("dve", cdcol.a[0:64, d * 2 + t:d * 2 + t + 1], lgc.a[0:64, d * 4 + 2 * t:d * 4 + 2 * t + 1], [lgc.r], [cdcol.r])
                        CP("dve", cdcol.a[64:128, d * 2 + t:d * 2 + t + 1], lgc.a[64:128, d * 4 + 2 * t + 1:d * 4 + 2 * t + 2], [lgc.r], [cdcol.r])
                qT = arena.alloc([128, 2, NT], BF16, "qT")
                kT = arena.alloc([128, 2, NT], BF16, "kT")
                rt = [arena.alloc([128, 512], F32, "rt%d" % i) for i in range(4)]
                cnt = 0
                for which, dst, sc in ((0, qT, 1.0), (1, kT, 0.125)):
                    for t in range(2):
                        for (t0, n) in BLKS:
                            b1 = inproj_fm(wbuf, which * 512 + t * 128, t0, n)
                            if t0 < T:
                                b2 = inproj_fm(wbuf, which * 512 + 256 + t * 128, t0, n)
                                r1, r2 = rt[(cnt * 2) % 4], rt[(cnt * 2 + 1) % 4]
                                cnt += 1
                                STT("dve", r1.a[:, 0:n], b1.a[:, 0:n], sc, rope.a[:, 0, t0:t0 + n], ALU.mult, ALU.mult, [b1.r, rope.r], [r1.r])
                                STT("dve", r2.a[:, 0:n], b2.a[:, 0:n], sc, rope.a[:, 1, t0:t0 + n], ALU.mult, ALU.mult, [b2.r, rope.r], [r2.r])
                                TT("pool", dst.a[:, t, t0:t0 + n], r1.a[:, 0:n], r2.a[:, 0:n], ALU.add, [r1.r, r2.r], [dst.r])
                            else:
                                ACT(dst.a[:, t, t0:t0 + n], b1.a[:, 0:n], AF.Copy, [b1.r], [dst.r], scale=sc)
                v_tok = arena.alloc([128, NCH, 256], BF16, "v_tok")
                kdt = [arena.alloc([128, NCH, 4, 64], BF16, "kdt%d" % i) for i in range(2)]
                for c in range(NCH):
                    b = inproj_tm(wbuf, 1024, 256, c)
                    CP("act" if c % 2 else "dve", v_tok.a[:, c, :], b.a[:, 0:256], [b.r], [v_tok.r])
                    pt = nb()
                    ptb = pt.a.bitcast(BF16)[:, 0:256]
                    for t in range(2):
                        TR(ptb[:, t * 128:(t + 1) * 128], kT.a[:, t, c * 128:(c + 1) * 128], ident_b.a, [kT.r, ident_b.r], [pt.r])
                    for d in range(2):
                        TT("dve", kdt[d].a[:, c], ptb.rearrange("p (h e) -> p h e", h=4), bc(kdec.a[:, d, :], [128, 4, 64], 2), ALU.mult, [pt.r, kdec.r], [kdt[d].r])
                o_acc = arena.alloc([128, NCH, 256], F32, "o_acc")
                kz = [arena.alloc([128, 4, 128], BF16, "kz%d" % i) for i in range(2)]
                att = [arena.alloc([128, 4, 128], BF16, "att%d" % i) for i in range(2)]
                S32s = [arena.alloc([128, 2, 128], F32, "S32_%d" % i) for i in range(2)]
                Sbds = [arena.alloc([128, 2, 128], BF16, "Sbd_%d" % i) for i in range(2)]
                tmpo = [arena.alloc([128, 256], F32, "tmpo%d" % i) for i in range(2)]
                for z_ in kz:
                    MSET("pool", z_.a, 0.0, [z_.r])
                for d in range(2):
                    MSET("pool", S32s[d].a, 0.0, [S32s[d].r])
                    MSET("pool", Sbds[d].a, 0.0, [Sbds[d].r])
                rordF = [16, 17] + list(range(16))
                rordB = [17, 16] + list(range(15, -1, -1))

                def ret_step(c, d):
                    cs = slice(c * 128, (c + 1) * 128)
                    kz_, at_, to_ = kz[d], att[d], tmpo[d]
                    S32, Sbd = S32s[d], Sbds[d]
                    for t in range(2):
                        CP("pool", kz_.a[0:64, 2 * t, :], kT.a[0:64, t, cs], [kT.r], [kz_.r])
                        CP("pool", kz_.a[64:128, 2 * t + 1, :], kT.a[64:128, t, cs], [kT.r], [kz_.r])
                    yield
                    sb_ = nb()
                    for h in range(4):
                        MM(sb_.a[:, h * 128:(h + 1) * 128], kz_.a[:, h, :], qT.a[:, h // 2, cs], True, True, [kz_.r, qT.r], [sb_.r])
                    TT("dve", at_.a, sb_.a.rearrange("p (h i) -> p h i", h=4), DT.a[:, d], ALU.mult, [sb_.r, DT.r], [at_.r])
                    oi = nb()
                    for t in range(2):
                        MM(oi.a[:, t * 128:(t + 1) * 128], qT.a[:, t, cs], Sbd.a[:, t, :], True, True, [qT.r, Sbd.r], [oi.r])
                    yield
                    oa = nb()
                    for h in range(4):
                        MM(oa.a[:, h * 64:(h + 1) * 64], at_.a[:, h, :], v_tok.a[:, c, h * 64:(h + 1) * 64], True, True, [at_.r, v_tok.r], [oa.r])
                    TT("dve", to_.a.rearrange("p (h e) -> p h e", h=4), oi.a[:, 0:256].rearrange("p (h e) -> p h e", h=4), bc(qdec.a[:, d, :], [128, 4, 64], 2), ALU.mult, [oi.r, qdec.r], [to_.r])
                    first = (rordF.index(c) < rordB.index(c)) == (d == 0)
                    if first:
                        TT("dve", o_acc.a[:, c, :], to_.a, oa.a[:, 0:256], ALU.add, [to_.r, oa.r], [o_acc.r])
                    else:
                        TT("dve", to_.a, to_.a, oa.a[:, 0:256], ALU.add, [to_.r, oa.r], [to_.r])
                        TT("pool", o_acc.a[:, c, :], o_acc.a[:, c, :], to_.a, ALU.add, [to_.r, o_acc.r], [o_acc.r])
                    yield
                    for t in range(2):
                        sp_ = nb()
                        MM(sp_.a[:, 0:128], kdt[d].a[:, c, 2 * t:2 * t + 2, :].rearrange("p h e -> p (h e)"), v_tok.a[:, c, t * 128:(t + 1) * 128], True, True, [kdt[d].r, v_tok.r], [sp_.r])
                        STT("dve", S32.a[:, t, :], S32.a[:, t, :], cdcol.a[:, d * 2 + t:d * 2 + t + 1], sp_.a[:, 0:128], ALU.mult, ALU.add, [S32.r, cdcol.r, sp_.r], [S32.r])
                        TT("pool", Sbd.a[:, t, :], S32.a[:, t, :], blockmask.a, ALU.mult, [S32.r, blockmask.r], [Sbd.r])

                def run_il3(gens):
                    gens = list(gens)
                    while gens:
                        for g_ in list(gens):
                            try:
                                next(g_)
                            except StopIteration:
                                gens.remove(g_)

                for s_i in range(NCH):
                    run_il3([ret_step(rordF[s_i], 0), ret_step(rordB[s_i], 1)])
                ycT = qT
                st4 = arena.alloc([128, 8], F32, "st4")
                dti = [arena.alloc([128, 4, 64], F32, "dti%d" % i) for i in range(2)]
                sqi = [arena.alloc([128, 4, 64], F32, "sqi%d" % i) for i in range(2)]
                zsi = [arena.alloc([128, 256], F32, "zsi%d" % i) for i in range(2)]
                yti = [arena.alloc([128, 256], BF16, "yti%d" % i) for i in range(2)]
                stt = [arena.alloc([128, 8], F32, "stt%d" % i) for i in range(2)]
                for c in range(NCH):
                    zb = inproj_tm(wz, 0, 256, c)
                    zs, dt_, sq_, yt_, s_ = zsi[c % 2], dti[c % 2], sqi[c % 2], yti[c % 2], stt[c % 2]
                    ACT(zs.a, zb.a[:, 0:256], AF.Silu, [zb.r], [zs.r])
                    o4 = o_acc.a[:, c, :].rearrange("p (h e) -> p h e", h=4)
                    P.op("dve", lambda e, o=s_.a[:, 0:4], i=o4: e.tensor_reduce(out=o, in_=i, axis=AX.X, op=ALU.add), [o_acc.r], [s_.r])
                    TS("dve", s_.a[:, 0:4], s_.a[:, 0:4], -1.0 / 64, ALU.mult, [s_.r], [s_.r])
                    TT("dve", dt_.a, o4, bc(s_.a[:, 0:4], [128, 4, 64], 2), ALU.add, [o_acc.r, s_.r], [dt_.r])
                    TT("pool", sq_.a, dt_.a, dt_.a, ALU.mult, [dt_.r], [sq_.r])
                    P.op("dve", lambda e, o=s_.a[:, 4:8], i=sq_.a: e.tensor_reduce(out=o, in_=i, axis=AX.X, op=ALU.add), [sq_.r], [s_.r])
                    ACT(s_.a[:, 4:8], s_.a[:, 4:8], AF.Sqrt, [s_.r, cst.r], [s_.r], bias=cst.a[:, 0:1], scale=1.0 / 64)
                    P.op("dve", lambda e, o=s_.a[:, 4:8]: e.reciprocal(out=o, in_=o), [s_.r], [s_.r])
                    TT("dve", dt_.a, dt_.a, bc(s_.a[:, 4:8], [128, 4, 64], 2), ALU.mult, [dt_.r, s_.r], [dt_.r])
                    TT("pool", yt_.a, dt_.a.rearrange("p h e -> p (h e)"), zs.a, ALU.mult, [dt_.r, zs.r], [yt_.r])
                    pt = nb()
                    ptb = pt.a.bitcast(BF16)[:, 0:256]
                    for t in range(2):
                        TR(ptb[:, t * 128:(t + 1) * 128], yt_.a[:, t * 128:(t + 1) * 128], ident_b.a, [yt_.r, ident_b.r], [pt.r])
                    CP("act", ycT.a[:, :, c * 128:(c + 1) * 128], ptb.rearrange("p (t i) -> p t i", t=2), [pt.r], [ycT.r])
                for t in range(2):
                    DMA("sp", ycat_d[4 + t], ycT.a[:, t, :], [ycT.r], [])
                arena.reset(m_mix2)
                P.barrier()

            if "D" in mixers:
                load_w(WD, 1152, wbuf)
                wz = arena.alloc([128, 8, 256], BF16, "wz")
                load_w(WDZ, 256, wz)
                esink = arena.alloc([128, 4], F32, "esink")
                ACT(esink.a, rowt.a[:, 88:92], AF.Exp, [rowt.r], [esink.r])
                qT = arena.alloc([128, 2, NT], BF16, "qT")
                kdT = arena.alloc([128, 2, NT], BF16, "kdT")
                rt = [arena.alloc([128, 512], F32, "rt%d" % i) for i in range(4)]
                cnt = 0
                for which, dst, sc in ((0, qT, 0.125), (1, kdT, 1.0)):
                    for t in range(2):
                        for (t0, n) in BLKS:
                            b1 = inproj_fm(wbuf, which * 512 + t * 128, t0, n)
                            if t0 < T:
                                b2 = inproj_fm(wbuf, which * 512 + 256 + t * 128, t0, n)
                                r1, r2 = rt[(cnt * 2) % 4], rt[(cnt * 2 + 1) % 4]
                                cnt += 1
                                STT("dve", r1.a[:, 0:n], b1.a[:, 0:n], sc, rope.a[:, 2, t0:t0 + n], ALU.mult, ALU.mult, [b1.r, rope.r], [r1.r])
                                STT("dve", r2.a[:, 0:n], b2.a[:, 0:n], sc, rope.a[:, 3, t0:t0 + n], ALU.mult, ALU.mult, [b2.r, rope.r], [r2.r])
                                TT("pool", dst.a[:, t, t0:t0 + n], r1.a[:, 0:n], r2.a[:, 0:n], ALU.add, [r1.r, r2.r], [dst.r])
                            else:
                                ACT(dst.a[:, t, t0:t0 + n], b1.a[:, 0:n], AF.Copy, [b1.r], [dst.r], scale=sc)
                v_aug = arena.alloc([128, NCH, 2, 65], BF16, "v_aug")
                MSET("pool", v_aug.a, 1.0, [v_aug.r])
                for c in range(NCH):
                    b = inproj_tm(wbuf, 1024, 128, c)
                    CP("act" if c % 2 else "dve", v_aug.a[:, c, :, 0:64], b.a[:, 0:128].rearrange("p (h e) -> p h e", h=2), [b.r], [v_aug.r])
                ycT = arena.alloc([128, 2, NT], BF16, "ycT")
                qbd = [arena.alloc([128, 2, 2, 128], BF16, "qbd%d" % i) for i in range(2)]
                for q_ in qbd:
                    MSET("pool", q_.a, 0.0, [q_.r])
                PT = [arena.alloc([128, 4, 128], BF16, "PT%d" % i) for i in range(10)]
                den = [arena.alloc([128, 8], F32, "den%d" % i) for i in range(2)]
                yf = [arena.alloc([128, 4, 64], F32, "yf%d" % i) for i in range(2)]
                zsi = [arena.alloc([128, 256], F32, "zsi%d" % i) for i in range(2)]
                yti = [arena.alloc([128, 256], BF16, "yti%d" % i) for i in range(2)]
                def swa_chunk(qi, n):
                        cs = slice(n * 128, (n + 1) * 128)
                        qb_ = qbd[qi % 2]
                        for kvh in range(2):
                            CP("pool", qb_.a[0:64, kvh, 0, :], qT.a[0:64, kvh, cs], [qT.r], [qb_.r])
                            CP("pool", qb_.a[64:128, kvh, 1, :], qT.a[64:128, kvh, cs], [qT.r], [qb_.r])
                        if n < 16:
                            keys = [m for m in (n - 1, n, n + 1) if 0 <= m <= 15] + [16, 17]
                        else:
                            keys = [16, 17]
                        yield
                        pts = []
                        for mi, m in enumerate(keys):
                            sb_ = nb()
                            for kvh in range(2):
                                MM(sb_.a[:, kvh * 256:(kvh + 1) * 256], kdT.a[:, kvh, m * 128:(m + 1) * 128], qb_.a[:, kvh].rearrange("p g i -> p (g i)"), True, True, [kdT.r, qb_.r], [sb_.r])
                            pt_ = PT[(qi % 2) * 5 + mi]
                            pts.append(pt_)
                            ACT(pt_.a.rearrange("p h i -> p (h i)"), sb_.a, AF.Exp, [sb_.r], [pt_.r])
                            if n < 16 and m == n - 1:
                                TT("pool", pt_.a, pt_.a, bc(maskB.a, [128, 4, 128], 1), ALU.mult, [pt_.r, maskB.r], [pt_.r])
                            elif n < 15 and m == n + 1:
                                TT("pool", pt_.a, pt_.a, bc(maskF.a, [128, 4, 128], 1), ALU.mult, [pt_.r, maskF.r], [pt_.r])
                        yield
                        ob = nb()
                        for hq in range(4):
                            for mi, m in enumerate(keys):
                                MM(ob.a[:, hq * 65:(hq + 1) * 65], pts[mi].a[:, hq, :], v_aug.a[:, m, hq // 2, :], mi == 0, mi == len(keys) - 1, [pts[mi].r, v_aug.r], [ob.r])
                        o4 = ob.a[:, 0:260].rearrange("p (h e) -> p h e", h=4)
                        dn, yf_, zs, yt_ = den[qi % 2], yf[qi % 2], zsi[qi % 2], yti[qi % 2]
                        TT("dve", dn.a[:, 0:4], o4[:, :, 64], esink.a, ALU.add, [ob.r, esink.r], [dn.r])
                        P.op("dve", lambda e, o=dn.a[:, 0:4]: e.reciprocal(out=o, in_=o), [dn.r], [dn.r])
                        TT("dve", yf_.a, o4[:, :, 0:64], bc(dn.a[:, 0:4], [128, 4, 64], 2), ALU.mult, [ob.r, dn.r], [yf_.r])
                        yield
                        zb = inproj_tm(wz, 0, 256, n)
                        ACT(zs.a, zb.a[:, 0:256], AF.Silu, [zb.r], [zs.r])
                        TT("pool", yt_.a, yf_.a.rearrange("p h e -> p (h e)"), zs.a, ALU.mult, [yf_.r, zs.r], [yt_.r])
                        yield
                        pt = nb()
                        ptb = pt.a.bitcast(BF16)[:, 0:256]
                        for t in range(2):
                            TR(ptb[:, t * 128:(t + 1) * 128], yt_.a[:, t * 128:(t + 1) * 128], ident_b.a, [yt_.r, ident_b.r], [pt.r])
                        CP("act", ycT.a[:, :, cs], ptb.rearrange("p (t i) -> p t i", t=2), [pt.r], [ycT.r])

                def run_il2(gens):
                    gens = list(gens)
                    while gens:
                        for g_ in list(gens):
                            try:
                                next(g_)
                            except StopIteration:
                                gens.remove(g_)

                for q0 in range(0, NCH, 2):
                    run_il2([swa_chunk(q0, q0), swa_chunk(q0 + 1, q0 + 1)])
                for t in range(2):
                    DMA("sp", ycat_d[6 + t], ycT.a[:, t, :], [ycT.r], [])
                arena.reset(m_mix2)
                P.barrier()

            arena.reset(m_mix)
            P.barrier()
            if debug and l == n_layers - 1:
                ydb = arena.alloc([128, NT], BF16, "ydb")
                for i in range(8):
                    DMA("sp", ydb.a, ycat_d[i], [], [ydb.r])
                    DMA("sp", dbg_y[i], ydb.a, [ydb.r], [])
                arena.reset(m_mix)
                P.barrier()
            m0 = arena.mark()
            og = P.group(dedicated=True)
            if "D" not in phases:
                DMA("sp", out_d[0:128, :], grow[0].a, [grow[0].r], [])
                continue
            wout = arena.alloc([128, 8, D], BF16, "wout")
            wst = [arena.alloc([128, D], F32, "wst%d" % i) for i in range(2)]
            for k in range(8):
                load_cast(wout.a[:, k, :], wout.r, wout_d[l, k * 128:(k + 1) * 128, :], D, wst[k % 2], wres[l]["out"])
            ylb = [arena.alloc([128, 8, 128], BF16, "yl%d" % i) for i in range(2)]
            xb = [arena.alloc([128, D], F32, "xb%d" % i) for i in range(2)]
            sqfs = [arena.alloc([128, 512], F32, "sqf%d" % i) for i in range(2)]
            t1 = [arena.alloc([128, D], F32, "t1_%d" % i) for i in range(2)]
            xo = [arena.alloc([128, D], F32, "xo%d" % i) for i in range(2)]
            for c in range(NCH):
                if last and c >= 16:
                    continue
                if "1" in phases:
                    continue
                yl = ylb[c % 2]
                if "5" not in phases:
                    DMA("sp", yl.a, ycat_d[:, :, c * 128:(c + 1) * 128].rearrange("k p t -> p k t"), [], [yl.r])
                else:
                    for k_ in range(8):
                        DMA("sp", yl.a[:, k_, :], ycat_d[k_, :, c * 128:(c + 1) * 128], [], [yl.r])
                xt = xb[c % 2]
                if from_x:
                    src = x_d[c * 128:(c + 1) * 128, :] if c < 16 else ctx_d[(c - 16) * 128:(c - 15) * 128, :]
                else:
                    src = xs_d[c * 128:(c + 1) * 128, :]
                DMA("sp", xt.a, src, [], [xt.r])
                w = 0 if c < 16 else 1
                tt_, xo_ = t1[c % 2], xo[c % 2]
                hb = []
                for half in range(2):
                    b = nb()
                    hb.append(b)
                    for k in range(8):
                        MM(b.a, yl.a[:, k, :], wout.a[:, k, half * 512:(half + 1) * 512], k == 0, k == 7, [yl.r, wout.r], [b.r])
                    sqf = sqfs[half]
                    ACT(sqf.a, b.a, AF.Square, [b.r], [sqf.r])
                    P.op("dve", lambda e, o=ss.a[:, 2 * c + half:2 * c + half + 1], i=sqf.a: e.tensor_reduce(out=o, in_=i, axis=AX.X, op=ALU.add), [sqf.r], [ss_r[2 * c + half]])
                    TT("dve", tt_.a[:, half * 512:(half + 1) * 512], b.a, grow[w].a[:, half * 512:(half + 1) * 512], ALU.mult, [b.r, grow[w].r], [tt_.r])
                TT("dve", rstd.a[:, c:c + 1], ss.a[:, 2 * c:2 * c + 1], ss.a[:, 2 * c + 1:2 * c + 2], ALU.add, [ss_r[2 * c], ss_r[2 * c + 1]], [rs_r[c]])
                ACT(rstd.a[:, c:c + 1], rstd.a[:, c:c + 1], AF.Sqrt, [rs_r[c], cst.r], [rs_r[c]], bias=cst.a[:, 0:1], scale=1.0 / D)
                P.op("dve", lambda e, o=rstd.a[:, c:c + 1]: e.reciprocal(out=o, in_=o), [rs_r[c]], [rs_r[c]])
                STT("dve", xo_.a, tt_.a, rstd.a[:, c:c + 1], xt.a, ALU.mult, ALU.add, [tt_.r, rs_r[c], xt.r], [xo_.r])
                if "2" in phases:
                    pass
                elif last:
                    DMA("sp", out_d[c * 128:(c + 1) * 128, :], xo_.a, [xo_.r], [])
                else:
                    DMA("sp", xs_d[c * 128:(c + 1) * 128, :], xo_.a, [xo_.r], [])
                if debug and l == n_layers - 1:
                    DMA("sp", dbg_xs[c * 128:(c + 1) * 128, :], xo_.a, [xo_.r], [])
            arena.reset(m0)
            P.barrier()
        P.barrier()
        P.replay()
        nc._arena_log = arena.log
        print("arena peak words", arena.peak, "ops", {k: len(v) for k, v in P.ops.items()})
    return nc


MIXERS_EXTRA = []


def kernel(**inputs):
    maps = _prep_inputs(inputs, sharded=False)
    nc = build_nc(sharded=False)
    res = run_bass_kernel_spmd(nc, maps, core_ids=list(range(8)))
    return np.stack([np.asarray(r["out"], dtype=np.float32) for r in res.results], axis=0)
```
